# Optimizing a Trainium2 kernel written in Bass

```python
import math
import jax, jax.numpy as jnp
from jax import lax
import numpy as np

D_MODEL = 1024
BATCH = 4
SEQ = 4096
DEPTH = 1

D_SSM = 512
SSM_CH = 16
SSM_GROUPS = D_SSM // SSM_CH
SSM_STATE = 64
D_ATT = 512
HEAD_DIM = 64
N_HEADS = D_ATT // HEAD_DIM
N_KV = 2
HPG = N_HEADS // N_KV
D_KV = N_KV * HEAD_DIM
N_BRANCH = 3
D_IN = D_SSM + D_ATT + 6 * D_KV + N_BRANCH * N_HEADS
D_MIX = D_SSM + D_ATT
CMP_STRIDE = 16
CMP_BLOCK = 2 * CMP_STRIDE
CMP_HIDDEN = 256
SEL_BLOCK = 64
N_SELECT = 16
WINDOW = 512
Q_BLOCK = 128
N_EXP_GROUPS = 4
EXPERTS_PER_GROUP = 8
N_EXPERTS = N_EXP_GROUPS * EXPERTS_PER_GROUP
TOP_K = 2
D_EXPERT = 256

EPS = 1e-6
NEG = -1e30
FORCE = 1e9

kernel_name = "hymba_s5_nsa_hier_moe"


def rms_norm(x, g):
    xf = x.astype(jnp.float32)
    y = xf * lax.rsqrt(jnp.mean(xf * xf, axis=-1, keepdims=True) + EPS)
    return (y * g.astype(jnp.float32)).astype(x.dtype)


def s5_mixer(u, lam_re, lam_im, log_step, b_re, b_im, c_re, c_im, d_skip, w_glu, b_glu):
    f32 = jnp.float32
    bsz, L, _ = u.shape
    ug = u.astype(f32).reshape(bsz, L, SSM_GROUPS, SSM_CH)
    lam = lax.complex(lam_re.astype(f32), lam_im.astype(f32))
    step = jnp.exp(log_step.astype(f32))[:, None]
    lam_bar = jnp.exp(lam * step)
    b = lax.complex(b_re.astype(f32), b_im.astype(f32))
    b_bar = ((lam_bar - 1.0) / lam)[..., None] * b
    bu = lax.complex(jnp.einsum('gph,blgh->blgp', jnp.real(b_bar), ug),
                     jnp.einsum('gph,blgh->blgp', jnp.imag(b_bar), ug))
    a = jnp.broadcast_to(lam_bar, bu.shape)

    def combine(left, right):
        a_l, b_l = left
        a_r, b_r = right
        return a_r * a_l, a_r * b_l + b_r

    _, states = lax.associative_scan(combine, (a, bu), axis=1)
    y = (jnp.einsum('ghp,blgp->blgh', c_re.astype(f32), jnp.real(states))
         - jnp.einsum('ghp,blgp->blgh', c_im.astype(f32), jnp.imag(states))
         + d_skip.astype(f32) * ug)
    y = jax.nn.gelu(y.reshape(bsz, L, D_SSM))
    y = y * jax.nn.sigmoid(y @ w_glu.astype(f32) + b_glu.astype(f32))
    return y.astype(u.dtype)


def compress_blocks(t, pos, w1, w2):
    bsz, L = t.shape[0], t.shape[1]
    chunks = t.reshape(bsz, L // CMP_STRIDE, CMP_STRIDE, N_KV, HEAD_DIM)
    blocks = jnp.concatenate([chunks[:, :-1], chunks[:, 1:]], axis=2)
    blocks = blocks + pos[None, None, :, None, :]
    nc = blocks.shape[1]
    flat = blocks.transpose(0, 1, 3, 2, 4).reshape(bsz, nc, N_KV, CMP_BLOCK * HEAD_DIM)
    return jax.nn.gelu(flat @ w1) @ w2


def nsa_mixer(att, g_q, g_kc, g_ks, g_kw, pos_k, pos_v, w_ck1, w_ck2, w_cv1, w_cv2):
    f32 = jnp.float32
    bsz, L, _ = att.shape
    splits = [D_ATT + i * D_KV for i in range(7)]
    q, kc, vc, ks, vs, kw, vw, gate = jnp.split(att, splits, axis=-1)
    kv_shape = (bsz, L, N_KV, HEAD_DIM)
    q = rms_norm(q.reshape(bsz, L, N_KV, HPG, HEAD_DIM), g_q) * (HEAD_DIM ** -0.5)
    ks = rms_norm(ks.reshape(kv_shape), g_ks)
    vs = vs.reshape(kv_shape)
    kw = rms_norm(kw.reshape(kv_shape), g_kw)
    vw = vw.reshape(kv_shape)
    qpos = jnp.arange(L)

    kcmp = rms_norm(compress_blocks(kc.reshape(kv_shape), pos_k, w_ck1, w_ck2), g_kc)
    vcmp = compress_blocks(vc.reshape(kv_shape), pos_v, w_cv1, w_cv2)
    nc = kcmp.shape[1]
    cstart = jnp.arange(nc) * CMP_STRIDE
    cmask = (cstart[None, :] + CMP_BLOCK - 1) <= qpos[:, None]
    s = jnp.einsum('blghd,bcgd->bghlc', q, kcmp).astype(f32)
    s = jnp.where(cmask, s, NEG)
    p_cmp = jax.nn.softmax(s, axis=-1) * jnp.any(cmask, axis=-1)[:, None].astype(f32)
    o_cmp = jnp.einsum('bghlc,bcgd->blghd', p_cmp.astype(vcmp.dtype), vcmp)

    ns = L // SEL_BLOCK
    sstart = jnp.arange(ns) * SEL_BLOCK
    overlap = ((cstart[:, None] < sstart[None, :] + SEL_BLOCK)
               & (cstart[:, None] + CMP_BLOCK > sstart[None, :])).astype(f32)
    imp = jnp.einsum('bghlc,cs->bgls', p_cmp, overlap)
    cur = qpos // SEL_BLOCK
    blk = jnp.arange(ns)
    valid = blk[None, :] <= cur[:, None]
    forced = (blk[None, :] == 0) | (blk[None, :] == cur[:, None]) | (blk[None, :] == cur[:, None] - 1)
    imp = jnp.where(forced, FORCE, jnp.where(valid, imp, NEG))
    n_sel = min(N_SELECT, ns)
    _, idx = lax.top_k(imp, n_sel)

    ks_blk = ks.reshape(bsz, ns, SEL_BLOCK, N_KV, HEAD_DIM).transpose(0, 3, 1, 2, 4)
    vs_blk = vs.reshape(bsz, ns, SEL_BLOCK, N_KV, HEAD_DIM).transpose(0, 3, 1, 2, 4)
    nq = L // Q_BLOCK
    q_chunks = q.reshape(bsz, nq, Q_BLOCK, N_KV, HPG, HEAD_DIM).transpose(1, 0, 2, 3, 4, 5)
    idx_chunks = idx.reshape(bsz, N_KV, nq, Q_BLOCK, n_sel).transpose(2, 0, 1, 3, 4)
    starts = jnp.arange(nq) * Q_BLOCK
    bi = jnp.arange(bsz)[:, None, None, None]
    gi = jnp.arange(N_KV)[None, :, None, None]

    def sel_block(args):
        qc, ic, st = args
        kb = ks_blk[bi, gi, ic]
        vb = vs_blk[bi, gi, ic]
        kpos = ic[..., None] * SEL_BLOCK + jnp.arange(SEL_BLOCK)
        tpos = st + jnp.arange(Q_BLOCK)
        m = kpos <= tpos[None, None, :, None, None]
        sc = jnp.einsum('bqghd,bgqnkd->bghqnk', qc, kb).astype(f32)
        sc = jnp.where(m[:, :, None], sc, NEG)
        sc = sc.reshape(bsz, N_KV, HPG, Q_BLOCK, n_sel * SEL_BLOCK)
        pr = jax.nn.softmax(sc, axis=-1).reshape(bsz, N_KV, HPG, Q_BLOCK, n_sel, SEL_BLOCK)
        return jnp.einsum('bghqnk,bgqnkd->bqghd', pr.astype(vb.dtype), vb)

    o_sel = lax.map(sel_block, (q_chunks, idx_chunks, starts))
    o_sel = o_sel.transpose(1, 0, 2, 3, 4, 5).reshape(bsz, L, N_KV, HPG, HEAD_DIM)

    pad = WINDOW // Q_BLOCK
    kw_p = jnp.pad(kw, ((0, 0), (WINDOW, 0), (0, 0), (0, 0))).reshape(bsz, nq + pad, Q_BLOCK, N_KV, HEAD_DIM)
    vw_p = jnp.pad(vw, ((0, 0), (WINDOW, 0), (0, 0), (0, 0))).reshape(bsz, nq + pad, Q_BLOCK, N_KV, HEAD_DIM)
    kw_band = jnp.concatenate([kw_p[:, i:i + nq] for i in range(pad + 1)], axis=2)
    vw_band = jnp.concatenate([vw_p[:, i:i + nq] for i in range(pad + 1)], axis=2)
    qb = q.reshape(bsz, nq, Q_BLOCK, N_KV, HPG, HEAD_DIM)
    sw = jnp.einsum('bnqghd,bnkgd->bnghqk', qb, kw_band).astype(f32)
    qp = jnp.arange(nq)[:, None] * Q_BLOCK + jnp.arange(Q_BLOCK)[None, :]
    kp = jnp.arange(nq)[:, None] * Q_BLOCK - WINDOW + jnp.arange((pad + 1) * Q_BLOCK)[None, :]
    wm = ((kp[:, None, :] <= qp[:, :, None]) & (kp[:, None, :] > qp[:, :, None] - WINDOW)
          & (kp[:, None, :] >= 0))
    sw = jnp.where(wm[None, :, None, None], sw, NEG)
    pw = jax.nn.softmax(sw, axis=-1)
    o_win = jnp.einsum('bnghqk,bnkgd->bnqghd', pw.astype(vw_band.dtype), vw_band)
    o_win = o_win.reshape(bsz, L, N_KV, HPG, HEAD_DIM)

    g = jax.nn.sigmoid(gate.astype(f32)).reshape(bsz, L, N_KV, HPG, N_BRANCH).astype(att.dtype)
    o = g[..., 0:1] * o_cmp + g[..., 1:2] * o_sel + g[..., 2:3] * o_win
    return o.reshape(bsz, L, D_ATT)


def hier_moe(h, w_grp, b_grp, w_exp, b_exp, w_gate, w_up, w_down):
    f32 = jnp.float32
    bsz, L, D = h.shape
    t = h.reshape(-1, D)
    n_tok = t.shape[0]
    glog = (t @ w_grp + b_grp).astype(f32)
    gprob = jax.nn.softmax(glog, axis=-1)
    gsel = jnp.argmax(glog, axis=-1)
    elog = (t @ w_exp + b_exp).astype(f32).reshape(n_tok, N_EXP_GROUPS, EXPERTS_PER_GROUP)
    elog_sel = jnp.take_along_axis(elog, gsel[:, None, None], axis=1)[:, 0]
    eprob = jax.nn.softmax(elog_sel, axis=-1)
    topv, topi = lax.top_k(eprob, TOP_K)
    topv = topv / jnp.sum(topv, axis=-1, keepdims=True)
    wsel = topv * jnp.take_along_axis(gprob, gsel[:, None], axis=1)
    eid = gsel[:, None] * EXPERTS_PER_GROUP + topi
    comb = jnp.sum(jax.nn.one_hot(eid, N_EXPERTS, dtype=f32) * wsel[..., None], axis=1)
    out = jnp.zeros((n_tok, D), f32)
    for e in range(N_EXPERTS):
        a = jax.nn.silu(t @ w_gate[e]) * (t @ w_up[e])
        out = out + comb[:, e:e + 1] * (a @ w_down[e]).astype(f32)
    return out.reshape(bsz, L, D).astype(h.dtype)


def setup_inputs(seed: int = 0) -> dict:
    key = jax.random.key(seed)
    ks = jax.random.split(key, 34)
    nrm = lambda k, shape, s: jax.random.normal(k, shape, jnp.float32) * s
    L = DEPTH
    G, P, H = SSM_GROUPS, SSM_STATE, SSM_CH
    lam_im0 = jnp.broadcast_to(jnp.pi * jnp.arange(P, dtype=jnp.float32), (L, G, P))
    return {
        "x": nrm(ks[0], (BATCH, SEQ, D_MODEL), 1.0),
        "norm1_g": 1.0 + nrm(ks[1], (L, D_MODEL), 0.02),
        "w_in": nrm(ks[2], (L, D_MODEL, D_IN), D_MODEL ** -0.5),
        "lam_re": -0.5 + nrm(ks[3], (L, G, P), 0.01),
        "lam_im": lam_im0 + nrm(ks[4], (L, G, P), 0.01),
        "log_step": jax.random.uniform(ks[5], (L, G), jnp.float32, math.log(1e-3), math.log(1e-1)),
        "b_re": nrm(ks[6], (L, G, P, H), (2 * H) ** -0.5),
        "b_im": nrm(ks[7], (L, G, P, H), (2 * H) ** -0.5),
        "c_re": nrm(ks[8], (L, G, H, P), (2 * P) ** -0.5),
        "c_im": nrm(ks[9], (L, G, H, P), (2 * P) ** -0.5),
        "d_skip": nrm(ks[10], (L, G, H), 1.0),
        "w_glu": nrm(ks[11], (L, D_SSM, D_SSM), D_SSM ** -0.5),
        "b_glu": nrm(ks[12], (L, D_SSM), 0.01),
        "g_q": 1.0 + nrm(ks[13], (L, HEAD_DIM), 0.02),
        "g_kc": 1.0 + nrm(ks[14], (L, HEAD_DIM), 0.02),
        "g_ks": 1.0 + nrm(ks[15], (L, HEAD_DIM), 0.02),
        "g_kw": 1.0 + nrm(ks[16], (L, HEAD_DIM), 0.02),
        "pos_k": nrm(ks[17], (L, CMP_BLOCK, HEAD_DIM), 0.02),
        "pos_v": nrm(ks[18], (L, CMP_BLOCK, HEAD_DIM), 0.02),
        "w_ck1": nrm(ks[19], (L, CMP_BLOCK * HEAD_DIM, CMP_HIDDEN), (CMP_BLOCK * HEAD_DIM) ** -0.5),
        "w_ck2": nrm(ks[20], (L, CMP_HIDDEN, HEAD_DIM), CMP_HIDDEN ** -0.5),
        "w_cv1": nrm(ks[21], (L, CMP_BLOCK * HEAD_DIM, CMP_HIDDEN), (CMP_BLOCK * HEAD_DIM) ** -0.5),
        "w_cv2": nrm(ks[22], (L, CMP_HIDDEN, HEAD_DIM), CMP_HIDDEN ** -0.5),
        "out_g_ssm": 1.0 + nrm(ks[23], (L, D_SSM), 0.02),
        "out_g_att": 1.0 + nrm(ks[24], (L, D_ATT), 0.02),
        "w_out": nrm(ks[25], (L, D_MIX, D_MODEL), D_MIX ** -0.5),
        "norm2_g": 1.0 + nrm(ks[26], (L, D_MODEL), 0.02),
        "w_grp": nrm(ks[27], (L, D_MODEL, N_EXP_GROUPS), D_MODEL ** -0.5),
        "b_grp": nrm(ks[28], (L, N_EXP_GROUPS), 0.01),
        "w_exp": nrm(ks[29], (L, D_MODEL, N_EXPERTS), D_MODEL ** -0.5),
        "b_exp": nrm(ks[30], (L, N_EXPERTS), 0.01),
        "w_gate": nrm(ks[31], (L, N_EXPERTS, D_MODEL, D_EXPERT), D_MODEL ** -0.5),
        "w_up": nrm(ks[32], (L, N_EXPERTS, D_MODEL, D_EXPERT), D_MODEL ** -0.5),
        "w_down": nrm(ks[33], (L, N_EXPERTS, D_EXPERT, D_MODEL), D_EXPERT ** -0.5),
    }


def reference(x, norm1_g, w_in, lam_re, lam_im, log_step, b_re, b_im, c_re, c_im, d_skip,
              w_glu, b_glu, g_q, g_kc, g_ks, g_kw, pos_k, pos_v, w_ck1, w_ck2, w_cv1, w_cv2,
              out_g_ssm, out_g_att, w_out, norm2_g, w_grp, b_grp, w_exp, b_exp,
              w_gate, w_up, w_down):
    for l in range(DEPTH):
        h = rms_norm(x, norm1_g[l])
        proj = h @ w_in[l]
        y_ssm = s5_mixer(proj[..., :D_SSM], lam_re[l], lam_im[l], log_step[l], b_re[l], b_im[l],
                         c_re[l], c_im[l], d_skip[l], w_glu[l], b_glu[l])
        y_att = nsa_mixer(proj[..., D_SSM:], g_q[l], g_kc[l], g_ks[l], g_kw[l], pos_k[l], pos_v[l],
                          w_ck1[l], w_ck2[l], w_cv1[l], w_cv2[l])
        mix = jnp.concatenate([rms_norm(y_ssm, out_g_ssm[l]), rms_norm(y_att, out_g_att[l])], axis=-1)
        x = x + mix @ w_out[l]
        x = x + hier_moe(rms_norm(x, norm2_g[l]), w_grp[l], b_grp[l], w_exp[l], b_exp[l],
                         w_gate[l], w_up[l], w_down[l])
    return x
```

```python
import math
from contextlib import ExitStack
from functools import partial

import numpy as np
import concourse.bass as bass
import concourse.mybir as mybir
from concourse.bass_utils import run_bass_kernel_spmd

F32 = mybir.dt.float32
BF16 = mybir.dt.bfloat16
I32 = mybir.dt.int32
ALU = mybir.AluOpType
AF = mybir.ActivationFunctionType
AX = mybir.AxisListType

BIG = 250.0
EPS = 1e-6
PI = math.pi
TCH = 256
NT = 32
NOWN = 16


class Reg:
    __slots__ = ("w", "r")

    def __init__(self):
        self.w = None
        self.r = []


class Op:
    __slots__ = ("idx", "eng", "fn", "preds", "dur", "lat", "ev", "nun", "succ", "ready", "fin", "is_dma", "ph", "prio")


def _free_elems(ap):
    n = 1
    for s in ap.shape[1:]:
        n *= int(s)
    return n


def _c0(f):
    return f()


def _c1(f):
    return f()


def _c2(f):
    return f()


def _c3(f):
    return f()


def _c4(f):
    return f()


def _c5(f):
    return f()


def _c6(f):
    return f()


def _c7(f):
    return f()


_CALLERS = [_c0, _c1, _c2, _c3, _c4, _c5, _c6, _c7]


class KB:
    NDMA = 24
    SCHED = True

    def __init__(self, nc, es):
        self.nc = nc
        self.eng = {"pe": nc.tensor, "act": nc.scalar, "dve": nc.vector,
                    "pool": nc.gpsimd, "sp": nc.sync}
        self.sems = {}
        self.cnt = {}
        for k in ("pe", "act", "dve", "pool"):
            self.sems[k] = es.enter_context(nc.semaphore("s_" + k))
            self.cnt[k] = 0
        for i in range(self.NDMA):
            k = "d%d" % i
            self.sems[k] = es.enter_context(nc.semaphore("s_" + k))
            self.cnt[k] = 0
        self.seen = {e: {} for e in self.eng}
        self.dma_rr = 0
        self.pending = []
        self.nops = 0
        self.phase = 0
        self.sim_log = []
        self.mute = False
        self.ck_limit = 99
        self.prio = 0

    def _wait(self, e, ev):
        if ev is None:
            return
        k, v = ev
        if e == "pe" and k == "pe":
            return
        if self.seen[e].get(k, 0) >= v:
            return
        self.eng[e].wait_ge(self.sems[k], v)
        self.seen[e][k] = v

    def _mk(self, e, reads, writes, fn, is_dma):
        o = Op()
        o.idx = self.nops
        self.nops += 1
        o.eng = e
        o.fn = fn
        o.ev = None
        o.is_dma = is_dma
        o.ph = self.phase
        o.prio = self.prio
        o.dur = 0.1
        o.lat = 0.0
        preds = {}
        for rg in reads:
            if rg.w is not None:
                preds[id(rg.w)] = rg.w
        for rg in writes:
            if rg.w is not None:
                preds[id(rg.w)] = rg.w
            for x in rg.r:
                preds[id(x)] = x
        preds.pop(id(o), None)
        o.preds = list(preds.values())
        for rg in reads:
            rg.r.append(o)
        for rg in writes:
            rg.w = o
            rg.r = []
        if self.mute:
            o.ev = ("pe", 0)
            o.preds = None
            return o
        self.pending.append(o)
        if not self.SCHED:
            self.flush()
        return o

    def op(self, e, reads, writes, fn):
        o = self._mk(e, reads, writes, fn, False)
        try:
            out = fn.keywords.get("out", fn.args[0] if fn.args else None)
            n = _free_elems(out)
            nm = fn.func.__name__
        except Exception:
            n, nm = 256, ""
        if e == "pe":
            o.dur = 0.035 + n / 2400.0
        elif e == "act":
            o.dur = 0.2 + n * 0.00105
        elif e == "dve":
            o.dur = 0.1 + n * (0.0029 if "scan" in nm else 0.0013)
        else:
            o.dur = 0.18 + n * 0.00212
        return o

    def dma(self, out, in_, reads, writes, q="sp", **kw):
        o = self._mk(q, reads, writes, partial(self.eng[q].dma_start, out=out, in_=in_, **kw), True)
        try:
            nb = 1
            for s in out.shape:
                nb *= int(s)
            nb *= 4
        except Exception:
            nb = 65536
        o.dur = 0.06
        o.lat = 2.0 + nb / 60000.0
        return o

    def _emit(self, o):
        e = o.eng
        if o.is_dma:
            dk = "d%d" % self.dma_rr
            self.dma_rr = (self.dma_rr + 1) % self.NDMA
            if self.cnt[dk] > 0:
                self._wait(e, (dk, self.cnt[dk]))
            for p in o.preds:
                self._wait(e, p.ev)
            ins = _CALLERS[o.ph](o.fn)
            self.cnt[dk] += 16
            ins.then_inc(self.sems[dk], 16)
            o.ev = (dk, self.cnt[dk])
        else:
            for p in o.preds:
                self._wait(e, p.ev)
            ins = _CALLERS[o.ph](o.fn)
            self.cnt[e] += 1
            ins.then_inc(self.sems[e], 1)
            o.ev = (e, self.cnt[e])
        o.fn = None
        o.preds = None
        o.succ = None

    def flush(self):
        ops = self.pending
        self.pending = []
        if not ops:
            return
        if len(ops) == 1:
            self._emit(ops[0])
            return
        for o in ops:
            o.succ = []
            o.ready = 0.0
        for o in ops:
            o.nun = 0
            for p in o.preds:
                if p.ev is None:
                    o.nun += 1
                    p.succ.append(o)
        tfree = {e: 0.0 for e in self.eng}
        cand = {e: [] for e in self.eng}
        for o in ops:
            if o.nun == 0:
                cand[o.eng].append(o)
        dma_fin = []
        nleft = len(ops)
        ndma = self.NDMA
        order = []
        while nleft:
            best = None
            bo = None
            for e, cl in cand.items():
                if not cl:
                    continue
                tf = tfree[e]
                if e == "sp" and len(dma_fin) >= ndma and dma_fin[-ndma] > tf:
                    tf = dma_fin[-ndma]
                for o in cl:
                    st = o.ready if o.ready > tf else tf
                    key = (st, -o.prio, o.idx)
                    if best is None or key < best:
                        best = key
                        bo = o
            o = bo
            st = best[0]
            e = o.eng
            cand[e].remove(o)
            tfree[e] = st + o.dur
            if o.is_dma:
                o.fin = st + o.lat
                dma_fin.append(o.fin)
            else:
                o.fin = tfree[e]
            order.append(o)
            for s in o.succ:
                if o.fin > s.ready:
                    s.ready = o.fin
                s.nun -= 1
                if s.nun == 0:
                    cand[s.eng].append(s)
            nleft -= 1
        self.sim_log.append((order[0].ph, len(order), max(o.fin for o in order), dict(tfree)))
        for o in order:
            self._emit(o)

    def ckpt(self, k):
        if k > self.ck_limit:
            self.mute = True

    def barrier(self, engines=("pe", "act", "dve", "pool", "sp")):
        self.mute = False
        self.flush()
        for e in engines:
            for k in self.sems:
                if self.cnt[k] > 0:
                    self._wait(e, (k, self.cnt[k]))


def build_nc(dbg=(), stage=99, ssm_stop=99, ck=99):
    nc = bass.Bass("TRN2", target_bir_lowering=False)
    D = {}

    def din(name, shape):
        D[name] = nc.dram_tensor(name, list(shape), F32, kind="ExternalInput").ap()
        return D[name]

    xp = din("xp", [4096, 1024])
    xown = din("xown", [2048, 1024])
    din("norm1_g", [1024]); din("w_in", [1024, 1816])
    din("lam_re", [32, 64]); din("lam_im", [32, 64]); din("log_step", [32])
    din("b_re", [32, 64, 16]); din("b_im", [32, 64, 16])
    din("c_re", [32, 16, 64]); din("c_im", [32, 16, 64]); din("d_skip", [32, 16])
    din("w_glu", [512, 512]); din("b_glu", [512])
    din("g_q", [64]); din("g_kc", [64]); din("g_ks", [64]); din("g_kw", [64])
    din("pos_k", [32, 64]); din("pos_v", [32, 64])
    din("w_ck1", [2048, 256]); din("w_ck2", [256, 64]); din("w_cv1", [2048, 256]); din("w_cv2", [256, 64])
    din("out_g_ssm", [512]); din("out_g_att", [512]); din("w_out", [1024, 1024])
    din("norm2_g", [1024]); din("w_grp", [1024, 4]); din("b_grp", [4]); din("w_exp", [1024, 32]); din("b_exp", [32])
    din("w_gate", [32, 1024, 256]); din("w_up", [32, 1024, 256]); din("w_down", [32, 256, 1024])
    din("c_ident", [128, 128]); din("c_tlb", [128, 128]); din("c_sub", [128, 128])
    din("c_ov", [256, 65]); din("c_dmat", [128, 128]); din("c_eband", [64, 4096])
    din("c_kbtok", [128, 32]); din("c_kbcmp", [128, 2]); din("c_f0", [128, 64])
    din("c_iota", [128, 256]); din("c_kall", [128, 24]); din("c_kab", [128, 24]); din("c_tmask", [128, 128])
    out = nc.dram_tensor("out", [2048, 1024], F32, kind="ExternalOutput").ap()
    x1d = nc.dram_tensor("x1d", [2048, 1024], F32, kind="Internal").ap()
    tTd = nc.dram_tensor("tTd", [128, 8, 2048], BF16, kind="Internal").ap()
    uTM = nc.dram_tensor("uTMd", [4096, 512], BF16, kind="Internal").ap()
    woutd = nc.dram_tensor("woutd", [128, 8, 1024], BF16, kind="Internal").ap()
    r_woutd = [Reg() for _ in range(8)]
    ysTd = nc.dram_tensor("ysTd", [128, 4, 2048], BF16, kind="Internal").ap()
    oattTd = nc.dram_tensor("oattTd", [128, 4, 2048], BF16, kind="Internal").ap()
    dbg_out = {}
    r_x1d = [Reg() for _ in range(NOWN)]
    r_tTd = [Reg() for _ in range(NOWN)]

    def dout(name, shape, dt=F32):
        dbg_out[name] = nc.dram_tensor("dbg_" + name, list(shape), dt, kind="ExternalOutput").ap()
        return dbg_out[name]

    with ExitStack() as es:
        kb = KB(nc, es)
        kb.ck_limit = ck
        V, G, A, PE = nc.vector, nc.gpsimd, nc.scalar, nc.tensor

        def T(st, name, shape, dt=F32):
            return st.enter_context(nc.sbuf_tensor(name, list(shape), dt))

        pb = [es.enter_context(nc.psum_tensor("pb%d" % i, [128, 512], F32)) for i in range(7)]
        r_pb = [Reg() for _ in range(7)]
        pT = es.enter_context(nc.psum_tensor("pT", [128, 1024], BF16))
        r_pT = Reg()

        idf = T(es, "idf", [128, 128]); r_idf = Reg()
        idb = T(es, "idb", [128, 128], BF16); r_idb = Reg()
        tlb4 = T(es, "tlb4", [128, 4, 128], BF16); r_tlb4 = Reg()
        sub4 = T(es, "sub4", [128, 4, 128], BF16); r_sub4 = Reg()
        dmat = T(es, "dmat", [128, 128]); r_dmat = Reg()
        ovb = T(es, "ovb", [128, 2, 65], BF16); r_ovb = Reg()
        kbtok = T(es, "kbtok", [128, 32]); r_kbtok = Reg()
        kbcmp = T(es, "kbcmp", [128, 2]); r_kbcmp = Reg()
        f0 = T(es, "f0", [128, 64]); r_f0 = Reg()
        gcols = T(es, "gcols", [128, 4]); r_gcols = Reg()
        comb = T(es, "comb", [128, NOWN, 32]); r_comb = [Reg() for _ in range(NOWN)]
        ss_ssm = T(es, "ss_ssm", [128, NOWN]); r_ss_ssm = [Reg() for _ in range(NOWN)]
        ss_att = T(es, "ss_att", [128, NOWN]); r_ss_att = [Reg() for _ in range(NOWN)]
        cst = T(es, "cst", [128, 512]); r_cst = Reg()
        onesf = T(es, "onesf", [128, 1]); r_onesf = Reg()

        kb.dma(idf[:], D["c_ident"], [], [r_idf])
        kb.dma(dmat[:], D["c_dmat"], [], [r_dmat])
        kb.dma(kbtok[:], D["c_kbtok"], [], [r_kbtok])
        kb.dma(kbcmp[:], D["c_kbcmp"], [], [r_kbcmp])
        kb.dma(f0[:], D["c_f0"], [], [r_f0])
        kb.op("dve", [r_idf], [r_idb], partial(V.tensor_copy, idb[:], idf[:]))
        kb.op("dve", [], [r_onesf], partial(V.memset, onesf[:], 1.0))
        kb.dma(cst[:, 0:128], D["c_tlb"], [], [r_cst])
        kb.dma(cst[:, 128:256], D["c_sub"], [], [r_cst])
        kb.op("dve", [r_cst], [r_tlb4], partial(V.tensor_copy, tlb4[:], cst[:, 0:128].unsqueeze(1).to_broadcast([128, 4, 128])))
        kb.op("dve", [r_cst], [r_sub4], partial(V.tensor_copy, sub4[:], cst[:, 128:256].unsqueeze(1).to_broadcast([128, 4, 128])))
        kb.dma(cst[:, 256:386].rearrange("p (j n) -> p j n", j=2), D["c_ov"].rearrange("(j p) n -> p j n", p=128), [], [r_cst])
        kb.op("dve", [r_cst], [r_ovb], partial(V.tensor_copy, ovb[:], cst[:, 256:386].rearrange("p (j n) -> p j n", j=2)))
        for gi, gname in enumerate(("g_q", "g_kc", "g_ks", "g_kw")):
            src = D[gname].rearrange("(d o) -> d o", o=1)
            kb.dma(gcols[0:64, gi:gi + 1], src, [], [r_gcols])
            kb.dma(gcols[64:128, gi:gi + 1], src, [], [r_gcols])
        kb.op("dve", [r_gcols], [r_gcols], partial(V.tensor_scalar, gcols[:, 0:1], gcols[:, 0:1], 0.125, None, op0=ALU.mult))

        rowst = T(es, "rowst", [16, 128]); r_rowst = Reg()

        def load_cols(dst, r_dst, src1d, n, bank=6):
            kb.dma(rowst[0:n, :], src1d.rearrange("(c p) -> c p", p=128), [], [r_rowst])
            kb.op("pe", [r_rowst, r_idf], [r_pb[bank]], partial(PE.transpose, pb[bank][:, 0:n], rowst[0:n, :], idf[0:n, 0:n]))
            kb.op("dve", [r_pb[bank]], [r_dst], partial(V.tensor_copy, dst, pb[bank][:, 0:n]))

        def rstd_from_ss(ss_ap, n, regs):
            kb.op("act", regs, regs, partial(A.activation, out=ss_ap, in_=ss_ap, func=AF.Sqrt, scale=1.0 / n, bias=EPS))
            kb.op("dve", regs, regs, partial(V.reciprocal, ss_ap, ss_ap))

        def rstd_lnexp(ss_ap, n, regs):
            kb.op("act", regs, regs, partial(A.activation, out=ss_ap, in_=ss_ap, func=AF.Ln, scale=1.0 / n, bias=EPS))
            kb.op("act", regs, regs, partial(A.activation, out=ss_ap, in_=ss_ap, func=AF.Exp, scale=-0.5))

        GK = 1.5957691216057308

        def gelu_ops(out_ap, y_ap, t1, t2, regs_in, r_t, r_out, shape_view=None):
            kb.op("act", regs_in, [r_t], partial(A.activation, out=t1, in_=y_ap, func=AF.Square, scale=math.sqrt(0.044715)))
            kb.op("dve", regs_in + [r_t], [r_t], partial(V.scalar_tensor_tensor, out=t1, in0=t1, scalar=1.0, in1=y_ap, op0=ALU.add, op1=ALU.mult))
            kb.op("act", [r_t], [r_t], partial(A.activation, out=t2, in_=t1, func=AF.Sigmoid, scale=GK))
            kb.op("dve", regs_in + [r_t], [r_out], partial(V.tensor_tensor, out_ap, y_ap, t2, op=ALU.mult))

        def phase_compress(L):
            kb.phase = 2
            with ExitStack() as LC:
                W1 = T(LC, "W1", [128, 32, 256], BF16); r_W1 = Reg()
                W2 = T(LC, "W2", [128, 2, 2, 64], BF16); r_W2 = Reg()
                posT = T(LC, "posT", [128, 32], BF16); r_posT = Reg()
                hcb = T(LC, "hcb", [128, 4]); r_hcb = Reg()
                hcT = T(LC, "hcT", [128, 2, 256], BF16); r_hcT = Reg()
                w1s = [T(LC, "w1s%d" % i, [128, 8, 256]) for i in range(2)]; r_w1s = [Reg(), Reg()]
                w2s = T(LC, "w2s", [128, 2, 2, 64]); r_w2s = Reg()
                pst = T(LC, "pst", [32, 128]); r_pst = Reg()
                yb = T(LC, "cyb", [128, 256]); r_yb = Reg()
                ct1 = T(LC, "ct1", [128, 256]); ct2 = T(LC, "ct2", [128, 256]); r_ct = Reg()
                csq = T(LC, "csq", [128, 64]); r_csq = Reg()
                css = T(LC, "css", [128, 1]); r_css = Reg()
                ckn = T(LC, "ckn", [128, 64], BF16); r_ckn = Reg()
                kb.op("pool", [], [r_hcT], partial(G.memset, hcT[:], 0.0))
                srcs = (D["w_ck1"].rearrange("(j d) h -> d j h", d=64), D["w_cv1"].rearrange("(j d) h -> d j h", d=64))
                for q4 in range(4):
                    ws, rw = w1s[q4 % 2], r_w1s[q4 % 2]
                    for kv in range(2):
                        kb.dma(ws[64 * kv:64 * kv + 64, :, :], srcs[kv][:, q4 * 8:(q4 + 1) * 8, :], [], [rw])
                    kb.op("dve", [rw], [r_W1], partial(V.tensor_copy, W1[:, q4 * 8:(q4 + 1) * 8, :], ws[:]))
                for kv, nm in enumerate(("w_ck2", "w_cv2")):
                    kb.dma(w2s[:, kv, :, :], D[nm].rearrange("(m p) d -> p m d", p=128), [], [r_w2s])
                kb.op("dve", [r_w2s], [r_W2], partial(V.tensor_copy, W2[:], w2s[:]))
                kb.dma(pst[:, 0:64], D["pos_k"], [], [r_pst])
                kb.dma(pst[:, 64:128], D["pos_v"], [], [r_pst])
                kb.op("pe", [r_pst, r_idf], [r_pb[6]], partial(PE.transpose, pb[6][:, 0:32], pst[:], idf[0:32, 0:32]))
                kb.op("dve", [r_pb[6]], [r_posT], partial(V.tensor_copy, posT[:], pb[6][:, 0:32]))
                for kv in range(2):
                    for m in range(2):
                        idx = kv * 2 + m
                        for j in range(32):
                            kb.op("pe", [r_W1, r_posT], [r_pb[5]], partial(PE.matmul,
                                pb[5][:, idx:idx + 1], lhsT=W1[64 * kv:64 * kv + 64, j, m * 128:(m + 1) * 128],
                                rhs=posT[64 * kv:64 * kv + 64, j:j + 1], start=(j == 0), stop=(j == 31)))
                kb.op("dve", [r_pb[5]], [r_hcb], partial(V.tensor_copy, hcb[:], pb[5][:, 0:4]))
                nb = 0
                for kv in range(2):
                    for g in range(2):
                        for m in range(2):
                            bk, rbk = pb[nb % 2], r_pb[nb % 2]; nb += 1
                            for j in range(32):
                                kb.op("pe", [r_W1] + r_kcv, [rbk], partial(PE.matmul,
                                    bk[:, 0:255], lhsT=W1[64 * kv:64 * kv + 64, j, m * 128:(m + 1) * 128],
                                    rhs=kcvT[64 * kv:64 * kv + 64, g, j % 16, (j // 16):(j // 16) + 255], start=(j == 0), stop=(j == 31)))
                            kb.op("act", [rbk, r_hcb], [r_yb], partial(A.activation,
                                out=yb[:, 0:255], in_=bk[:, 0:255], func=AF.Identity, bias=hcb[:, kv * 2 + m:kv * 2 + m + 1], scale=1.0))
                            gelu_ops(hcT[:, m, 0:255], yb[:, 0:255], ct1[:, 0:255], ct2[:, 0:255], [r_yb], r_ct, r_hcT)
                        for ct in range(2):
                            b2, rb2 = pb[2 + ct], r_pb[2 + ct]
                            for m in range(2):
                                kb.op("pe", [r_hcT, r_W2], [rb2], partial(PE.matmul,
                                    b2[:, 0:64], lhsT=hcT[:, m, ct * 128:(ct + 1) * 128], rhs=W2[:, kv, m, :], start=(m == 0), stop=(m == 1)))
                            if kv == 1:
                                kb.op("act", [rb2], [r_cmp], partial(A.copy, Vc[:, ct, g, 0:64], b2[:, 0:64]))
                            else:
                                kb.op("act", [rb2], [r_csq], partial(A.activation, out=csq[:], in_=b2[:, 0:64], func=AF.Square))
                                kb.op("dve", [r_csq], [r_css], partial(V.tensor_reduce, out=css[:], in_=csq[:], axis=AX.X, op=ALU.add))
                                rstd_from_ss(css[:], 64, [r_css])
                                kb.op("dve", [rb2, r_css], [r_ckn], partial(V.tensor_scalar, ckn[:], b2[:, 0:64], css[:, 0:1], None, op0=ALU.mult))
                                kb.op("pe", [r_ckn, r_idb], [r_pT], partial(PE.transpose, pT[0:64, 0:128], ckn[:], idb[:]))
                                kb.op("act", [r_pT, r_gcols], [r_cmp], partial(A.activation,
                                    out=KcT[g][:, ct * 128:(ct + 1) * 128], in_=pT[0:64, 0:128], func=AF.Copy, scale=gcols[0:64, 1:2]))
                if "cmp" in dbg:
                    kb.dma(dout("KcT0", [64, 256], BF16), KcT[0][:], [r_cmp], [])
                    kb.dma(dout("KcT1", [64, 256], BF16), KcT[1][:], [r_cmp], [])
                    kb.dma(dout("Vc", [128, 2, 2, 65], BF16), Vc[:], [r_cmp], [])
                kb.barrier()

        def phase_ssm(L):
            kb.phase = 3
            TS = 128
            with ExitStack() as LS:
                WB = T(LS, "WB", [128, 16, 2, 128], BF16); r_WB = Reg()
                Toep = T(LS, "Toep", [128, 32, 128], BF16); r_Toep = Reg()
                Qt = T(LS, "Qt", [128, 2, 2, 16, 128], BF16); r_Qt = Reg()
                cosT = T(LS, "cosT", [128, 16, TS]); sinT = T(LS, "sinT", [128, 16, TS]); r_tab = Reg()
                rcol = T(LS, "rcol", [128, 16]); cTs = T(LS, "cTs", [128, 2, 16]); r_par = Reg()
                bgl = T(LS, "bgl", [128, 4]); r_dsk = Reg()
                wglu = T(LS, "wglu", [128, 4, 512], BF16); r_wglu = Reg()
                with ExitStack() as LP:
                    lamre = T(LP, "lamre", [128, 16]); lamim = T(LP, "lamim", [128, 16]); lsb = T(LP, "lsb", [128, 32])
                    dlt = T(LP, "dlt", [128, 16]); th = T(LP, "th", [128, 16]); r_p = Reg()
                    cth = T(LP, "cth", [128, 16]); sth = T(LP, "sth", [128, 16])
                    er = T(LP, "er", [128, 16]); ei = T(LP, "ei", [128, 16]); cr = T(LP, "cr", [128, 16]); ci = T(LP, "ci", [128, 16])
                    tA = T(LP, "tA", [128, 16]); tB = T(LP, "tB", [128, 16]); tD = T(LP, "tD", [128, 16])
                    ki = T(LP, "ki", [128, 384], I32); kf = T(LP, "kf", [128, 384]); red = T(LP, "red", [128, 384]); ang = T(LP, "ang", [128, TS])
                    kab = T(LP, "kab", [128, 24]); kall = T(LP, "kall", [128, 24])
                    bre = T(LP, "bre", [128, 16, 16]); bim = T(LP, "bim", [128, 16, 16])
                    Bb = T(LP, "Bb", [128, 16, 2, 16]); tb1 = T(LP, "tb1", [128, 16, 16]); tb2 = T(LP, "tb2", [128, 16, 16])
                    cnat = T(LP, "cnat", [128, 2, 4, 64]); CTt = T(LP, "CTt", [64, 2, 4, 128]); CQ = T(LP, "CQ", [128, 16, 2, 16])
                    wgs = T(LP, "wgs", [128, 2, 512]); r_wgs = Reg()
                    angk = T(LP, "angk", [128, 16, 24]); magk = T(LP, "magk", [128, 16, 24])
                    ckk = T(LP, "ckk", [128, 16, 24]); skk = T(LP, "skk", [128, 16, 24])
                    PWr = T(LP, "PWr", [128, 16, 24]); PWi = T(LP, "PWi", [128, 16, 24])
                    tmA = T(LP, "tmA", [128, 16, 8, 16]); tmB = T(LP, "tmB", [128, 16, 8, 16])
                    PwB = T(LP, "PwB", [128, 2, 16, 128], BF16); PpB = T(LP, "PpB", [128, 2, 16, 128], BF16); QT2 = T(LP, "QT2", [128, 2, 16, 128], BF16)
                    tmask = T(LP, "tmask", [128, 128]); ToepF = T(LP, "ToepF", [128, 4, 128])
                    dsr = T(LP, "dsr", [32, 16]); dsr8 = T(LP, "dsr8", [32, 8, 16]); dskT = T(LP, "dskT", [128, 32])
                    r_s = Reg()

                    def S(e, fn):
                        kb.op(e, [r_s, r_p], [r_s, r_p], fn)

                    def sincos(cos_out, sin_out, a_ap, n):
                        K_, F_, R_ = ki[:, 0:n], kf[:, 0:n], red[:, 0:n]
                        S("dve", partial(V.tensor_scalar, K_, a_ap, 1.0 / (2 * PI), None, op0=ALU.mult))
                        S("dve", partial(V.tensor_copy, F_, K_))
                        S("dve", partial(V.scalar_tensor_tensor, out=R_, in0=F_, scalar=-2 * PI, in1=a_ap, op0=ALU.mult, op1=ALU.add))
                        S("dve", partial(V.tensor_scalar, F_, R_, PI, None, op0=ALU.is_gt))
                        S("dve", partial(V.scalar_tensor_tensor, out=R_, in0=F_, scalar=-2 * PI, in1=R_, op0=ALU.mult, op1=ALU.add))
                        S("dve", partial(V.tensor_scalar, F_, R_, -PI, None, op0=ALU.is_lt))
                        S("dve", partial(V.scalar_tensor_tensor, out=R_, in0=F_, scalar=2 * PI, in1=R_, op0=ALU.mult, op1=ALU.add))
                        S("act", partial(A.activation, out=sin_out, in_=R_, func=AF.Sin))
                        S("dve", partial(V.tensor_scalar, R_, R_, PI / 2, None, op0=ALU.add))
                        S("dve", partial(V.tensor_scalar, F_, R_, PI, None, op0=ALU.is_gt))
                        S("dve", partial(V.scalar_tensor_tensor, out=R_, in0=F_, scalar=-2 * PI, in1=R_, op0=ALU.mult, op1=ALU.add))
                        S("act", partial(A.activation, out=cos_out, in_=R_, func=AF.Sin))

                    load_cols(lamre[:], r_p, D["lam_re"].rearrange("g p -> (g p)"), 16)
                    load_cols(lamim[:], r_p, D["lam_im"].rearrange("g p -> (g p)"), 16)
                    load_cols(bgl[:], r_dsk, D["b_glu"], 4)
                    kb.dma(lsb[:], D["log_step"].rearrange("(o g) -> o g", o=1).partition_broadcast(128), [], [r_p])
                    kb.dma(kab[:], D["c_kab"], [], [r_p])
                    kb.dma(kall[:], D["c_kall"], [], [r_p])
                    kb.dma(tmask[:], D["c_tmask"], [], [r_p])
                    kb.dma(dsr[:], D["d_skip"], [], [r_p])
                    kb.dma(bre[:], D["b_re"].rearrange("g p h -> (g p) h").rearrange("(st q) h -> q st h", q=128), [], [r_p])
                    kb.dma(bim[:], D["b_im"].rearrange("g p h -> (g p) h").rearrange("(st q) h -> q st h", q=128), [], [r_p])
                    kb.dma(cnat[:, 0, :, :], D["c_re"].rearrange("g h p -> (g h) p").rearrange("(m q) p -> q m p", q=128), [], [r_p])
                    kb.dma(cnat[:, 1, :, :], D["c_im"].rearrange("g h p -> (g h) p").rearrange("(m q) p -> q m p", q=128), [], [r_p])
                    for hh in range(2):
                        kb.dma(wgs[:], D["w_glu"].rearrange("(m p) n -> p m n", p=128)[:, 2 * hh:2 * hh + 2, :], [], [r_wgs])
                        kb.op("pool", [r_wgs], [r_wglu], partial(G.tensor_copy, wglu[:, 2 * hh:2 * hh + 2, :], wgs[:]))
                    lsv = lsb[:].rearrange("p (st gg) -> p gg st", gg=2)
                    S("dve", partial(V.tensor_copy, dlt[0:64, :], lsv[0:64, 0, :]))
                    S("dve", partial(V.tensor_copy, dlt[64:128, :], lsv[64:128, 1, :]))
                    S("act", partial(A.activation, out=dlt[:], in_=dlt[:], func=AF.Exp))
                    S("dve", partial(V.tensor_tensor, th[:], lamim[:], dlt[:], op=ALU.mult))
                    S("dve", partial(V.tensor_tensor, tD[:], lamre[:], dlt[:], op=ALU.mult))
                    kb_ = kall[:].unsqueeze(1).to_broadcast([128, 16, 24])
                    S("dve", partial(V.tensor_tensor, angk[:], th[:].unsqueeze(2).to_broadcast([128, 16, 24]), kb_, op=ALU.mult))
                    S("dve", partial(V.tensor_tensor, magk[:], tD[:].unsqueeze(2).to_broadcast([128, 16, 24]), kb_, op=ALU.mult))
                    S("act", partial(A.activation, out=magk[:], in_=magk[:], func=AF.Exp))
                    sincos(ckk[:].rearrange("p a b -> p (a b)"), skk[:].rearrange("p a b -> p (a b)"), angk[:].rearrange("p a b -> p (a b)"), 384)
                    S("dve", partial(V.tensor_tensor, PWr[:], magk[:], ckk[:], op=ALU.mult))
                    S("dve", partial(V.tensor_tensor, PWi[:], magk[:], skk[:], op=ALU.mult))
                    kb.ckpt(1)
                    S("act", partial(A.activation, out=tA[:], in_=tD[:], func=AF.Exp))
                    sincos(cth[:], sth[:], th[:], 16)
                    S("dve", partial(V.tensor_tensor, er[:], tA[:], cth[:], op=ALU.mult))
                    S("dve", partial(V.tensor_scalar, er[:], er[:], -1.0, None, op0=ALU.add))
                    S("dve", partial(V.tensor_tensor, ei[:], tA[:], sth[:], op=ALU.mult))
                    S("dve", partial(V.tensor_tensor, tA[:], lamre[:], lamre[:], op=ALU.mult))
                    S("dve", partial(V.tensor_tensor, tB[:], lamim[:], lamim[:], op=ALU.mult))
                    S("dve", partial(V.tensor_tensor, tA[:], tA[:], tB[:], op=ALU.add))
                    S("dve", partial(V.reciprocal, tA[:], tA[:]))
                    S("dve", partial(V.tensor_tensor, cr[:], er[:], lamre[:], op=ALU.mult))
                    S("dve", partial(V.tensor_tensor, tB[:], ei[:], lamim[:], op=ALU.mult))
                    S("dve", partial(V.tensor_tensor, cr[:], cr[:], tB[:], op=ALU.add))
                    S("dve", partial(V.tensor_tensor, cr[:], cr[:], tA[:], op=ALU.mult))
                    S("dve", partial(V.tensor_tensor, ci[:], ei[:], lamre[:], op=ALU.mult))
                    S("dve", partial(V.tensor_tensor, tB[:], er[:], lamim[:], op=ALU.mult))
                    S("dve", partial(V.tensor_tensor, ci[:], ci[:], tB[:], op=ALU.subtract))
                    S("dve", partial(V.tensor_tensor, ci[:], ci[:], tA[:], op=ALU.mult))
                    crb = cr[:].unsqueeze(2).to_broadcast([128, 16, 16])
                    cib = ci[:].unsqueeze(2).to_broadcast([128, 16, 16])
                    S("dve", partial(V.tensor_tensor, tb1[:], bre[:], crb, op=ALU.mult))
                    S("dve", partial(V.tensor_tensor, tb2[:], bim[:], cib, op=ALU.mult))
                    S("dve", partial(V.tensor_tensor, Bb[:, :, 0, :], tb1[:], tb2[:], op=ALU.subtract))
                    S("dve", partial(V.tensor_tensor, tb1[:], bim[:], crb, op=ALU.mult))
                    S("dve", partial(V.tensor_tensor, tb2[:], bre[:], cib, op=ALU.mult))
                    S("dve", partial(V.tensor_tensor, Bb[:, :, 1, :], tb1[:], tb2[:], op=ALU.add))
                    kb.ckpt(2)
                    for ri in range(2):
                        for m in range(4):
                            kb.op("pe", [r_s, r_p, r_idf], [r_pb[5]], partial(PE.transpose, pb[5][0:64, m * 128:(m + 1) * 128], cnat[:, ri, m, :], idf[:]))
                        kb.op("dve", [r_pb[5]], [r_s], partial(V.tensor_copy, CTt[:, ri, :, :], pb[5][0:64, :].rearrange("p (m n) -> p m n", m=4)))
                    for ri in range(2):
                        cv = CTt[0:64, ri, :, :].rearrange("p m (gl h) -> p (m gl) h", h=16)
                        for gg in range(2):
                            S("act", partial(A.copy, CQ[64 * gg:64 * gg + 64, :, ri, :], cv[:, gg::2, :]))

                    def cmul(k0, bsrc, out_re, out_im, neg_im=False, asrc=None, extra_w=()):
                        def SO(e, fn):
                            kb.op(e, [r_s, r_p], [r_s, r_p] + list(extra_w), fn)
                        a_r, a_i = asrc if asrc is not None else (PWr, PWi)
                        ar = a_r[:, :, k0:k0 + 8].unsqueeze(3).to_broadcast([128, 16, 8, 16])
                        ai = a_i[:, :, k0:k0 + 8].unsqueeze(3).to_broadcast([128, 16, 8, 16])
                        br_ = bsrc[:, :, 0, :].unsqueeze(2).to_broadcast([128, 16, 8, 16])
                        bi_ = bsrc[:, :, 1, :].unsqueeze(2).to_broadcast([128, 16, 8, 16])
                        S("dve", partial(V.tensor_tensor, tmA[:], ar, br_, op=ALU.mult))
                        S("pool", partial(G.tensor_tensor, tmB[:], ai, bi_, op=ALU.mult))
                        SO("dve", partial(V.tensor_tensor, out_re, tmA[:], tmB[:], op=ALU.subtract))
                        S("dve", partial(V.tensor_tensor, tmA[:], ar, bi_, op=ALU.mult))
                        S("pool", partial(G.tensor_tensor, tmB[:], ai, br_, op=ALU.mult))
                        if neg_im:
                            SO("dve", partial(V.scalar_tensor_tensor, out=out_im, in0=tmA[:], scalar=-1.0, in1=tmB[:], op0=ALU.mult, op1=ALU.subtract))
                        else:
                            SO("dve", partial(V.tensor_tensor, out_im, tmA[:], tmB[:], op=ALU.add))

                    kb.ckpt(3)
                    v4 = lambda ap: ap.rearrange("p st (s h) -> p st s h", h=16)
                    r_PwB = Reg()
                    cmul(0, Bb, v4(PwB[:, 0, :, :]), v4(PwB[:, 1, :, :]), extra_w=[r_PwB])
                    for ri in range(2):
                        for q4 in range(4):
                            for k in range(4):
                                kb.op("pe", [r_PwB, r_idb], [r_pT], partial(PE.transpose, pT[:, k * 128:(k + 1) * 128], PwB[:, ri, q4 * 4 + k, :], idb[:]))
                            kb.op("act", [r_pT], [r_WB], partial(A.copy, WB[:, q4 * 4:(q4 + 1) * 4, ri, :], pT[:, 0:512].rearrange("p (k n) -> p k n", k=4)))
                    cmul(8, Bb, v4(PpB[:, 0, :, :]), v4(PpB[:, 1, :, :]))
                    S("dve", partial(V.tensor_scalar, tB[:], th[:], 8.0, None, op0=ALU.mult))
                    S("dve", partial(V.tensor_tensor, angk[:], tB[:].unsqueeze(2).to_broadcast([128, 16, 24]), kab[:].unsqueeze(1).to_broadcast([128, 16, 24]), op=ALU.mult))
                    sincos(ckk[:].rearrange("p a b -> p (a b)"), skk[:].rearrange("p a b -> p (a b)"), angk[:].rearrange("p a b -> p (a b)"), 384)
                    S("dve", partial(V.tensor_copy, Bb[:, :, 0, :], ckk[:, :, 8:24]))
                    S("dve", partial(V.tensor_copy, Bb[:, :, 1, :], skk[:, :, 8:24]))
                    cmul(0, Bb, cosT[:].rearrange("p st (a b) -> p st a b", b=16), sinT[:].rearrange("p st (a b) -> p st a b", b=16), asrc=(ckk, skk))
                    S("dve", partial(V.tensor_scalar, tA[:], tB[:], float(TS), None, op0=ALU.mult))
                    sincos(cTs[:, 0, :], cTs[:, 1, :], tA[:], 16)
                    kb.op("act", [r_s, r_p], [r_par, r_tab, r_s], partial(A.activation, out=rcol[:], in_=tD[:], func=AF.Exp, scale=8.0))
                    cmul(16, CQ, v4(QT2[:, 0, :, :]), v4(QT2[:, 1, :, :]), neg_im=True)
                    kb.op("pool", [r_s, r_p], [r_Qt, r_s], partial(G.memset, Qt[:], 0.0))
                    for gg in range(2):
                        sl = slice(64 * gg, 64 * gg + 64)
                        kb.op("dve", [r_s, r_p, r_Qt], [r_Qt, r_s], partial(V.tensor_copy, Qt[sl, gg, 0, :, :], QT2[sl, 0, :, :]))
                        kb.op("act", [r_s, r_p, r_Qt], [r_Qt, r_s], partial(A.activation, out=Qt[sl, gg, 1, :, :], in_=QT2[sl, 1, :, :], func=AF.Copy, scale=-1.0))
                    PZ = [tmA[:].rearrange("p a b c -> p (a b c)").bitcast(BF16).rearrange("p (r s n) -> p r s n", r=2, s=16),
                          tmB[:].rearrange("p a b c -> p (a b c)").bitcast(BF16).rearrange("p (r s n) -> p r s n", r=2, s=16)]
                    S("pool", partial(G.memset, tmA[:], 0.0))
                    S("pool", partial(G.memset, tmB[:], 0.0))
                    for gg in range(2):
                        sl = slice(64 * gg, 64 * gg + 64)
                        S("dve", partial(V.tensor_copy, PZ[gg][sl, :, :, :], PpB[sl, :, :, :]))
                    kb.ckpt(4)
                    kb.ckpt(5)
                    S("dve", partial(V.tensor_copy, dsr8[:], dsr[:].unsqueeze(1).to_broadcast([32, 8, 16])))
                    kb.op("pe", [r_s, r_p, r_idf], [r_pb[6]], partial(PE.transpose, pb[6][:, 0:32], dsr8[:].rearrange("p s h -> p (s h)"), idf[0:32, 0:32]))
                    kb.op("dve", [r_pb[6]], [r_s], partial(V.tensor_copy, dskT[:], pb[6][:, 0:32]))
                    kb.ckpt(6)
                    for g4 in range(8):
                        bk, rbk = pb[g4 % 2], r_pb[g4 % 2]
                        for k in range(4):
                            g = 4 * g4 + k
                            st, gg = g // 2, g % 2
                            kb.op("pe", [r_s, r_Qt], [rbk], partial(PE.matmul, bk[:, k * 128:(k + 1) * 128], lhsT=PZ[gg][:, 0, st, :], rhs=QT2[:, 0, st, :], start=True, stop=False))
                            kb.op("pe", [r_s, r_Qt], [rbk], partial(PE.matmul, bk[:, k * 128:(k + 1) * 128], lhsT=PZ[gg][:, 1, st, :], rhs=QT2[:, 1, st, :], start=False, stop=True))
                        kb.ckpt(6.5)
                        kb.op("dve", [r_s, r_p, rbk], [r_s, r_p], partial(V.tensor_tensor, ToepF[:], bk[:].rearrange("p (k n) -> p k n", k=4), tmask[:].unsqueeze(1).to_broadcast([128, 4, 128]), op=ALU.mult))
                        kb.ckpt(6.8)
                        for k in range(4):
                            g = 4 * g4 + k
                            kb.op("dve", [r_s, r_idf], [r_Toep, r_s], partial(V.scalar_tensor_tensor, out=Toep[:, g, :], in0=idf[:], scalar=dskT[:, g:g + 1], in1=ToepF[:, k, :], op0=ALU.mult, op1=ALU.add))
                    kb.ckpt(7)
                    kb.barrier()
                kb.phase = 4
                Ug = T(LS, "Ug", [128, 32, 256], BF16); r_Ug = [Reg() for _ in range(32)]
                Uc = T(LS, "Uc", [128, 4, 512], BF16); r_Uc = Reg()
                Uc2 = T(LS, "Uc2", [128, 32, 128], BF16); r_Uc2 = Reg()
                Zs = [T(LS, "Zs%d" % i, [128, 2, 256]) for i in range(2)]; r_Zs = [Reg(), Reg()]
                pc = T(LS, "pc", [128, 2, 256]); psn = T(LS, "psn", [128, 2, 256]); r_pc = Reg(); r_psn = Reg()
                Zh0_ = T(LS, "Zh0", [128, 2, 256])
                vb = [T(LS, "vb%d" % i, [128, 2, 256]) for i in range(2)]; r_vb = [Reg(), Reg()]
                carry = T(LS, "carry", [128, 2, 16]); r_carry = [Reg() for _ in range(16)]
                ctmp = T(LS, "ctmp", [128, 2, 16]); r_ctmp = [Reg() for _ in range(16)]
                lastq = T(LS, "lastq", [128, 16, 3, 2], BF16); r_lastq = [Reg() for _ in range(16)]
                qcs = [T(LS, "qcs%d" % i, [128, 3, 2, 258], BF16) for i in range(2)]; r_qcs = [Reg(), Reg()]
                gt1 = T(LS, "gt1", [128, 256]); r_gt = Reg()
                ygel = [T(LS, "ygel0", [128, 8, 256], BF16)] * 2; r_ygel = [[Reg() for _ in range(8)]] * 2
                selt = [T(LS, "selt0", [128, 1152], BF16)] * 2; r_selt = [Reg()] * 2
                yT = T(LS, "yT", [128, 4, 2048], BF16); r_yT = [Reg() for _ in range(4)]
                sg = gt1; r_sg = r_gt
                yo = T(LS, "yo", [128, 256]); r_yo = Reg()
                yst = [T(LS, "yst0", [128, 4, 256], BF16)] * 2; r_yst = [Reg()] * 2
                sqo = T(LS, "sqo", [128, 4, 256], BF16); r_sqo = Reg()
                onesb = T(LS, "onesb", [128, 2], BF16); r_onesb = Reg()
                kb.op("dve", [], [r_onesb], partial(V.memset, onesb[:], 1.0))
                r_pTh = [Reg(), Reg()]
                kb.op("dve", [], r_carry, partial(V.memset, carry[:], 0.0))
                yT3f = yT[:, 3, :].bitcast(F32)
                Zh = [Zh0_, yT3f[:, 0:512].rearrange("p (a n) -> p a n", a=2)]; r_Zh = [Reg(), Reg()]
                pcs = [pc, yT3f[:, 512:1024].rearrange("p (a n) -> p a n", a=2)]; r_pcs = [r_pc, Reg()]
                kb.op("pool", [], [r_selt[0]], partial(G.memset, selt[0][:], 0.0))
                uview = uTM.rearrange("(c j) ch -> c j ch", j=8)
                nev = 0
                for pas in range(2 if ssm_stop >= 1 else 0):
                    if pas == 1 and ssm_stop < 3:
                        break
                    for cbl in range(2):
                        cb = 2 * pas + cbl
                        kb.ckpt(8.0)
                        for jh in range(2):
                            kb.dma(Uc[:], uview[cb * 128:(cb + 1) * 128, jh * 4:jh * 4 + 4, :], [r_uTM[i] for i in range(cb * 8, cb * 8 + 8)], [r_Uc])
                            kb.op("pool", [r_Uc], [r_Uc2], partial(G.tensor_copy, Uc2[:, 0:16, :].rearrange("p g (j h) -> p j g h", j=8)[:, jh * 4:jh * 4 + 4, :, :], Uc[:, :, 0:256].rearrange("p j (g h) -> p j g h", h=16)))
                            kb.op("pool", [r_Uc], [r_Uc2], partial(G.tensor_copy, Uc2[:, 16:32, :].rearrange("p g (j h) -> p j g h", j=8)[:, jh * 4:jh * 4 + 4, :, :], Uc[:, :, 256:512].rearrange("p j (g h) -> p j g h", h=16)))
                        kb.ckpt(8.1)
                        for g4 in range(8):
                            hf = g4 % 2
                            for k in range(4):
                                g = 4 * g4 + k
                                kb.op("pe", [r_Uc2, r_idb], [r_pb[4 + hf]], partial(PE.matmul, pb[4 + hf][:, k * 128:(k + 1) * 128], lhsT=Uc2[:, g, :], rhs=idb[:], start=True, stop=True))
                            src = pb[4 + hf][:].rearrange("p (k n) -> p k n", k=4)
                            dst = Ug[:, 4 * g4:4 * g4 + 4, cbl * 128:(cbl + 1) * 128]
                            wr = [r_Ug[4 * g4 + k] for k in range(4)]
                            kb.op("act", [r_pb[4 + hf]], wr, partial(A.copy, dst, src))
                            nev += 1
                    for st in range(16 if ssm_stop >= 2 else 0):
                        b = st % 2
                        pc, r_pc = pcs[b], r_pcs[b]
                        bk, rbk = pb[b], r_pb[b]
                        for ri in range(2):
                            for gg in range(2):
                                kb.op("pe", [r_WB, r_Ug[2 * st + gg]], [rbk], partial(PE.matmul, bk[64 * gg:64 * gg + 64, ri * 256:(ri + 1) * 256], lhsT=WB[:, st, ri, 64 * gg:64 * gg + 64], rhs=Ug[:, 2 * st + gg, :], start=True, stop=True))
                        kb.op("act", [rbk], [r_Zs[b]], partial(A.copy, Zs[b][:].rearrange("p a n -> p (a n)"), bk[:]))
                        z4 = lambda ap: ap.rearrange("p a (s n) -> p a s n", s=2)
                        cb_ = cosT[:, st, :].unsqueeze(1).unsqueeze(1).to_broadcast([128, 2, 2, TS])
                        sb_ = sinT[:, st, :].unsqueeze(1).unsqueeze(1).to_broadcast([128, 2, 2, TS])
                        kb.op("pool", [r_Zs[b], r_tab], [r_pc], partial(G.tensor_tensor, z4(pc[:]), z4(Zs[b][:]), cb_, op=ALU.mult))
                        kb.op("dve", [r_Zs[b], r_tab], [r_psn], partial(V.tensor_tensor, z4(psn[:]), z4(Zs[b][:]), sb_, op=ALU.mult))
                        kb.op("pool", [r_pc, r_psn], [r_Zh[b]], partial(G.tensor_tensor, Zh[b][:, 0, :], pc[:, 0, :], psn[:, 1, :], op=ALU.add))
                        kb.op("pool", [r_pc, r_psn], [r_Zh[b]], partial(G.tensor_tensor, Zh[b][:, 1, :], pc[:, 1, :], psn[:, 0, :], op=ALU.subtract))
                        for seg in range(2):
                            cs = slice(seg * TS, (seg + 1) * TS)
                            for ri in range(2):
                                kb.op("dve", [r_Zh[b], r_par, r_carry[st]], [r_vb[b]], partial(V.tensor_tensor_scan,
                                    vb[b][:, ri, cs], rcol[:, st:st + 1].to_broadcast([128, TS]), Zh[b][:, ri, cs], carry[:, ri, st:st + 1], op0=ALU.mult, op1=ALU.add))
                            lc = seg * TS + TS - 1
                            vlr = vb[b][:, 0, lc:lc + 1]
                            vli = vb[b][:, 1, lc:lc + 1]
                            cc_, cs_ = cTs[:, 0, st:st + 1], cTs[:, 1, st:st + 1]
                            kb.op("dve", [r_vb[b], r_par], [r_ctmp[st]], partial(V.tensor_scalar, ctmp[:, 0, st:st + 1], vli, cs_, None, op0=ALU.mult))
                            kb.op("dve", [r_vb[b], r_par], [r_ctmp[st]], partial(V.tensor_scalar, ctmp[:, 1, st:st + 1], vlr, cs_, None, op0=ALU.mult))
                            kb.op("dve", [r_vb[b], r_par, r_ctmp[st]], [r_carry[st]], partial(V.scalar_tensor_tensor, out=carry[:, 0, st:st + 1], in0=vlr, scalar=cc_, in1=ctmp[:, 0, st:st + 1], op0=ALU.mult, op1=ALU.subtract))
                            kb.op("dve", [r_vb[b], r_par, r_ctmp[st]], [r_carry[st]], partial(V.scalar_tensor_tensor, out=carry[:, 1, st:st + 1], in0=vli, scalar=cc_, in1=ctmp[:, 1, st:st + 1], op0=ALU.mult, op1=ALU.add))
                        if pas == 0:
                            kb.op("dve", [r_vb[b], r_tab], [r_lastq[st]], partial(V.tensor_scalar, lastq[:, st, 0, :], vb[b][:, :, 2 * TS - 1], cosT[:, st, TS - 1:TS], None, op0=ALU.mult))
                            kb.op("dve", [r_vb[b], r_tab], [r_lastq[st]], partial(V.tensor_scalar, lastq[:, st, 1, :], vb[b][:, :, 2 * TS - 1], sinT[:, st, TS - 1:TS], -1.0, op0=ALU.mult, op1=ALU.mult))
                            kb.op("dve", [r_vb[b], r_tab], [r_lastq[st]], partial(V.tensor_scalar, lastq[:, st, 2, :], vb[b][:, :, 2 * TS - 1], cosT[:, st, TS - 1:TS], -1.0, op0=ALU.mult, op1=ALU.mult))
                            continue
                        q, rq = qcs[b], r_qcs[b]
                        kb.op("act", [r_lastq[st]], [rq], partial(A.copy, q[:, :, :, 0], lastq[:, st, :, :]))
                        kb.op("dve", [r_vb[b], r_tab], [rq], partial(V.tensor_tensor, z4(q[:, 0, :, 1:257]), z4(vb[b][:]), cb_, op=ALU.mult))
                        kb.op("act", [r_vb[b], r_pc], [r_pc], partial(A.activation, out=pc[:], in_=vb[b][:], func=AF.Copy, scale=-1.0))
                        kb.op("dve", [r_pc, r_tab], [rq], partial(V.tensor_tensor, z4(q[:, 1, :, 1:257]), z4(pc[:]), sb_, op=ALU.mult))
                        kb.op("dve", [r_pc, r_tab], [rq], partial(V.tensor_tensor, q[:, 2, 1, 1:257].rearrange("p (s n) -> p s n", s=2), pc[:, 1, :].rearrange("p (s n) -> p s n", s=2),
                                                                   cosT[:, st, :].unsqueeze(1).to_broadcast([128, 2, TS]), op=ALU.mult))
                        m = st // 4
                        yg_, ryg = ygel[m % 2], r_ygel[m % 2]
                        for gg in range(2):
                            g = 2 * st + gg
                            gl = g % 8
                            yb_, ryb = pb[2 + gg], r_pb[2 + gg]
                            kb.op("pe", [r_Toep, r_Ug[g]], [ryb], partial(PE.matmul, yb_[:, 0:256], lhsT=Toep[:, g, :], rhs=Ug[:, g, :], start=True, stop=False))
                            terms = ((0, q[:, 0, 0, 0:256]), (0, q[:, 1, 1, 0:256]), (1, q[:, 1, 0, 0:256]), (1, q[:, 2, 1, 0:256]))
                            for ti, (kq, rhs_) in enumerate(terms):
                                kb.op("pe", [r_Qt, rq], [ryb], partial(PE.matmul, yb_[:, 0:256], lhsT=Qt[:, gg, kq, st, :], rhs=rhs_, start=False, stop=(ti == 3)))
                            kb.op("act", [ryb], [r_gt], partial(A.activation, out=gt1[:], in_=yb_[:, 0:256], func=AF.Square, scale=math.sqrt(0.044715)))
                            kb.op("dve", [r_gt, ryb], [r_gt], partial(V.scalar_tensor_tensor, out=gt1[:], in0=gt1[:], scalar=1.0, in1=yb_[:, 0:256], op0=ALU.add, op1=ALU.mult))
                            kb.op("act", [r_gt], [r_gt], partial(A.activation, out=gt1[:], in_=gt1[:], func=AF.Sigmoid, scale=GK))
                            kb.op("dve", [r_gt, ryb], [ryg[gl]], partial(V.tensor_tensor, yg_[:, gl, :], yb_[:, 0:256], gt1[:], op=ALU.mult))
                        if st % 4 == 3:
                            for t in range(8):
                                se, rse = selt[t % 2], r_selt[t % 2]
                                kb.op("pool", [r_idb], [rse], partial(G.tensor_copy, se[:].rearrange("p (g x) -> p g x", x=144)[:, :, 0:16], idb[:, 16 * t:16 * t + 16].unsqueeze(1).to_broadcast([128, 8, 16])))
                                pk, rpk = pb[4 + t % 2], r_pb[4 + t % 2]
                                for gl in range(8):
                                    kb.op("pe", [rse, ryg[gl]], [rpk], partial(PE.matmul, pk[:, 0:256], lhsT=se[:, gl * 128:(gl + 1) * 128], rhs=yg_[:, gl, :], start=(gl == 0), stop=(gl == 7)))
                                dst = yT[:, m, :].rearrange("p (c t) -> p c t", t=8)[:, :, t]
                                if t % 2 == 0:
                                    kb.op("act", [rpk], [r_yT[m]], partial(A.copy, dst, pk[:, 0:256]))
                                else:
                                    kb.op("dve", [rpk], [r_yT[m]], partial(V.tensor_copy, dst, pk[:, 0:256]))
                Ucf = Uc[:].rearrange("p a b -> p (a b)").bitcast(F32)
                sg2 = [sg, Ucf[:, 0:256]]; r_sg2 = [r_sg, Reg()]
                yo2 = [yo, Ucf[:, 256:512]]; r_yo2 = [r_yo, Reg()]
                yst2 = [yst[0], Uc2[:, 0:8, :].rearrange("p (m a) n -> p m (a n)", m=4)]; r_yst2 = [r_yst[0], Reg()]
                for oc in range(8 if ssm_stop >= 4 else 0):
                    ts_ = slice(oc * 256, (oc + 1) * 256)
                    ysb, rys = yst2[oc % 2], r_yst2[oc % 2]
                    for mo in range(4):
                        gb, rgb = pb[mo % 2], r_pb[mo % 2]
                        sg, r_sg, yo, r_yo = sg2[mo % 2], r_sg2[mo % 2], yo2[mo % 2], r_yo2[mo % 2]
                        for mi in range(4):
                            kb.op("pe", [r_wglu, r_yT[mi]], [rgb], partial(PE.matmul, gb[:, 0:256], lhsT=wglu[:, mi, mo * 128:(mo + 1) * 128], rhs=yT[:, mi, ts_], start=(mi == 0), stop=(mi == 3)))
                        kb.op("act", [rgb, r_dsk], [r_sg], partial(A.activation, out=sg[:, 0:256], in_=gb[:, 0:256], func=AF.Sigmoid, bias=bgl[:, mo:mo + 1], scale=1.0))
                        kb.op("dve", [r_sg, r_yT[mo]], [r_yo], partial(V.tensor_tensor, yo[:, 0:256], yT[:, mo, ts_], sg[:, 0:256], op=ALU.mult))
                        kb.op("act", [r_yo], [rys], partial(A.copy, ysb[:, mo, :], yo[:, 0:256]))
                        kb.op("pool", [r_yo], [r_sqo], partial(G.tensor_tensor, sqo[:, mo, :], yo[:, 0:256], yo[:, 0:256], op=ALU.mult))
                    kb.dma(ysT[:, :, oc * 256:(oc + 1) * 256], ysb[:, :, :], [rys], [r_ysT[oc]])
                    for half in range(2):
                        qi = oc * 2 + half
                        for mo in range(4):
                            kb.op("pe", [r_sqo, r_onesb], [r_pb[6]], partial(PE.matmul,
                                pb[6][:, (qi % 8):(qi % 8) + 1], lhsT=sqo[:, mo, half * 128:(half + 1) * 128], rhs=onesb[:, 0:1], start=(mo == 0), stop=(mo == 3)))
                        kb.op("dve", [r_pb[6]], [r_ss_ssm[qi]], partial(V.tensor_copy, ss_ssm[:, qi:qi + 1], pb[6][:, (qi % 8):(qi % 8) + 1]))
                if "ssm" in dbg:
                    kb.dma(dout("ysT", [128, 4, 2048], BF16), ysT, r_ysT, [])
                    kb.dma(dout("ss_ssm", [128, NOWN]), ss_ssm[:], r_ss_ssm, [])
                kb.barrier()

        def phase_attn(L):
            kb.phase = 5
            with ExitStack() as LA:
                Qa = [T(LA, "Qa%d" % i, [128, 512], BF16) for i in range(2)]
                r_Qq = [Reg(), Reg()]; r_Qs = [Reg(), Reg()]
                cb4 = [T(LA, "cb4%d" % i, [128, 4, 128], BF16) for i in range(2)]; r_cb4 = [Reg(), Reg()]
                Pc2 = [T(LA, "Pc%d" % i, [128, 2, 512], BF16) for i in range(2)]; r_Pc2 = [[Reg(), Reg()] for _ in range(2)]
                Pw2 = [T(LA, "Pw%d" % i, [128, 5, 512], BF16) for i in range(2)]; r_Pw2 = [[Reg() for _ in range(5)] for _ in range(2)]
                Ps2 = [T(LA, "Ps%d" % i, [128, 32, 512], BF16) for i in range(2)]; r_Ps2 = [[Reg() for _ in range(32)] for _ in range(2)]
                impt = T(LA, "impt", [128, 64]); impw = T(LA, "impw", [128, 64]); fm = T(LA, "fm", [128, 64])
                m8 = T(LA, "m8", [128, 8]); rdi = T(LA, "rdi", [128, 4]); r_imp = Reg(); r_fm = Reg()
                selp = T(LA, "selp", [128, 128], BF16); r_selp = Reg()
                rden = T(LA, "rden", [128, 4]); wgt = T(LA, "wgt", [128, 4]); otmp = T(LA, "otmp", [128, 256]); r_fin = Reg()
                oatt2 = [T(LA, "oatt%d" % i, [128, 512]) for i in range(2)]; r_oatt2 = [Reg(), Reg()]
                oatt, r_oatt = oatt2[0], r_oatt2[0]
                oab = T(LA, "oab", [128, 512], BF16); osq = T(LA, "osq", [128, 512]); r_oab = Reg(); r_osq = Reg()
                ost = [T(LA, "ost%d" % i, [128, 4, 128], BF16) for i in range(2)]; r_ost = [Reg(), Reg()]
                kb.op("dve", [], [r_selp], partial(V.memset, selp[:], 0.0))
                kb.op("act", r_gates, r_gates, partial(A.activation, out=gates[:], in_=gates[:], func=AF.Sigmoid))
                ogc_a = T(LA, "ogc_a", [128, 8]); r_ogc_a = Reg()
                wos_a = [T(LA, "wos_a%d" % i, [128, 1024]) for i in range(2)]; r_wos_a = [Reg(), Reg()]
                wob_a = [T(LA, "wob_a%d" % i, [128, 1024], BF16) for i in range(2)]; r_wob_a = [Reg(), Reg()]
                load_cols(ogc_a[:, 0:4], r_ogc_a, D["out_g_ssm"], 4)
                load_cols(ogc_a[:, 4:8], r_ogc_a, D["out_g_att"], 4)
                for c in range(8):
                    kb.dma(wos_a[c % 2][:], D["w_out"][c * 128:(c + 1) * 128, :], [], [r_wos_a[c % 2]])
                    kb.op("dve", [r_wos_a[c % 2], r_ogc_a], [r_wob_a[c % 2]], partial(V.tensor_scalar, wob_a[c % 2][:], wos_a[c % 2][:], ogc_a[:, c:c + 1], None, op0=ALU.mult))
                    kb.dma(woutd[:, c, :], wob_a[c % 2][:], [r_wob_a[c % 2]], [r_woutd[c]])
                oT = [T(LA, "oT%d" % i, [128, 512]) for i in range(2)]; r_oT = [Reg(), Reg()]

                def finalize(acc, racc, br, first, g, qi):
                    av = acc[:, 0:260].rearrange("p (h e) -> p h e", h=4)
                    kb.op("dve", [racc], [r_fin], partial(V.tensor_scalar, rden[:], av[:, :, 64], 1e-30, None, op0=ALU.add))
                    kb.op("dve", [r_fin], [r_fin], partial(V.reciprocal, rden[:], rden[:]))
                    gv = gates[:, qi, g * 12:(g + 1) * 12].rearrange("p (h b) -> p h b", b=3)[:, :, br]
                    kb.op("dve", [r_fin, r_gates[qi]], [r_fin], partial(V.tensor_tensor, wgt[:], rden[:], gv, op=ALU.mult))
                    wb_ = wgt[:].unsqueeze(2).to_broadcast([128, 4, 64])
                    ov_ = oatt[:, g * 256:(g + 1) * 256].rearrange("p (h d) -> p h d", h=4)
                    if first:
                        kb.op("dve", [racc, r_fin], [r_oatt], partial(V.tensor_tensor, ov_, av[:, :, 0:64], wb_, op=ALU.mult))
                    else:
                        tv = otmp[:].rearrange("p (h d) -> p h d", h=4)
                        kb.op("dve", [racc, r_fin], [r_fin], partial(V.tensor_tensor, tv, av[:, :, 0:64], wb_, op=ALU.mult))
                        kb.op("dve", [r_fin, r_oatt], [r_oatt], partial(V.tensor_tensor, ov_, ov_, tv, op=ALU.add))

                sbank = [0]

                def next_s():
                    i = sbank[0] % 2
                    sbank[0] += 1
                    return pb[i], r_pb[i]

                for n in range(NT - NOWN, NT):
                    qi = n - (NT - NOWN)
                    oatt, r_oatt = oatt2[qi % 2], r_oatt2[qi % 2]
                    kb.prio = 1
                    kb.op("pool", [r_fm], [r_fm], partial(G.memset, fm[:], 0.0))
                    kb.op("pool", [r_fm], [r_fm], partial(G.memset, fm[0:64, 2 * n - 1:2 * n + 1], 1e9))
                    kb.op("pool", [r_fm], [r_fm], partial(G.memset, fm[64:128, 2 * n:2 * n + 2], 1e9))
                    kb.op("dve", [r_fm, r_f0], [r_fm], partial(V.tensor_tensor, fm[:], fm[:], f0[:], op=ALU.max))
                    for j in range(2):
                        tau = float(128 * n - 2048 * j)
                        kb.op("dve", [r_dmat], [r_cb4[j]], partial(V.tensor_scalar,
                            cb4[j][:], dmat[:].unsqueeze(1).to_broadcast([128, 4, 128]), tau, -BIG, op0=ALU.is_gt, op1=ALU.mult))
                    for g in range(2):
                        qa = Qa[g]
                        kb.prio = 1
                        Pc, r_Pc, Pw, r_Pw, Ps, r_Ps = Pc2[g], r_Pc2[g], Pw2[g], r_Pw2[g], Ps2[g], r_Ps2[g]
                        for h in range(4):
                            kb.op("pe", [r_Q[qi], r_idb], [r_pT], partial(PE.transpose,
                                pT[0:64, h * 128:(h + 1) * 128], Qraw[:, qi, (4 * g + h) * 64:(4 * g + h + 1) * 64], idb[:]))
                        kb.op("act", [r_pT, r_gcols], [r_Qq[g]], partial(A.activation, out=qa[0:64, :], in_=pT[0:64, 0:512], func=AF.Copy, scale=gcols[0:64, 0:1]))
                        for j in range(2):
                            sb_, rsb = pb[6], r_pb[6]
                            kb.op("pe", [r_cmp, r_Qq[g]], [rsb], partial(PE.matmul, sb_[:], lhsT=KcT[g][:, j * 128:(j + 1) * 128], rhs=qa[0:64, :], start=True, stop=False))
                            kb.op("pe", [r_idb, r_cb4[j]], [rsb], partial(PE.matmul, sb_[:], lhsT=idb[:], rhs=cb4[j][:].rearrange("p h q -> p (h q)"), start=False, stop=True))
                            kb.op("act", [rsb, r_kbcmp], [r_Pc[j]], partial(A.activation, out=Pc[:, j, :], in_=sb_[:], func=AF.Exp, bias=kbcmp[:, j:j + 1], scale=1.0))
                        for h in range(4):
                            for j in range(2):
                                kb.op("pe", [r_Pc[j], r_cmp], [r_pb[2]], partial(PE.matmul,
                                    pb[2][:, h * 65:(h + 1) * 65], lhsT=Pc[:, j, h * 128:(h + 1) * 128], rhs=Vc[:, j, g, :], start=(j == 0), stop=(j == 1)))
                        for h in range(4):
                            for j in range(2):
                                kb.op("pe", [r_Pc[j], r_ovb], [r_pb[3]], partial(PE.matmul,
                                    pb[3][:, h * 65:(h + 1) * 65], lhsT=Pc[:, j, h * 128:(h + 1) * 128], rhs=ovb[:, j, :], start=(j == 0), stop=(j == 1)))
                        iv = pb[3][:, 0:260].rearrange("p (h e) -> p h e", h=4)
                        kb.op("dve", [r_pb[3]], [r_imp], partial(V.tensor_scalar, rdi[:], iv[:, :, 64], 1e-30, None, op0=ALU.add))
                        kb.op("dve", [r_imp], [r_imp], partial(V.reciprocal, rdi[:], rdi[:]))
                        kb.op("dve", [r_pb[3], r_imp], [r_imp], partial(V.tensor_scalar, impt[:], iv[:, 0, 0:64], rdi[:, 0:1], None, op0=ALU.mult))
                        for h in range(1, 4):
                            kb.op("dve", [r_pb[3], r_imp], [r_imp], partial(V.scalar_tensor_tensor,
                                out=impt[:], in0=iv[:, h, 0:64], scalar=rdi[:, h:h + 1], in1=impt[:], op0=ALU.mult, op1=ALU.add))
                        kb.op("dve", [r_imp, r_fm], [r_imp], partial(V.tensor_tensor, impt[:], impt[:], fm[:], op=ALU.max))
                        kb.op("dve", [r_imp], [r_imp], partial(V.max, out=m8[:], in_=impt[:]))
                        kb.op("dve", [r_imp], [r_imp], partial(V.match_replace, out=impw[:], in_to_replace=m8[:], in_values=impt[:], imm_value=-1e30))
                        kb.op("dve", [r_imp], [r_imp], partial(V.max, out=m8[:], in_=impw[:]))
                        kb.op("dve", [r_imp], [r_selp], partial(V.tensor_scalar, selp[:, 64:128], impt[:], m8[:, 7:8], -1.0, op0=ALU.is_ge, op1=ALU.add))
                        kb.op("pe", [r_selp, r_idb], [r_pT], partial(PE.transpose, pT[:, 512:640], selp[:], idb[:]))
                        kb.op("act", [r_pT], [r_Qs[g]], partial(A.copy,
                            qa[64:128, :].rearrange("p (h q) -> p h q", h=4), pT[64:128, 512:640].unsqueeze(1).to_broadcast([64, 4, 128])))
                        finalize(pb[2], r_pb[2], 0, True, g, qi)
                        kb.prio = 0
                        for kt in range(n + 1):
                            sb_, rsb = next_s()
                            kb.op("pe", [r_K[kt], r_eb, r_Qq[g], r_Qs[g]], [rsb], partial(PE.matmul,
                                sb_[:], lhsT=KsT[g][:, kt * 128:(kt + 1) * 128], rhs=qa[:, :], start=True, stop=(kt != n)))
                            if kt == n:
                                kb.op("pe", [r_idb, r_tlb4], [rsb], partial(PE.matmul, sb_[:], lhsT=idb[:], rhs=tlb4[:].rearrange("p h q -> p (h q)"), start=False, stop=True))
                            kb.op("act", [rsb, r_kbtok], [r_Ps[kt]], partial(A.activation, out=Ps[:, kt, :], in_=sb_[:], func=AF.Exp, bias=kbtok[:, kt:kt + 1], scale=1.0))
                        for kt in range(n + 1):
                            kb.op("pe", [r_Ps[kt], r_K[kt]], [r_pb[4]], partial(PE.matmul, pb[4][0:65, :], lhsT=Vs[:, kt, g, :], rhs=Ps[:, kt, :], start=(kt == 0), stop=(kt == n)))
                        kb.op("dve", [r_pb[4]], [r_oT[0]], partial(V.tensor_copy, oT[0][0:65, :], pb[4][0:65, :]))
                        for h in range(4):
                            kb.op("pe", [r_oT[0], r_idf], [r_pb[4]], partial(PE.transpose, pb[4][:, h * 65:(h + 1) * 65], oT[0][0:65, h * 128:(h + 1) * 128], idf[0:65, 0:65]))
                        kts = list(range(n - 4, n + 1))
                        for wi, kt in enumerate(kts):
                            sb_, rsb = next_s()
                            plain = (kt != n and kt != n - 4)
                            kb.op("pe", [r_K[kt], r_Qq[g]], [rsb], partial(PE.matmul,
                                sb_[:], lhsT=KwT[g][:, kt * 128:(kt + 1) * 128], rhs=qa[0:64, :], start=True, stop=plain))
                            if not plain:
                                bt, rbt = (tlb4, r_tlb4) if kt == n else (sub4, r_sub4)
                                kb.op("pe", [r_idb, rbt], [rsb], partial(PE.matmul, sb_[:], lhsT=idb[:], rhs=bt[:].rearrange("p h q -> p (h q)"), start=False, stop=True))
                            kb.op("act", [rsb, r_kbtok], [r_Pw[wi]], partial(A.activation, out=Pw[:, wi, :], in_=sb_[:], func=AF.Exp, bias=kbtok[:, kt:kt + 1], scale=1.0))
                        for wi, kt in enumerate(kts):
                            kb.op("pe", [r_Pw[wi], r_K[kt]], [r_pb[5]], partial(PE.matmul, pb[5][0:65, :], lhsT=Vw[:, kt, g, :], rhs=Pw[:, wi, :], start=(wi == 0), stop=(wi == 4)))
                        kb.op("dve", [r_pb[5]], [r_oT[1]], partial(V.tensor_copy, oT[1][0:65, :], pb[5][0:65, :]))
                        for h in range(4):
                            kb.op("pe", [r_oT[1], r_idf], [r_pb[5]], partial(PE.transpose, pb[5][:, h * 65:(h + 1) * 65], oT[1][0:65, h * 128:(h + 1) * 128], idf[0:65, 0:65]))
                        finalize(pb[4], r_pb[4], 1, False, g, qi)
                        finalize(pb[5], r_pb[5], 2, False, g, qi)
                    kb.op("dve", [r_oatt], [r_osq], partial(V.tensor_tensor, osq[:], oatt[:], oatt[:], op=ALU.mult))
                    kb.op("dve", [r_osq], [r_ss_att[qi]], partial(V.tensor_reduce, out=ss_att[:, qi:qi + 1], in_=osq[:], axis=AX.X, op=ALU.add))
                    for m in range(4):
                        kb.op("pe", [r_oatt, r_idf], [r_pb[5]], partial(PE.transpose, pb[5][:, m * 128:(m + 1) * 128], oatt[:, m * 128:(m + 1) * 128], idf[:]))
                    osb, ros = ost[qi % 2], r_ost[qi % 2]
                    kb.op("dve", [r_pb[5]], [ros], partial(V.tensor_copy, osb[:], pb[5][:].rearrange("p (m n) -> p m n", m=4)))
                    kb.dma(oattT[:, :, qi * 128:(qi + 1) * 128], osb[:], [ros], [r_oattT[qi]])
                    if "att" in dbg and qi == NOWN - 1:
                        pass
                if "att" in dbg:
                    kb.dma(dout("oattT", [128, 4, 2048], BF16), oattT, r_oattT, [])
                    kb.dma(dout("ss_att", [128, NOWN]), ss_att[:], r_ss_att, [])
                kb.barrier()

        def phase_post(L):
            kb.phase = 6
            with ExitStack() as LO:
                wout = T(LO, "wout", [128, 8, 1024], BF16); r_wout = Reg()
                wr = T(LO, "wr", [128, 8, 36]); r_wr = Reg()
                wrs = T(LO, "wrs", [128, 8, 36]); r_wrs = Reg()
                g2c = T(LO, "g2c", [128, 8]); r_g2c = Reg()
                brt = T(LO, "brt", [128, 36]); r_brt = Reg()
                ysb = [T(LO, "ysb%d" % i, [128, 4, 128], BF16) for i in range(2)]; r_ysb = [Reg(), Reg()]
                oab2 = [T(LO, "oab2%d" % i, [128, 4, 128], BF16) for i in range(2)]; r_oab2 = [Reg(), Reg()]
                xt = [T(LO, "xt%d" % i, [128, 1024]) for i in range(2)]; r_xt = [Reg(), Reg()]
                x1 = [T(LO, "x1%d" % i, [128, 1024]) for i in range(2)]; r_x1 = [Reg(), Reg()]
                rs2 = T(LO, "rs2", [128, 2]); r_rs2 = Reg()
                junk2s = [T(LO, "junk2%d" % i, [128, 1024]) for i in range(2)]; r_junk2s = [Reg(), Reg()]
                ssn = T(LO, "ssn", [128, 1]); r_ssn = Reg()
                t32s = [T(LO, "t32%d" % i, [128, 1024]) for i in range(2)]; r_t32s = [Reg(), Reg()]
                tTfs = [T(LO, "tTf%d" % i, [128, 8, 128]) for i in range(2)]; r_tTfs = [Reg(), Reg()]
                tTb = [T(LO, "tTb%d" % i, [128, 8, 128], BF16) for i in range(2)]; r_tTb = [Reg(), Reg()]
                lg = T(LO, "lg", [128, 36]); r_lg = Reg()
                rt = T(LO, "rt", [128, 64]); r_rt = Reg()
                m8r = T(LO, "m8r", [128, 8])
                load_cols(g2c[:], r_g2c, D["norm2_g"], 8)
                for c2 in range(2):
                    kb.dma(wout[:, 4 * c2:4 * c2 + 4, :], woutd[:, 4 * c2:4 * c2 + 4, :], r_woutd[4 * c2:4 * c2 + 4], [r_wout])
                load_cols(MB["g2m"][:], MB["r_g2m"], D["norm2_g"], 8)
                if stage >= 6:
                    moe_load_expert(0)
                kb.dma(wrs[:, :, 0:4], D["w_grp"].rearrange("(c p) n -> p c n", p=128), [], [r_wrs])
                kb.dma(wrs[:, :, 4:36], D["w_exp"].rearrange("(c p) n -> p c n", p=128), [], [r_wrs])
                kb.op("dve", [r_wrs, r_g2c], [r_wr], partial(V.tensor_tensor, wr[:], wrs[:], g2c[:].unsqueeze(2).to_broadcast([128, 8, 36]), op=ALU.mult))
                kb.dma(brt[:, 0:4], D["b_grp"].rearrange("(o n) -> o n", o=1).partition_broadcast(128), [], [r_brt])
                kb.dma(brt[:, 4:36], D["b_exp"].rearrange("(o n) -> o n", o=1).partition_broadcast(128), [], [r_brt])
                for qi in range(NOWN if stage >= 5.15 else 0):
                    b = qi % 2
                    junk2, r_junk2, t32, r_t32, tTf, r_tTf = junk2s[b], r_junk2s[b], t32s[b], r_t32s[b], tTfs[b], r_tTfs[b]
                    kb.prio = 1
                    kb.dma(ysb[b][:], ysT[:, :, qi * 128:(qi + 1) * 128], [r_ysT[qi // 2]], [r_ysb[b]])
                    kb.dma(oab2[b][:], oattT[:, :, qi * 128:(qi + 1) * 128], [r_oattT[qi]], [r_oab2[b]])
                    kb.dma(xt[b][:], xown[qi * 128:(qi + 1) * 128, :], [], [r_xt[b]])
                    for nb in range(2):
                        for m in range(4):
                            kb.op("pe", [r_ysb[b], r_wout], [r_pb[nb]], partial(PE.matmul, pb[nb][:], lhsT=ysb[b][:, m, :], rhs=wout[:, m, nb * 512:(nb + 1) * 512], start=(m == 0), stop=(m == 3)))
                        for m in range(4):
                            kb.op("pe", [r_oab2[b], r_wout], [r_pb[2 + nb]], partial(PE.matmul, pb[2 + nb][:], lhsT=oab2[b][:, m, :], rhs=wout[:, 4 + m, nb * 512:(nb + 1) * 512], start=(m == 0), stop=(m == 3)))
                    kb.op("dve", [r_ss_ssm[qi]], [r_rs2], partial(V.tensor_copy, rs2[:, 0:1], ss_ssm[:, qi:qi + 1]))
                    kb.op("dve", [r_ss_att[qi]], [r_rs2], partial(V.tensor_copy, rs2[:, 1:2], ss_att[:, qi:qi + 1]))
                    rstd_lnexp(rs2[:], 512, [r_rs2])
                    for nb in range(2):
                        sl = slice(nb * 512, (nb + 1) * 512)
                        kb.op("dve", [r_pb[nb], r_rs2, r_xt[b]], [r_x1[b]], partial(V.scalar_tensor_tensor,
                            out=x1[b][:, sl], in0=pb[nb][:], scalar=rs2[:, 0:1], in1=xt[b][:, sl], op0=ALU.mult, op1=ALU.add))
                        kb.op("dve", [r_pb[2 + nb], r_rs2, r_x1[b]], [r_x1[b]], partial(V.scalar_tensor_tensor,
                            out=x1[b][:, sl], in0=pb[2 + nb][:], scalar=rs2[:, 1:2], in1=x1[b][:, sl], op0=ALU.mult, op1=ALU.add))
                    kb.dma(x1d[qi * 128:(qi + 1) * 128, :], x1[b][:], [r_x1[b]], [r_x1d[qi]])
                    if stage < 5.25:
                        continue
                    kb.op("act", [r_x1[b]], [r_junk2], partial(A.activation, out=junk2[:], in_=x1[b][:], func=AF.Square))
                    kb.op("dve", [r_junk2], [r_ssn], partial(V.tensor_reduce, out=ssn[:], in_=junk2[:], axis=AX.X, op=ALU.add))
                    rstd_lnexp(ssn[:], 1024, [r_ssn])
                    kb.op("dve", [r_x1[b], r_ssn], [r_t32], partial(V.tensor_scalar, t32[:], x1[b][:], ssn[:, 0:1], None, op0=ALU.mult))
                    if stage < 5.26:
                        continue
                    for c in range(8):
                        bk = 4 + c // 4
                        kb.op("pe", [r_t32, r_idf], [r_pb[bk]], partial(PE.transpose, pb[bk][:, (c % 4) * 128:(c % 4 + 1) * 128], t32[:, c * 128:(c + 1) * 128], idf[:]))
                    if stage < 5.27:
                        continue
                    for hh in range(2):
                        kb.op("act", [r_pb[4 + hh]], [r_tTf], partial(A.copy, tTf[:, hh * 4:(hh + 1) * 4, :], pb[4 + hh][:].rearrange("p (c n) -> p c n", c=4)))
                        kb.op("dve", [r_tTf], [MB["r_tTq"][qi]], partial(V.tensor_copy, MB["tT"][:, hh * 4:(hh + 1) * 4, qi * 128:(qi + 1) * 128], tTf[:, hh * 4:(hh + 1) * 4, :]))
                    if stage < 5.28:
                        continue
                    if stage < 5.35:
                        continue
                    for c in range(8):
                        kb.op("pe", [r_tTf, r_wr], [r_pb[6]], partial(PE.matmul, pb[6][:, 0:36], lhsT=tTf[:, c, :], rhs=wr[:, c, :], start=(c == 0), stop=(c == 7)))
                    kb.op("dve", [r_pb[6], r_brt], [r_lg], partial(V.tensor_tensor, lg[:], pb[6][:, 0:36], brt[:], op=ALU.add))
                    if stage < 5.45:
                        continue
                    kb.prio = 0
                    def R(e, fn, extra_r=(), extra_w=()):
                        kb.op(e, [r_rt, r_lg] + list(extra_r), [r_rt] + list(extra_w), fn)
                    gl = lg[:, 0:4]
                    el = lg[:, 4:36].rearrange("p (g j) -> p g j", g=4)
                    gmax, ngmax, goh, gex, gsum = rt[:, 0:1], rt[:, 1:2], rt[:, 2:6], rt[:, 6:10], rt[:, 10:11]
                    els, msk, ee, wsum, nv1 = rt[:, 16:24], rt[:, 24:32], rt[:, 32:40], rt[:, 11:12], rt[:, 12:13]
                    R("dve", partial(V.tensor_reduce, out=gmax, in_=gl, axis=AX.X, op=ALU.max))
                    R("dve", partial(V.tensor_scalar, ngmax, gmax, -1.0, None, op0=ALU.mult))
                    R("dve", partial(V.tensor_scalar, goh, gl, gmax, None, op0=ALU.is_ge))
                    R("act", partial(A.activation, out=gex, in_=gl, func=AF.Exp, bias=ngmax, scale=1.0))
                    R("dve", partial(V.tensor_reduce, out=gsum, in_=gex, axis=AX.X, op=ALU.add))
                    R("dve", partial(V.reciprocal, gsum, gsum))
                    R("dve", partial(V.tensor_scalar, els, el[:, 0, :], goh[:, 0:1], None, op0=ALU.mult))
                    for g in range(1, 4):
                        R("dve", partial(V.scalar_tensor_tensor, out=els, in0=el[:, g, :], scalar=goh[:, g:g + 1], in1=els, op0=ALU.mult, op1=ALU.add))
                    R("dve", partial(V.max, out=m8r[:], in_=els))
                    R("dve", partial(V.tensor_scalar, msk, els, m8r[:, 1:2], None, op0=ALU.is_ge))
                    R("dve", partial(V.tensor_scalar, nv1, m8r[:, 0:1], -1.0, None, op0=ALU.mult))
                    R("act", partial(A.activation, out=ee, in_=els, func=AF.Exp, bias=nv1, scale=1.0))
                    R("dve", partial(V.tensor_tensor, ee, ee, msk, op=ALU.mult))
                    R("dve", partial(V.tensor_reduce, out=wsum, in_=ee, axis=AX.X, op=ALU.add))
                    R("dve", partial(V.reciprocal, wsum, wsum))
                    R("dve", partial(V.tensor_tensor, wsum, wsum, gsum, op=ALU.mult))
                    R("dve", partial(V.tensor_scalar, ee, ee, wsum, None, op0=ALU.mult))
                    for g in range(4):
                        R("dve", partial(V.tensor_scalar, comb[:, qi, g * 8:(g + 1) * 8], ee, goh[:, g:g + 1], None, op0=ALU.mult), extra_w=[r_comb[qi]])
                if "post" in dbg:
                    dx1 = dout("x1", [2048, 1024])
                    for qi in range(NOWN):
                        kb.dma(dx1[qi * 128:(qi + 1) * 128, :], x1d[qi * 128:(qi + 1) * 128, :], [r_x1d[qi]], [])
                    kb.dma(dout("comb", [128, NOWN, 32]), comb[:], r_comb, [])
                kb.barrier()

        def moe_load_expert(e):
            s2 = e % 2
            g2b = MB["g2m"][:].unsqueeze(2).to_broadcast([128, 8, 256])
            kb.dma(MB["wgs_"][:], D["w_gate"][e].rearrange("(c p) f -> p c f", p=128), [], [MB["r_wgs"]])
            kb.dma(MB["wus_"][:], D["w_up"][e].rearrange("(c p) f -> p c f", p=128), [], [MB["r_wus"]])
            kb.dma(MB["wds_"][:], D["w_down"][e].rearrange("(c p) d -> p c d", p=128), [], [MB["r_wds"]])
            kb.op("pool", [MB["r_wgs"], MB["r_g2m"]], [MB["r_wb"][s2]], partial(G.tensor_tensor, MB["wgb"][s2][:], MB["wgs_"][:], g2b, op=ALU.mult))
            kb.op("pool", [MB["r_wus"], MB["r_g2m"]], [MB["r_wb"][s2]], partial(G.tensor_tensor, MB["wub"][s2][:], MB["wus_"][:], g2b, op=ALU.mult))
            kb.op("act", [MB["r_wds"]], [MB["r_wb"][s2]], partial(A.copy, MB["wdb"][s2][:], MB["wds_"][:]))

        def phase_moe(L):
            kb.phase = 7
            acc = T(L, "acc", [128, NOWN, 1024]); r_acc = [Reg() for _ in range(NOWN)]
            tT, r_tTq = MB["tT"], MB["r_tTq"]
            wgb, wub, wdb, r_wb = MB["wgb"], MB["wub"], MB["wdb"], MB["r_wb"]
            abf = T(L, "abf", [128, 2, 2048], BF16); r_abf = [Reg() for _ in range(4)]
            sil = [T(L, "sil%d" % i, [128, 512]) for i in range(2)]; r_sil = [Reg(), Reg()]
            for qi in range(NOWN):
                kb.dma(acc[:, qi, :], x1d[qi * 128:(qi + 1) * 128, :], [r_x1d[qi]], [r_acc[qi]])
            k = 0
            for e in range(32):
                s2 = e % 2
                if e > 0:
                    moe_load_expert(e)
                for nt in range(4):
                    for f in range(2):
                        bg, rbg = pb[k % 2], r_pb[k % 2]
                        bu, rbu = pb[2 + k % 2], r_pb[2 + k % 2]
                        sl_, rsl = sil[k % 2], r_sil[k % 2]
                        k += 1
                        for c in range(8):
                            kb.op("pe", [r_wb[s2]] + r_tTq[4 * nt:4 * nt + 4], [rbg], partial(PE.matmul, bg[:], lhsT=wgb[s2][:, c, f * 128:(f + 1) * 128], rhs=tT[:, c, nt * 512:(nt + 1) * 512], start=(c == 0), stop=(c == 7)))
                        for c in range(8):
                            kb.op("pe", [r_wb[s2]] + r_tTq[4 * nt:4 * nt + 4], [rbu], partial(PE.matmul, bu[:], lhsT=wub[s2][:, c, f * 128:(f + 1) * 128], rhs=tT[:, c, nt * 512:(nt + 1) * 512], start=(c == 0), stop=(c == 7)))
                        kb.op("act", [rbg], [rsl], partial(A.activation, out=sl_[:], in_=bg[:], func=AF.Silu))
                        kb.op("dve", [rsl, rbu], [r_abf[nt]], partial(V.tensor_tensor, abf[:, f, nt * 512:(nt + 1) * 512], sl_[:], bu[:], op=ALU.mult))
                for tt in range(NOWN):
                    for nb in range(2):
                        bd, rbd = pb[4 + (tt * 2 + nb) % 3], r_pb[4 + (tt * 2 + nb) % 3]
                        for f in range(2):
                            kb.op("pe", [r_abf[tt // 4], r_wb[s2]], [rbd], partial(PE.matmul, bd[:], lhsT=abf[:, f, tt * 128:(tt + 1) * 128], rhs=wdb[s2][:, f, nb * 512:(nb + 1) * 512], start=(f == 0), stop=(f == 1)))
                        kb.op("dve", [rbd, r_comb[tt], r_acc[tt]], [r_acc[tt]], partial(V.scalar_tensor_tensor,
                            out=acc[:, tt, nb * 512:(nb + 1) * 512], in0=bd[:], scalar=comb[:, tt, e:e + 1], in1=acc[:, tt, nb * 512:(nb + 1) * 512], op0=ALU.mult, op1=ALU.add))
            for qi in range(NOWN):
                kb.dma(out[qi * 128:(qi + 1) * 128, :], acc[:, qi, :], [r_acc[qi]], [])

        with ExitStack() as L1:
            ysT = ysTd; r_ysT = [Reg() for _ in range(8)]
            oattT = oattTd; r_oattT = [Reg() for _ in range(NOWN)]
            with ExitStack() as L2:
                KsT = [T(L2, "KsT%d" % g, [128, 4096], BF16) for g in range(2)]
                KwT = [T(L2, "KwT%d" % g, [64, 4096], BF16) for g in range(2)]
                Vs = T(L2, "Vs", [128, NT, 2, 65], BF16)
                Vw = T(L2, "Vw", [128, NT, 2, 65], BF16)
                r_K = [Reg() for _ in range(NT)]
                KcT = [T(L2, "KcT%d" % g, [64, 256], BF16) for g in range(2)]
                Vc = T(L2, "Vc", [128, 2, 2, 65], BF16)
                r_cmp = Reg()
                Qraw = T(L2, "Qraw", [128, NOWN, 512], BF16); r_Q = [Reg() for _ in range(NOWN)]
                gates = T(L2, "gates", [128, NOWN, 24]); r_gates = [Reg() for _ in range(NOWN)]
                r_eb = Reg()
                kb.op("pool", [], [r_K[i] for i in range(NT)], partial(G.memset, Vs[:], 1.0))
                kb.op("pool", [], [r_K[i] for i in range(NT)], partial(G.memset, Vw[:], 1.0))
                kb.op("pool", [], [r_cmp], partial(G.memset, Vc[:], 1.0))
                for g in range(2):
                    kb.op("pool", [], [r_cmp], partial(G.memset, KcT[g][:], 0.0))
                with ExitStack() as L3:
                    r_uTM = [Reg() for _ in range(NT)]
                    with ExitStack() as L4:
                        wtm = T(L4, "wtm", [128, 8, 1048], BF16)
                        wfu = T(L4, "wfu", [128, 8, 512], BF16)
                        wfc = T(L4, "wfc", [128, 8, 2, 128], BF16)
                        r_w4 = [Reg() for _ in range(4)]
                        g1c = T(L4, "g1c", [128, 8]); r_g1c = Reg()
                        kcvT = T(L4, "kcvT", [128, 2, 16, 256], BF16); r_kcv = [Reg() for _ in range(8)]
                        load_cols(g1c[:], r_g1c, D["norm1_g"], 8)
                        if "uT" in dbg:
                            kb.dma(dout("g1c", [128, 8]), g1c[:], [r_g1c], [])
                            kb.dma(dout("gcols", [128, 4]), gcols[:], [r_gcols], [])
                        with ExitStack() as L5:
                            wst = [T(L5, "wst%d" % i, [128, 1816]) for i in range(2)]
                            r_wst = [Reg(), Reg()]
                            for q4 in range(4):
                                ebs = wst[q4 % 2]; r_ebs = r_wst[q4 % 2]
                                kb.dma(ebs[0:64, 0:1024], D["c_eband"][:, q4 * 1024:(q4 + 1) * 1024], [], [r_ebs])
                                for g in range(2):
                                    kb.op("pool", [r_ebs], [r_eb], partial(G.tensor_copy, KsT[g][64:128, q4 * 1024:(q4 + 1) * 1024], ebs[0:64, 0:1024]))
                            for c in range(8):
                                ws = wst[c % 2]; rw = r_wst[c % 2]
                                kb.dma(ws[:], D["w_in"][c * 128:(c + 1) * 128, :], [], [rw])
                                sc = g1c[:, c:c + 1]
                                e1, e2 = ("dve", V), ("pool", G)
                                kb.op("dve", [rw, r_g1c], [r_w4[0]], partial(V.tensor_scalar, wfu[:, c, :], ws[:, 0:512], sc, None, op0=ALU.mult))
                                kb.op("act", [rw, r_g1c], [r_w4[1]], partial(A.activation, out=wtm[:, c, 0:512], in_=ws[:, 512:1024], func=AF.Copy, scale=sc))
                                kb.op("dve", [rw, r_g1c], [r_w4[2]], partial(V.tensor_scalar, wtm[:, c, 512:1048], ws[:, 1280:1816], sc, None, op0=ALU.mult))
                                kb.op("dve", [rw, r_g1c], [r_w4[3]], partial(V.tensor_scalar,
                                    wfc[:, c, :, :].rearrange("p g (k d) -> p g k d", k=2),
                                    ws[:, 1024:1280].rearrange("p (k g d) -> p g k d", k=2, g=2), sc, None, op0=ALU.mult))
                            kb.barrier()
                        kb.phase = 1
                        xs = [T(L4, "xs%d" % i, [128, 1024]) for i in range(2)]; r_xs = [Reg(), Reg()]
                        utm = [T(L4, "utm%d" % i, [128, 512], BF16) for i in range(2)]; r_utm = [Reg(), Reg()]
                        junk = T(L4, "junk", [128, 1024]); r_junk = Reg()
                        ssx = T(L4, "ssx", [128, NT]); r_ssx = [Reg() for _ in range(NT)]
                        xn = [T(L4, "xn%d" % i, [128, 1024], BF16) for i in range(2)]; r_xn = [Reg(), Reg()]
                        hT = [T(L4, "hT%d" % i, [128, 8, 512], BF16) for i in range(2)]
                        r_hT = [[Reg() for _ in range(4)] for _ in range(2)]
                        sq = [T(L4, "sq%d" % i, [128, 512]) for i in range(2)]; r_sq = [Reg(), Reg()]
                        ss8 = [T(L4, "ss8%d" % i, [128, 8]) for i in range(2)]; r_ss8 = [Reg(), Reg()]
                        kn = [T(L4, "kn%d" % i, [128, 256], BF16) for i in range(2)]; r_kn = [Reg(), Reg()]
                        sqq = T(L4, "sqq", [128, 512]); r_sqq = Reg()
                        ssq = T(L4, "ssq", [128, 8]); r_ssq = Reg()

                        kb.dma(xs[0][:], xp[0:128, :], [], [r_xs[0]])
                        for sup in range(8):
                            hb = hT[sup % 2]; rhb = r_hT[sup % 2]
                            for t in range(4):
                                i = sup * 4 + t
                                xb = xs[i % 2]; rxb = r_xs[i % 2]
                                if i + 1 < NT:
                                    kb.dma(xs[(i + 1) % 2][:], xp[(i + 1) * 128:(i + 2) * 128, :], [], [r_xs[(i + 1) % 2]])
                                own = i >= NT - NOWN
                                qi = i - (NT - NOWN)
                                xnb = xn[i % 2]; rxn = r_xn[i % 2]
                                kb.prio = 1
                                kb.op("act", [rxb], [r_junk], partial(A.activation, out=junk[:], in_=xb[:], func=AF.Square))
                                kb.op("dve", [r_junk], [r_ssx[i]], partial(V.tensor_reduce, out=ssx[:, i:i + 1], in_=junk[:], axis=AX.X, op=ALU.add))
                                rstd_from_ss(ssx[:, i:i + 1], 1024, [r_ssx[i]])
                                kb.op("dve", [rxb, r_ssx[i]], [rxn], partial(V.tensor_scalar, xnb[:], xb[:], ssx[:, i:i + 1], None, op0=ALU.mult))
                                for c in range(8):
                                    kb.op("pe", [rxn, r_idb], [r_pT], partial(PE.transpose, pT[:, c * 128:(c + 1) * 128], xnb[:, c * 128:(c + 1) * 128], idb[:]))
                                kb.op("act", [r_pT], [rhb[t]], partial(A.copy, hb[:, :, t * 128:(t + 1) * 128], pT[:].rearrange("p (c n) -> p c n", c=8)))
                                kb.prio = 0
                                bA, rA = pb[0 + (i % 2)], r_pb[0 + (i % 2)]
                                for c in range(8):
                                    kb.op("pe", [rhb[t]] + r_w4, [rA], partial(PE.matmul, bA[:], lhsT=hb[:, c, t * 128:(t + 1) * 128], rhs=wtm[:, c, 512:1024], start=(c == 0), stop=(c == 7)))
                                bU, rU = pb[6], r_pb[6]
                                for c in range(8):
                                    kb.op("pe", [rhb[t]] + r_w4, [rU], partial(PE.matmul, bU[:], lhsT=hb[:, c, t * 128:(t + 1) * 128], rhs=wfu[:, c, :], start=(c == 0), stop=(c == 7)))
                                kb.op("dve", [rU], [r_utm[i % 2]], partial(V.tensor_copy, utm[i % 2][:], bU[:]))
                                kb.dma(uTM[i * 128:(i + 1) * 128, :], utm[i % 2][:], [r_utm[i % 2]], [r_uTM[i]])
                                sqb, rsq = sq[i % 2], r_sq[i % 2]
                                s8, rs8 = ss8[i % 2], r_ss8[i % 2]
                                knb, rkn = kn[i % 2], r_kn[i % 2]
                                kb.op("act", [rA], [r_K[i]], partial(A.copy, Vs[:, i, :, 0:64], bA[:, 128:256].rearrange("p (g d) -> p g d", g=2)))
                                kb.op("act", [rA], [r_K[i]], partial(A.copy, Vw[:, i, :, 0:64], bA[:, 384:512].rearrange("p (g d) -> p g d", g=2)))
                                kb.op("act", [rA], [rsq], partial(A.activation, out=sqb[:], in_=bA[:], func=AF.Square))
                                kb.op("dve", [rsq], [rs8], partial(V.tensor_reduce, out=s8[:], in_=sqb[:].rearrange("p (g d) -> p g d", g=8), axis=AX.X, op=ALU.add))
                                rstd_from_ss(s8[:], 64, [rs8])
                                kb.op("dve", [rA, rs8], [rkn], partial(V.tensor_tensor,
                                    knb[:].rearrange("p (a g d) -> p a g d", a=2, g=2),
                                    bA[:].rearrange("p (a v g d) -> p a v g d", a=2, v=2, g=2)[:, :, 0, :, :],
                                    s8[:].rearrange("p (a v g) -> p a v g", a=2, v=2)[:, :, 0, :].unsqueeze(3).to_broadcast([128, 2, 2, 64]),
                                    op=ALU.mult))
                                for a in range(2):
                                    kb.op("pe", [rkn, r_idb], [r_pb[3]], partial(PE.matmul, pb[3][:, 256 + a * 128:256 + (a + 1) * 128], lhsT=knb[:, a * 128:(a + 1) * 128], rhs=idb[:], start=True, stop=True))
                                for g in range(2):
                                    kb.op("act", [r_pb[3], r_gcols, r_eb], [r_K[i]], partial(A.activation,
                                        out=KsT[g][0:64, i * 128:(i + 1) * 128], in_=pb[3][64 * g:64 * g + 64, 256:384], func=AF.Copy, scale=gcols[64 * g:64 * g + 64, 2:3]))
                                    kb.op("act", [r_pb[3], r_gcols], [r_K[i]], partial(A.activation,
                                        out=KwT[g][0:64, i * 128:(i + 1) * 128], in_=pb[3][64 * g:64 * g + 64, 384:512], func=AF.Copy, scale=gcols[64 * g:64 * g + 64, 3:4]))
                                if own:
                                    bQ, rQ = pb[2], r_pb[2]
                                    bG, rG = pb[3], r_pb[3]
                                    for c in range(8):
                                        kb.op("pe", [rhb[t]] + r_w4, [rQ], partial(PE.matmul, bQ[:], lhsT=hb[:, c, t * 128:(t + 1) * 128], rhs=wtm[:, c, 0:512], start=(c == 0), stop=(c == 7)))
                                    for c in range(8):
                                        kb.op("pe", [rhb[t]] + r_w4, [rG], partial(PE.matmul, bG[:, 0:24], lhsT=hb[:, c, t * 128:(t + 1) * 128], rhs=wtm[:, c, 1024:1048], start=(c == 0), stop=(c == 7)))
                                    kb.op("act", [rG], [r_gates[qi]], partial(A.copy, gates[:, qi, :], bG[:, 0:24]))
                                    kb.op("act", [rQ], [r_sqq], partial(A.activation, out=sqq[:], in_=bQ[:], func=AF.Square))
                                    kb.op("dve", [r_sqq], [r_ssq], partial(V.tensor_reduce, out=ssq[:], in_=sqq[:].rearrange("p (g d) -> p g d", g=8), axis=AX.X, op=ALU.add))
                                    rstd_from_ss(ssq[:], 64, [r_ssq])
                                    kb.op("dve", [rQ, r_ssq], [r_Q[qi]], partial(V.tensor_tensor,
                                        Qraw[:, qi, :].rearrange("p (g d) -> p g d", g=8), bQ[:].rearrange("p (g d) -> p g d", g=8),
                                        ssq[:].unsqueeze(2).to_broadcast([128, 8, 64]), op=ALU.mult))
                            for m in (4, 5):
                                bF, rF = pb[4 + (m % 2)], r_pb[4 + (m % 2)]
                                for c in range(8):
                                    lhs = wfc[:, c, m - 4, :]
                                    kb.op("pe", rhb + r_w4, [rF], partial(PE.matmul, bF[:], lhsT=lhs, rhs=hb[:, c, :], start=(c == 0), stop=(c == 7)))
                                kb.op("act", [rF], [r_kcv[sup]], partial(A.copy, kcvT[:, m - 4, :, sup * 32:(sup + 1) * 32], bF[:].rearrange("p (c r) -> p r c", r=16)))

                        if "uT" in dbg:
                            kb.dma(dout("uTM", [4096, 512], BF16), uTM, r_uTM, [])
                            kb.dma(dout("kcvT", [128, 2, 16, 256], BF16), kcvT[:], r_kcv, [])
                            kb.dma(dout("KsT0", [128, 4096], BF16), KsT[0][:], r_K + [r_eb], [])
                            kb.dma(dout("KwT1", [64, 4096], BF16), KwT[1][:], r_K, [])
                            kb.dma(dout("Vs", [128, NT, 2, 65], BF16), Vs[:], r_K, [])
                            kb.dma(dout("Qraw", [128, NOWN, 512], BF16), Qraw[:], r_Q, [])
                            kb.dma(dout("gates", [128, NOWN, 24]), gates[:], r_gates, [])
                            kb.dma(dout("ssx", [128, NT]), ssx[:], r_ssx, [])
                            kb.dma(dout("xn1", [128, 1024], BF16), xn[1][:], r_xn, [])
                            kb.dma(dout("hT", [128, 8, 512], BF16), hT[0][:], r_hT[0], [])
                            kb.dma(dout("wfu", [128, 8, 512], BF16), wfu[:], r_w4, [])
                            kb.dma(dout("wtm", [128, 8, 1048], BF16), wtm[:], r_w4, [])

                        if stage >= 2:
                            kb.barrier()
                            phase_compress(L4)
                    kb.barrier()
                    if stage >= 3:
                        phase_ssm(L3)
                kb.barrier()
                if stage >= 4:
                    phase_attn(L2)
            kb.barrier()
            if stage >= 5:
                MB = {}
                MB["tT"] = T(L1, "tT", [128, 8, 2048], BF16); MB["r_tTq"] = [Reg() for _ in range(NOWN)]
                MB["g2m"] = T(L1, "g2m", [128, 8]); MB["r_g2m"] = Reg()
                MB["wgs_"] = T(L1, "wgs_", [128, 8, 256]); MB["wus_"] = T(L1, "wus_", [128, 8, 256]); MB["wds_"] = T(L1, "wds_", [128, 2, 1024])
                MB["r_wgs"], MB["r_wus"], MB["r_wds"] = Reg(), Reg(), Reg()
                MB["wgb"] = [T(L1, "wgb%d" % i, [128, 8, 256], BF16) for i in range(2)]
                MB["wub"] = [T(L1, "wub%d" % i, [128, 8, 256], BF16) for i in range(2)]
                MB["wdb"] = [T(L1, "wdb%d" % i, [128, 2, 1024], BF16) for i in range(2)]
                MB["r_wb"] = [Reg(), Reg()]
                phase_post(L1)
                if stage >= 6:
                    phase_moe(L1)
        kb.barrier(("sp",))
    build_nc.sim_log = kb.sim_log
    return nc, dbg_out


def _consts(s):
    a = np.arange(128)
    c = {}
    c["c_ident"] = np.eye(128, dtype=np.float32)
    c["c_tlb"] = np.where(a[:, None] > a[None, :], -BIG, 0.0).astype(np.float32)
    c["c_sub"] = np.where(a[:, None] <= a[None, :], -BIG, 0.0).astype(np.float32)
    cc = np.arange(256)[:, None]
    ss = np.arange(64)[None, :]
    ov = ((cc * 16 < ss * 64 + 64) & (cc * 16 + 32 > ss * 64)).astype(np.float32)
    c["c_ov"] = np.concatenate([ov, np.ones((256, 1), np.float32)], axis=1)
    c["c_dmat"] = (16.0 * a[:, None] + 31.0 - a[None, :]).astype(np.float32)
    eb = np.zeros((64, 4096), np.float32)
    for blk in range(64):
        eb[blk, blk * 64:(blk + 1) * 64] = BIG
    c["c_eband"] = eb
    off = 2048 * (1 - s)
    pos = (np.arange(32)[None, :] * 128 + a[:, None])
    c["c_kbtok"] = np.where(pos < off, -BIG, 0.0).astype(np.float32)
    cb = (np.arange(2)[None, :] * 128 + a[:, None])
    c["c_kbcmp"] = np.where((cb < off // 16) | (cb > 254), -BIG, 0.0).astype(np.float32)
    f0 = np.zeros((128, 64), np.float32)
    f0[:, off // 64] = 1e9
    c["c_f0"] = f0
    c["c_iota"] = np.broadcast_to(np.arange(256, dtype=np.float32)[None, :], (128, 256)).copy()
    kall = np.concatenate([np.arange(7, -1, -1), -np.arange(1, 9), np.arange(1, 9)]).astype(np.float32)
    c["c_kall"] = np.broadcast_to(kall[None, :], (128, 24)).copy()
    kab = np.concatenate([16.0 * np.arange(8), np.arange(16)]).astype(np.float32)
    c["c_kab"] = np.broadcast_to(kab[None, :], (128, 24)).copy()
    c["c_tmask"] = ((a[None, :] // 16) >= (a[:, None] // 16)).astype(np.float32)
    return c


_WNAMES = ["norm1_g", "w_in", "lam_re", "lam_im", "log_step", "b_re", "b_im", "c_re", "c_im", "d_skip",
           "w_glu", "b_glu", "g_q", "g_kc", "g_ks", "g_kw", "pos_k", "pos_v", "w_ck1", "w_ck2", "w_cv1", "w_cv2",
           "out_g_ssm", "out_g_att", "w_out", "norm2_g", "w_grp", "b_grp", "w_exp", "b_exp", "w_gate", "w_up", "w_down"]


def make_in_maps(inputs, cores=range(8)):
    x = np.asarray(inputs["x"], dtype=np.float32)
    w = {k: np.ascontiguousarray(np.asarray(inputs[k], dtype=np.float32)[0]) for k in _WNAMES}
    maps = []
    for core in cores:
        b, s = core // 2, core % 2
        m = dict(w)
        if s == 1:
            m["xp"] = np.ascontiguousarray(x[b])
        else:
            m["xp"] = np.concatenate([np.zeros((2048, 1024), np.float32), x[b, :2048]], axis=0)
        m["xown"] = np.ascontiguousarray(x[b, 2048 * s:2048 * (s + 1)])
        m.update(_consts(s))
        maps.append(m)
    return maps


def kernel(**inputs):
    nc, _ = build_nc()
    maps = make_in_maps(inputs)
    res = run_bass_kernel_spmd(nc, maps, core_ids=list(range(8)))
    outp = np.empty((4, 4096, 1024), np.float32)
    for core in range(8):
        b, s = core // 2, core % 2
        outp[b, 2048 * s:2048 * (s + 1)] = res.results[core]["out"]
    return outp
```

```python
import math
from contextlib import ExitStack
from functools import partial

import numpy as np
import concourse.bass as bass
import concourse.mybir as mybir
from concourse.bass_utils import run_bass_kernel_spmd

F32 = mybir.dt.float32
BF16 = mybir.dt.bfloat16
I32 = mybir.dt.int32
ALU = mybir.AluOpType
AF = mybir.ActivationFunctionType
AX = mybir.AxisListType

BIG = 250.0
EPS = 1e-6
PI = math.pi
TCH = 256
NT = 32
NOWN = 16


class Reg:
    __slots__ = ("w", "r")

    def __init__(self):
        self.w = None
        self.r = []


class Op:
    __slots__ = ("idx", "eng", "fn", "preds", "dur", "lat", "ev", "nun", "succ", "ready", "fin", "is_dma", "ph", "prio")


def _free_elems(ap):
    n = 1
    for s in ap.shape[1:]:
        n *= int(s)
    return n


def _c0(f):
    return f()


def _c1(f):
    return f()


def _c2(f):
    return f()


def _c3(f):
    return f()


def _c4(f):
    return f()


def _c5(f):
    return f()


def _c6(f):
    return f()


def _c7(f):
    return f()


_CALLERS = [_c0, _c1, _c2, _c3, _c4, _c5, _c6, _c7]


class KB:
    NDMA = 24
    SCHED = True

    def __init__(self, nc, es):
        self.nc = nc
        self.eng = {"pe": nc.tensor, "act": nc.scalar, "dve": nc.vector,
                    "pool": nc.gpsimd, "sp": nc.sync}
        self.sems = {}
        self.cnt = {}
        for k in ("pe", "act", "dve", "pool"):
            self.sems[k] = es.enter_context(nc.semaphore("s_" + k))
            self.cnt[k] = 0
        for i in range(self.NDMA):
            k = "d%d" % i
            self.sems[k] = es.enter_context(nc.semaphore("s_" + k))
            self.cnt[k] = 0
        self.seen = {e: {} for e in self.eng}
        self.dma_rr = 0
        self.pending = []
        self.nops = 0
        self.phase = 0
        self.sim_log = []
        self.mute = False
        self.ck_limit = 99
        self.prio = 0

    def _wait(self, e, ev):
        if ev is None:
            return
        k, v = ev
        if e == "pe" and k == "pe":
            return
        if self.seen[e].get(k, 0) >= v:
            return
        self.eng[e].wait_ge(self.sems[k], v)
        self.seen[e][k] = v

    def _mk(self, e, reads, writes, fn, is_dma):
        o = Op()
        o.idx = self.nops
        self.nops += 1
        o.eng = e
        o.fn = fn
        o.ev = None
        o.is_dma = is_dma
        o.ph = self.phase
        o.prio = self.prio
        o.dur = 0.1
        o.lat = 0.0
        preds = {}
        for rg in reads:
            if rg.w is not None:
                preds[id(rg.w)] = rg.w
        for rg in writes:
            if rg.w is not None:
                preds[id(rg.w)] = rg.w
            for x in rg.r:
                preds[id(x)] = x
        preds.pop(id(o), None)
        o.preds = list(preds.values())
        for rg in reads:
            rg.r.append(o)
        for rg in writes:
            rg.w = o
            rg.r = []
        if self.mute:
            o.ev = ("pe", 0)
            o.preds = None
            return o
        self.pending.append(o)
        if not self.SCHED:
            self.flush()
        return o

    def op(self, e, reads, writes, fn):
        o = self._mk(e, reads, writes, fn, False)
        try:
            out = fn.keywords.get("out", fn.args[0] if fn.args else None)
            n = _free_elems(out)
            nm = fn.func.__name__
        except Exception:
            n, nm = 256, ""
        if e == "pe":
            o.dur = 0.035 + n / 2400.0
        elif e == "act":
            o.dur = 0.2 + n * 0.00105
        elif e == "dve":
            o.dur = 0.1 + n * (0.0029 if "scan" in nm else 0.0013)
        else:
            o.dur = 0.18 + n * 0.00212
        return o

    def dma(self, out, in_, reads, writes, q="sp", **kw):
        o = self._mk(q, reads, writes, partial(self.eng[q].dma_start, out=out, in_=in_, **kw), True)
        try:
            nb = 1
            for s in out.shape:
                nb *= int(s)
            nb *= 4
        except Exception:
            nb = 65536
        o.dur = 0.06
        o.lat = 2.0 + nb / 60000.0
        return o

    def _emit(self, o):
        e = o.eng
        if o.is_dma:
            dk = "d%d" % self.dma_rr
            self.dma_rr = (self.dma_rr + 1) % self.NDMA
            if self.cnt[dk] > 0:
                self._wait(e, (dk, self.cnt[dk]))
            for p in o.preds:
                self._wait(e, p.ev)
            ins = _CALLERS[o.ph](o.fn)
            self.cnt[dk] += 16
            ins.then_inc(self.sems[dk], 16)
            o.ev = (dk, self.cnt[dk])
        else:
            for p in o.preds:
                self._wait(e, p.ev)
            ins = _CALLERS[o.ph](o.fn)
            self.cnt[e] += 1
            ins.then_inc(self.sems[e], 1)
            o.ev = (e, self.cnt[e])
        o.fn = None
        o.preds = None
        o.succ = None

    def flush(self):
        ops = self.pending
        self.pending = []
        if not ops:
            return
        if len(ops) == 1:
            self._emit(ops[0])
            return
        for o in ops:
            o.succ = []
            o.ready = 0.0
        for o in ops:
            o.nun = 0
            for p in o.preds:
                if p.ev is None:
                    o.nun += 1
                    p.succ.append(o)
        tfree = {e: 0.0 for e in self.eng}
        cand = {e: [] for e in self.eng}
        for o in ops:
            if o.nun == 0:
                cand[o.eng].append(o)
        dma_fin = []
        nleft = len(ops)
        ndma = self.NDMA
        order = []
        while nleft:
            best = None
            bo = None
            for e, cl in cand.items():
                if not cl:
                    continue
                tf = tfree[e]
                if e == "sp" and len(dma_fin) >= ndma and dma_fin[-ndma] > tf:
                    tf = dma_fin[-ndma]
                for o in cl:
                    st = o.ready if o.ready > tf else tf
                    key = (st, -o.prio, o.idx)
                    if best is None or key < best:
                        best = key
                        bo = o
            o = bo
            st = best[0]
            e = o.eng
            cand[e].remove(o)
            tfree[e] = st + o.dur
            if o.is_dma:
                o.fin = st + o.lat
                dma_fin.append(o.fin)
            else:
                o.fin = tfree[e]
            order.append(o)
            for s in o.succ:
                if o.fin > s.ready:
                    s.ready = o.fin
                s.nun -= 1
                if s.nun == 0:
                    cand[s.eng].append(s)
            nleft -= 1
        self.sim_log.append((order[0].ph, len(order), max(o.fin for o in order), dict(tfree)))
        for o in order:
            self._emit(o)

    def ckpt(self, k):
        if k > self.ck_limit:
            self.mute = True

    def barrier(self, engines=("pe", "act", "dve", "pool", "sp")):
        self.mute = False
        self.flush()
        for e in engines:
            for k in self.sems:
                if self.cnt[k] > 0:
                    self._wait(e, (k, self.cnt[k]))


def build_nc(dbg=(), stage=99, ssm_stop=99, ck=99):
    nc = bass.Bass("TRN2", target_bir_lowering=False)
    D = {}

    def din(name, shape):
        D[name] = nc.dram_tensor(name, list(shape), F32, kind="ExternalInput").ap()
        return D[name]

    xp = din("xp", [4096, 1024])
    xown = din("xown", [2048, 1024])
    din("norm1_g", [1024]); din("w_in", [1024, 1816])
    din("lam_re", [32, 64]); din("lam_im", [32, 64]); din("log_step", [32])
    din("b_re", [32, 64, 16]); din("b_im", [32, 64, 16])
    din("c_re", [32, 16, 64]); din("c_im", [32, 16, 64]); din("d_skip", [32, 16])
    din("w_glu", [512, 512]); din("b_glu", [512])
    din("g_q", [64]); din("g_kc", [64]); din("g_ks", [64]); din("g_kw", [64])
    din("pos_k", [32, 64]); din("pos_v", [32, 64])
    din("w_ck1", [2048, 256]); din("w_ck2", [256, 64]); din("w_cv1", [2048, 256]); din("w_cv2", [256, 64])
    din("out_g_ssm", [512]); din("out_g_att", [512]); din("w_out", [1024, 1024])
    din("norm2_g", [1024]); din("w_grp", [1024, 4]); din("b_grp", [4]); din("w_exp", [1024, 32]); din("b_exp", [32])
    din("w_gate", [32, 1024, 256]); din("w_up", [32, 1024, 256]); din("w_down", [32, 256, 1024])
    din("c_ident", [128, 128]); din("c_tlb", [128, 128]); din("c_sub", [128, 128])
    din("c_ov", [256, 65]); din("c_dmat", [128, 128]); din("c_eband", [64, 4096])
    din("c_kbtok", [128, 32]); din("c_kbcmp", [128, 2]); din("c_f0", [128, 64])
    din("c_iota", [128, 256]); din("c_kall", [128, 24]); din("c_kab", [128, 24]); din("c_tmask", [128, 128])
    out = nc.dram_tensor("out", [2048, 1024], F32, kind="ExternalOutput").ap()
    x1d = nc.dram_tensor("x1d", [2048, 1024], F32, kind="Internal").ap()
    tTd = nc.dram_tensor("tTd", [128, 8, 2048], BF16, kind="Internal").ap()
    uTM = nc.dram_tensor("uTMd", [4096, 512], BF16, kind="Internal").ap()
    woutd = nc.dram_tensor("woutd", [128, 8, 1024], BF16, kind="Internal").ap()
    r_woutd = [Reg() for _ in range(8)]
    ysTd = nc.dram_tensor("ysTd", [128, 4, 2048], BF16, kind="Internal").ap()
    oattTd = nc.dram_tensor("oattTd", [128, 4, 2048], BF16, kind="Internal").ap()
    dbg_out = {}
    r_x1d = [Reg() for _ in range(NOWN)]
    r_tTd = [Reg() for _ in range(NOWN)]

    def dout(name, shape, dt=F32):
        dbg_out[name] = nc.dram_tensor("dbg_" + name, list(shape), dt, kind="ExternalOutput").ap()
        return dbg_out[name]

    with ExitStack() as es:
        kb = KB(nc, es)
        kb.ck_limit = ck
        V, G, A, PE = nc.vector, nc.gpsimd, nc.scalar, nc.tensor

        def T(st, name, shape, dt=F32):
            return st.enter_context(nc.sbuf_tensor(name, list(shape), dt))

        pb = [es.enter_context(nc.psum_tensor("pb%d" % i, [128, 512], F32)) for i in range(7)]
        r_pb = [Reg() for _ in range(7)]
        pT = es.enter_context(nc.psum_tensor("pT", [128, 1024], BF16))
        r_pT = Reg()

        idf = T(es, "idf", [128, 128]); r_idf = Reg()
        idb = T(es, "idb", [128, 128], BF16); r_idb = Reg()
        tlb4 = T(es, "tlb4", [128, 4, 128], BF16); r_tlb4 = Reg()
        sub4 = T(es, "sub4", [128, 4, 128], BF16); r_sub4 = Reg()
        dmat = T(es, "dmat", [128, 128]); r_dmat = Reg()
        ovb = T(es, "ovb", [128, 2, 65], BF16); r_ovb = Reg()
        kbtok = T(es, "kbtok", [128, 32]); r_kbtok = Reg()
        kbcmp = T(es, "kbcmp", [128, 2]); r_kbcmp = Reg()
        f0 = T(es, "f0", [128, 64]); r_f0 = Reg()
        gcols = T(es, "gcols", [128, 4]); r_gcols = Reg()
        comb = T(es, "comb", [128, NOWN, 32]); r_comb = [Reg() for _ in range(NOWN)]
        ss_ssm = T(es, "ss_ssm", [128, NOWN]); r_ss_ssm = [Reg() for _ in range(NOWN)]
        ss_att = T(es, "ss_att", [128, NOWN]); r_ss_att = [Reg() for _ in range(NOWN)]
        cst = T(es, "cst", [128, 512]); r_cst = Reg()
        onesf = T(es, "onesf", [128, 1]); r_onesf = Reg()

        kb.dma(idf[:], D["c_ident"], [], [r_idf])
        kb.dma(dmat[:], D["c_dmat"], [], [r_dmat])
        kb.dma(kbtok[:], D["c_kbtok"], [], [r_kbtok])
        kb.dma(kbcmp[:], D["c_kbcmp"], [], [r_kbcmp])
        kb.dma(f0[:], D["c_f0"], [], [r_f0])
        kb.op("dve", [r_idf], [r_idb], partial(V.tensor_copy, idb[:], idf[:]))
        kb.op("dve", [], [r_onesf], partial(V.memset, onesf[:], 1.0))
        kb.dma(cst[:, 0:128], D["c_tlb"], [], [r_cst])
        kb.dma(cst[:, 128:256], D["c_sub"], [], [r_cst])
        kb.op("dve", [r_cst], [r_tlb4], partial(V.tensor_copy, tlb4[:], cst[:, 0:128].unsqueeze(1).to_broadcast([128, 4, 128])))
        kb.op("dve", [r_cst], [r_sub4], partial(V.tensor_copy, sub4[:], cst[:, 128:256].unsqueeze(1).to_broadcast([128, 4, 128])))
        kb.dma(cst[:, 256:386].rearrange("p (j n) -> p j n", j=2), D["c_ov"].rearrange("(j p) n -> p j n", p=128), [], [r_cst])
        kb.op("dve", [r_cst], [r_ovb], partial(V.tensor_copy, ovb[:], cst[:, 256:386].rearrange("p (j n) -> p j n", j=2)))
        for gi, gname in enumerate(("g_q", "g_kc", "g_ks", "g_kw")):
            src = D[gname].rearrange("(d o) -> d o", o=1)
            kb.dma(gcols[0:64, gi:gi + 1], src, [], [r_gcols])
            kb.dma(gcols[64:128, gi:gi + 1], src, [], [r_gcols])
        kb.op("dve", [r_gcols], [r_gcols], partial(V.tensor_scalar, gcols[:, 0:1], gcols[:, 0:1], 0.125, None, op0=ALU.mult))

        rowst = T(es, "rowst", [16, 128]); r_rowst = Reg()

        def load_cols(dst, r_dst, src1d, n, bank=6):
            kb.dma(rowst[0:n, :], src1d.rearrange("(c p) -> c p", p=128), [], [r_rowst])
            kb.op("pe", [r_rowst, r_idf], [r_pb[bank]], partial(PE.transpose, pb[bank][:, 0:n], rowst[0:n, :], idf[0:n, 0:n]))
            kb.op("dve", [r_pb[bank]], [r_dst], partial(V.tensor_copy, dst, pb[bank][:, 0:n]))

        def rstd_from_ss(ss_ap, n, regs):
            kb.op("act", regs, regs, partial(A.activation, out=ss_ap, in_=ss_ap, func=AF.Sqrt, scale=1.0 / n, bias=EPS))
            kb.op("dve", regs, regs, partial(V.reciprocal, ss_ap, ss_ap))

        def rstd_lnexp(ss_ap, n, regs):
            kb.op("act", regs, regs, partial(A.activation, out=ss_ap, in_=ss_ap, func=AF.Ln, scale=1.0 / n, bias=EPS))
            kb.op("act", regs, regs, partial(A.activation, out=ss_ap, in_=ss_ap, func=AF.Exp, scale=-0.5))

        GK = 1.5957691216057308

        def gelu_ops(out_ap, y_ap, t1, t2, regs_in, r_t, r_out, shape_view=None):
            kb.op("act", regs_in, [r_t], partial(A.activation, out=t1, in_=y_ap, func=AF.Square, scale=math.sqrt(0.044715)))
            kb.op("dve", regs_in + [r_t], [r_t], partial(V.scalar_tensor_tensor, out=t1, in0=t1, scalar=1.0, in1=y_ap, op0=ALU.add, op1=ALU.mult))
            kb.op("act", [r_t], [r_t], partial(A.activation, out=t2, in_=t1, func=AF.Sigmoid, scale=GK))
            kb.op("dve", regs_in + [r_t], [r_out], partial(V.tensor_tensor, out_ap, y_ap, t2, op=ALU.mult))

        def phase_compress(L):
            kb.phase = 2
            with ExitStack() as LC:
                W1 = T(LC, "W1", [128, 32, 256], BF16); r_W1 = Reg()
                W2 = T(LC, "W2", [128, 2, 2, 64], BF16); r_W2 = Reg()
                posT = T(LC, "posT", [128, 32], BF16); r_posT = Reg()
                hcb = T(LC, "hcb", [128, 4]); r_hcb = Reg()
                hcT = T(LC, "hcT", [128, 2, 256], BF16); r_hcT = Reg()
                w1s = [T(LC, "w1s%d" % i, [128, 8, 256]) for i in range(2)]; r_w1s = [Reg(), Reg()]
                w2s = T(LC, "w2s", [128, 2, 2, 64]); r_w2s = Reg()
                pst = T(LC, "pst", [32, 128]); r_pst = Reg()
                yb = T(LC, "cyb", [128, 256]); r_yb = Reg()
                ct1 = T(LC, "ct1", [128, 256]); ct2 = T(LC, "ct2", [128, 256]); r_ct = Reg()
                csq = T(LC, "csq", [128, 64]); r_csq = Reg()
                css = T(LC, "css", [128, 1]); r_css = Reg()
                ckn = T(LC, "ckn", [128, 64], BF16); r_ckn = Reg()
                kb.op("pool", [], [r_hcT], partial(G.memset, hcT[:], 0.0))
                srcs = (D["w_ck1"].rearrange("(j d) h -> d j h", d=64), D["w_cv1"].rearrange("(j d) h -> d j h", d=64))
                for q4 in range(4):
                    ws, rw = w1s[q4 % 2], r_w1s[q4 % 2]
                    for kv in range(2):
                        kb.dma(ws[64 * kv:64 * kv + 64, :, :], srcs[kv][:, q4 * 8:(q4 + 1) * 8, :], [], [rw])
                    kb.op("dve", [rw], [r_W1], partial(V.tensor_copy, W1[:, q4 * 8:(q4 + 1) * 8, :], ws[:]))
                for kv, nm in enumerate(("w_ck2", "w_cv2")):
                    kb.dma(w2s[:, kv, :, :], D[nm].rearrange("(m p) d -> p m d", p=128), [], [r_w2s])
                kb.op("dve", [r_w2s], [r_W2], partial(V.tensor_copy, W2[:], w2s[:]))
                kb.dma(pst[:, 0:64], D["pos_k"], [], [r_pst])
                kb.dma(pst[:, 64:128], D["pos_v"], [], [r_pst])
                kb.op("pe", [r_pst, r_idf], [r_pb[6]], partial(PE.transpose, pb[6][:, 0:32], pst[:], idf[0:32, 0:32]))
                kb.op("dve", [r_pb[6]], [r_posT], partial(V.tensor_copy, posT[:], pb[6][:, 0:32]))
                for kv in range(2):
                    for m in range(2):
                        idx = kv * 2 + m
                        for j in range(32):
                            kb.op("pe", [r_W1, r_posT], [r_pb[5]], partial(PE.matmul,
                                pb[5][:, idx:idx + 1], lhsT=W1[64 * kv:64 * kv + 64, j, m * 128:(m + 1) * 128],
                                rhs=posT[64 * kv:64 * kv + 64, j:j + 1], start=(j == 0), stop=(j == 31)))
                kb.op("dve", [r_pb[5]], [r_hcb], partial(V.tensor_copy, hcb[:], pb[5][:, 0:4]))
                nb = 0
                for kv in range(2):
                    for g in range(2):
                        for m in range(2):
                            bk, rbk = pb[nb % 2], r_pb[nb % 2]; nb += 1
                            for j in range(32):
                                kb.op("pe", [r_W1] + r_kcv, [rbk], partial(PE.matmul,
                                    bk[:, 0:255], lhsT=W1[64 * kv:64 * kv + 64, j, m * 128:(m + 1) * 128],
                                    rhs=kcvT[64 * kv:64 * kv + 64, g, j % 16, (j // 16):(j // 16) + 255], start=(j == 0), stop=(j == 31)))
                            kb.op("act", [rbk, r_hcb], [r_yb], partial(A.activation,
                                out=yb[:, 0:255], in_=bk[:, 0:255], func=AF.Identity, bias=hcb[:, kv * 2 + m:kv * 2 + m + 1], scale=1.0))
                            gelu_ops(hcT[:, m, 0:255], yb[:, 0:255], ct1[:, 0:255], ct2[:, 0:255], [r_yb], r_ct, r_hcT)
                        for ct in range(2):
                            b2, rb2 = pb[2 + ct], r_pb[2 + ct]
                            for m in range(2):
                                kb.op("pe", [r_hcT, r_W2], [rb2], partial(PE.matmul,
                                    b2[:, 0:64], lhsT=hcT[:, m, ct * 128:(ct + 1) * 128], rhs=W2[:, kv, m, :], start=(m == 0), stop=(m == 1)))
                            if kv == 1:
                                kb.op("act", [rb2], [r_cmp], partial(A.copy, Vc[:, ct, g, 0:64], b2[:, 0:64]))
                            else:
                                kb.op("act", [rb2], [r_csq], partial(A.activation, out=csq[:], in_=b2[:, 0:64], func=AF.Square))
                                kb.op("dve", [r_csq], [r_css], partial(V.tensor_reduce, out=css[:], in_=csq[:], axis=AX.X, op=ALU.add))
                                rstd_from_ss(css[:], 64, [r_css])
                                kb.op("dve", [rb2, r_css], [r_ckn], partial(V.tensor_scalar, ckn[:], b2[:, 0:64], css[:, 0:1], None, op0=ALU.mult))
                                kb.op("pe", [r_ckn, r_idb], [r_pT], partial(PE.transpose, pT[0:64, 0:128], ckn[:], idb[:]))
                                kb.op("act", [r_pT, r_gcols], [r_cmp], partial(A.activation,
                                    out=KcT[g][:, ct * 128:(ct + 1) * 128], in_=pT[0:64, 0:128], func=AF.Copy, scale=gcols[0:64, 1:2]))
                if "cmp" in dbg:
                    kb.dma(dout("KcT0", [64, 256], BF16), KcT[0][:], [r_cmp], [])
                    kb.dma(dout("KcT1", [64, 256], BF16), KcT[1][:], [r_cmp], [])
                    kb.dma(dout("Vc", [128, 2, 2, 65], BF16), Vc[:], [r_cmp], [])
                kb.barrier()

        def phase_ssm(L):
            kb.phase = 3
            TS = 128
            with ExitStack() as LS:
                WB = T(LS, "WB", [128, 16, 2, 128], BF16); r_WB = Reg()
                Toep = T(LS, "Toep", [128, 32, 128], BF16); r_Toep = Reg()
                Qt = T(LS, "Qt", [128, 2, 2, 16, 128], BF16); r_Qt = Reg()
                cosT = T(LS, "cosT", [128, 16, TS]); sinT = T(LS, "sinT", [128, 16, TS]); r_tab = Reg()
                rcol = T(LS, "rcol", [128, 16]); cTs = T(LS, "cTs", [128, 2, 16]); r_par = Reg()
                bgl = T(LS, "bgl", [128, 4]); r_dsk = Reg()
                wglu = T(LS, "wglu", [128, 4, 512], BF16); r_wglu = Reg()
                with ExitStack() as LP:
                    lamre = T(LP, "lamre", [128, 16]); lamim = T(LP, "lamim", [128, 16]); lsb = T(LP, "lsb", [128, 32])
                    dlt = T(LP, "dlt", [128, 16]); th = T(LP, "th", [128, 16]); r_p = Reg()
                    cth = T(LP, "cth", [128, 16]); sth = T(LP, "sth", [128, 16])
                    er = T(LP, "er", [128, 16]); ei = T(LP, "ei", [128, 16]); cr = T(LP, "cr", [128, 16]); ci = T(LP, "ci", [128, 16])
                    tA = T(LP, "tA", [128, 16]); tB = T(LP, "tB", [128, 16]); tD = T(LP, "tD", [128, 16])
                    ki = T(LP, "ki", [128, 384], I32); kf = T(LP, "kf", [128, 384]); red = T(LP, "red", [128, 384]); ang = T(LP, "ang", [128, TS])
                    kab = T(LP, "kab", [128, 24]); kall = T(LP, "kall", [128, 24])
                    bre = T(LP, "bre", [128, 16, 16]); bim = T(LP, "bim", [128, 16, 16])
                    Bb = T(LP, "Bb", [128, 16, 2, 16]); tb1 = T(LP, "tb1", [128, 16, 16]); tb2 = T(LP, "tb2", [128, 16, 16])
                    cnat = T(LP, "cnat", [128, 2, 4, 64]); CTt = T(LP, "CTt", [64, 2, 4, 128]); CQ = T(LP, "CQ", [128, 16, 2, 16])
                    wgs = T(LP, "wgs", [128, 2, 512]); r_wgs = Reg()
                    angk = T(LP, "angk", [128, 16, 24]); magk = T(LP, "magk", [128, 16, 24])
                    ckk = T(LP, "ckk", [128, 16, 24]); skk = T(LP, "skk", [128, 16, 24])
                    PWr = T(LP, "PWr", [128, 16, 24]); PWi = T(LP, "PWi", [128, 16, 24])
                    tmA = T(LP, "tmA", [128, 16, 8, 16]); tmB = T(LP, "tmB", [128, 16, 8, 16])
                    PwB = T(LP, "PwB", [128, 2, 16, 128], BF16); PpB = T(LP, "PpB", [128, 2, 16, 128], BF16); QT2 = T(LP, "QT2", [128, 2, 16, 128], BF16)
                    tmask = T(LP, "tmask", [128, 128]); ToepF = T(LP, "ToepF", [128, 4, 128])
                    dsr = T(LP, "dsr", [32, 16]); dsr8 = T(LP, "dsr8", [32, 8, 16]); dskT = T(LP, "dskT", [128, 32])
                    r_s = Reg()

                    def S(e, fn):
                        kb.op(e, [r_s, r_p], [r_s, r_p], fn)

                    def sincos(cos_out, sin_out, a_ap, n):
                        K_, F_, R_ = ki[:, 0:n], kf[:, 0:n], red[:, 0:n]
                        S("dve", partial(V.tensor_scalar, K_, a_ap, 1.0 / (2 * PI), None, op0=ALU.mult))
                        S("dve", partial(V.tensor_copy, F_, K_))
                        S("dve", partial(V.scalar_tensor_tensor, out=R_, in0=F_, scalar=-2 * PI, in1=a_ap, op0=ALU.mult, op1=ALU.add))
                        S("dve", partial(V.tensor_scalar, F_, R_, PI, None, op0=ALU.is_gt))
                        S("dve", partial(V.scalar_tensor_tensor, out=R_, in0=F_, scalar=-2 * PI, in1=R_, op0=ALU.mult, op1=ALU.add))
                        S("dve", partial(V.tensor_scalar, F_, R_, -PI, None, op0=ALU.is_lt))
                        S("dve", partial(V.scalar_tensor_tensor, out=R_, in0=F_, scalar=2 * PI, in1=R_, op0=ALU.mult, op1=ALU.add))
                        S("act", partial(A.activation, out=sin_out, in_=R_, func=AF.Sin))
                        S("dve", partial(V.tensor_scalar, R_, R_, PI / 2, None, op0=ALU.add))
                        S("dve", partial(V.tensor_scalar, F_, R_, PI, None, op0=ALU.is_gt))
                        S("dve", partial(V.scalar_tensor_tensor, out=R_, in0=F_, scalar=-2 * PI, in1=R_, op0=ALU.mult, op1=ALU.add))
                        S("act", partial(A.activation, out=cos_out, in_=R_, func=AF.Sin))

                    load_cols(lamre[:], r_p, D["lam_re"].rearrange("g p -> (g p)"), 16)
                    load_cols(lamim[:], r_p, D["lam_im"].rearrange("g p -> (g p)"), 16)
                    load_cols(bgl[:], r_dsk, D["b_glu"], 4)
                    kb.dma(lsb[:], D["log_step"].rearrange("(o g) -> o g", o=1).partition_broadcast(128), [], [r_p])
                    kb.dma(kab[:], D["c_kab"], [], [r_p])
                    kb.dma(kall[:], D["c_kall"], [], [r_p])
                    kb.dma(tmask[:], D["c_tmask"], [], [r_p])
                    kb.dma(dsr[:], D["d_skip"], [], [r_p])
                    kb.dma(bre[:], D["b_re"].rearrange("g p h -> (g p) h").rearrange("(st q) h -> q st h", q=128), [], [r_p])
                    kb.dma(bim[:], D["b_im"].rearrange("g p h -> (g p) h").rearrange("(st q) h -> q st h", q=128), [], [r_p])
                    kb.dma(cnat[:, 0, :, :], D["c_re"].rearrange("g h p -> (g h) p").rearrange("(m q) p -> q m p", q=128), [], [r_p])
                    kb.dma(cnat[:, 1, :, :], D["c_im"].rearrange("g h p -> (g h) p").rearrange("(m q) p -> q m p", q=128), [], [r_p])
                    for hh in range(2):
                        kb.dma(wgs[:], D["w_glu"].rearrange("(m p) n -> p m n", p=128)[:, 2 * hh:2 * hh + 2, :], [], [r_wgs])
                        kb.op("pool", [r_wgs], [r_wglu], partial(G.tensor_copy, wglu[:, 2 * hh:2 * hh + 2, :], wgs[:]))
                    lsv = lsb[:].rearrange("p (st gg) -> p gg st", gg=2)
                    S("dve", partial(V.tensor_copy, dlt[0:64, :], lsv[0:64, 0, :]))
                    S("dve", partial(V.tensor_copy, dlt[64:128, :], lsv[64:128, 1, :]))
                    S("act", partial(A.activation, out=dlt[:], in_=dlt[:], func=AF.Exp))
                    S("dve", partial(V.tensor_tensor, th[:], lamim[:], dlt[:], op=ALU.mult))
                    S("dve", partial(V.tensor_tensor, tD[:], lamre[:], dlt[:], op=ALU.mult))
                    kb_ = kall[:].unsqueeze(1).to_broadcast([128, 16, 24])
                    S("dve", partial(V.tensor_tensor, angk[:], th[:].unsqueeze(2).to_broadcast([128, 16, 24]), kb_, op=ALU.mult))
                    S("dve", partial(V.tensor_tensor, magk[:], tD[:].unsqueeze(2).to_broadcast([128, 16, 24]), kb_, op=ALU.mult))
                    S("act", partial(A.activation, out=magk[:], in_=magk[:], func=AF.Exp))
                    sincos(ckk[:].rearrange("p a b -> p (a b)"), skk[:].rearrange("p a b -> p (a b)"), angk[:].rearrange("p a b -> p (a b)"), 384)
                    S("dve", partial(V.tensor_tensor, PWr[:], magk[:], ckk[:], op=ALU.mult))
                    S("dve", partial(V.tensor_tensor, PWi[:], magk[:], skk[:], op=ALU.mult))
                    kb.ckpt(1)
                    S("act", partial(A.activation, out=tA[:], in_=tD[:], func=AF.Exp))
                    sincos(cth[:], sth[:], th[:], 16)
                    S("dve", partial(V.tensor_tensor, er[:], tA[:], cth[:], op=ALU.mult))
                    S("dve", partial(V.tensor_scalar, er[:], er[:], -1.0, None, op0=ALU.add))
                    S("dve", partial(V.tensor_tensor, ei[:], tA[:], sth[:], op=ALU.mult))
                    S("dve", partial(V.tensor_tensor, tA[:], lamre[:], lamre[:], op=ALU.mult))
                    S("dve", partial(V.tensor_tensor, tB[:], lamim[:], lamim[:], op=ALU.mult))
                    S("dve", partial(V.tensor_tensor, tA[:], tA[:], tB[:], op=ALU.add))
                    S("dve", partial(V.reciprocal, tA[:], tA[:]))
                    S("dve", partial(V.tensor_tensor, cr[:], er[:], lamre[:], op=ALU.mult))
                    S("dve", partial(V.tensor_tensor, tB[:], ei[:], lamim[:], op=ALU.mult))
                    S("dve", partial(V.tensor_tensor, cr[:], cr[:], tB[:], op=ALU.add))
                    S("dve", partial(V.tensor_tensor, cr[:], cr[:], tA[:], op=ALU.mult))
                    S("dve", partial(V.tensor_tensor, ci[:], ei[:], lamre[:], op=ALU.mult))
                    S("dve", partial(V.tensor_tensor, tB[:], er[:], lamim[:], op=ALU.mult))
                    S("dve", partial(V.tensor_tensor, ci[:], ci[:], tB[:], op=ALU.subtract))
                    S("dve", partial(V.tensor_tensor, ci[:], ci[:], tA[:], op=ALU.mult))
                    crb = cr[:].unsqueeze(2).to_broadcast([128, 16, 16])
                    cib = ci[:].unsqueeze(2).to_broadcast([128, 16, 16])
                    S("dve", partial(V.tensor_tensor, tb1[:], bre[:], crb, op=ALU.mult))
                    S("dve", partial(V.tensor_tensor, tb2[:], bim[:], cib, op=ALU.mult))
                    S("dve", partial(V.tensor_tensor, Bb[:, :, 0, :], tb1[:], tb2[:], op=ALU.subtract))
                    S("dve", partial(V.tensor_tensor, tb1[:], bim[:], crb, op=ALU.mult))
                    S("dve", partial(V.tensor_tensor, tb2[:], bre[:], cib, op=ALU.mult))
                    S("dve", partial(V.tensor_tensor, Bb[:, :, 1, :], tb1[:], tb2[:], op=ALU.add))
                    kb.ckpt(2)
                    for ri in range(2):
                        for m in range(4):
                            kb.op("pe", [r_s, r_p, r_idf], [r_pb[5]], partial(PE.transpose, pb[5][0:64, m * 128:(m + 1) * 128], cnat[:, ri, m, :], idf[:]))
                        kb.op("dve", [r_pb[5]], [r_s], partial(V.tensor_copy, CTt[:, ri, :, :], pb[5][0:64, :].rearrange("p (m n) -> p m n", m=4)))
                    for ri in range(2):
                        cv = CTt[0:64, ri, :, :].rearrange("p m (gl h) -> p (m gl) h", h=16)
                        for gg in range(2):
                            S("act", partial(A.copy, CQ[64 * gg:64 * gg + 64, :, ri, :], cv[:, gg::2, :]))

                    def cmul(k0, bsrc, out_re, out_im, neg_im=False, asrc=None, extra_w=()):
                        def SO(e, fn):
                            kb.op(e, [r_s, r_p], [r_s, r_p] + list(extra_w), fn)
                        a_r, a_i = asrc if asrc is not None else (PWr, PWi)
                        ar = a_r[:, :, k0:k0 + 8].unsqueeze(3).to_broadcast([128, 16, 8, 16])
                        ai = a_i[:, :, k0:k0 + 8].unsqueeze(3).to_broadcast([128, 16, 8, 16])
                        br_ = bsrc[:, :, 0, :].unsqueeze(2).to_broadcast([128, 16, 8, 16])
                        bi_ = bsrc[:, :, 1, :].unsqueeze(2).to_broadcast([128, 16, 8, 16])
                        S("dve", partial(V.tensor_tensor, tmA[:], ar, br_, op=ALU.mult))
                        S("pool", partial(G.tensor_tensor, tmB[:], ai, bi_, op=ALU.mult))
                        SO("dve", partial(V.tensor_tensor, out_re, tmA[:], tmB[:], op=ALU.subtract))
                        S("dve", partial(V.tensor_tensor, tmA[:], ar, bi_, op=ALU.mult))
                        S("pool", partial(G.tensor_tensor, tmB[:], ai, br_, op=ALU.mult))
                        if neg_im:
                            SO("dve", partial(V.scalar_tensor_tensor, out=out_im, in0=tmA[:], scalar=-1.0, in1=tmB[:], op0=ALU.mult, op1=ALU.subtract))
                        else:
                            SO("dve", partial(V.tensor_tensor, out_im, tmA[:], tmB[:], op=ALU.add))

                    kb.ckpt(3)
                    v4 = lambda ap: ap.rearrange("p st (s h) -> p st s h", h=16)
                    r_PwB = Reg()
                    cmul(0, Bb, v4(PwB[:, 0, :, :]), v4(PwB[:, 1, :, :]), extra_w=[r_PwB])
                    for ri in range(2):
                        for q4 in range(4):
                            for k in range(4):
                                kb.op("pe", [r_PwB, r_idb], [r_pT], partial(PE.transpose, pT[:, k * 128:(k + 1) * 128], PwB[:, ri, q4 * 4 + k, :], idb[:]))
                            kb.op("act", [r_pT], [r_WB], partial(A.copy, WB[:, q4 * 4:(q4 + 1) * 4, ri, :], pT[:, 0:512].rearrange("p (k n) -> p k n", k=4)))
                    cmul(8, Bb, v4(PpB[:, 0, :, :]), v4(PpB[:, 1, :, :]))
                    S("dve", partial(V.tensor_scalar, tB[:], th[:], 8.0, None, op0=ALU.mult))
                    S("dve", partial(V.tensor_tensor, angk[:], tB[:].unsqueeze(2).to_broadcast([128, 16, 24]), kab[:].unsqueeze(1).to_broadcast([128, 16, 24]), op=ALU.mult))
                    sincos(ckk[:].rearrange("p a b -> p (a b)"), skk[:].rearrange("p a b -> p (a b)"), angk[:].rearrange("p a b -> p (a b)"), 384)
                    S("dve", partial(V.tensor_copy, Bb[:, :, 0, :], ckk[:, :, 8:24]))
                    S("dve", partial(V.tensor_copy, Bb[:, :, 1, :], skk[:, :, 8:24]))
                    cmul(0, Bb, cosT[:].rearrange("p st (a b) -> p st a b", b=16), sinT[:].rearrange("p st (a b) -> p st a b", b=16), asrc=(ckk, skk))
                    S("dve", partial(V.tensor_scalar, tA[:], tB[:], float(TS), None, op0=ALU.mult))
                    sincos(cTs[:, 0, :], cTs[:, 1, :], tA[:], 16)
                    kb.op("act", [r_s, r_p], [r_par, r_tab, r_s], partial(A.activation, out=rcol[:], in_=tD[:], func=AF.Exp, scale=8.0))
                    cmul(16, CQ, v4(QT2[:, 0, :, :]), v4(QT2[:, 1, :, :]), neg_im=True)
                    kb.op("pool", [r_s, r_p], [r_Qt, r_s], partial(G.memset, Qt[:], 0.0))
                    for gg in range(2):
                        sl = slice(64 * gg, 64 * gg + 64)
                        kb.op("dve", [r_s, r_p, r_Qt], [r_Qt, r_s], partial(V.tensor_copy, Qt[sl, gg, 0, :, :], QT2[sl, 0, :, :]))
                        kb.op("act", [r_s, r_p, r_Qt], [r_Qt, r_s], partial(A.activation, out=Qt[sl, gg, 1, :, :], in_=QT2[sl, 1, :, :], func=AF.Copy, scale=-1.0))
                    PZ = [tmA[:].rearrange("p a b c -> p (a b c)").bitcast(BF16).rearrange("p (r s n) -> p r s n", r=2, s=16),
                          tmB[:].rearrange("p a b c -> p (a b c)").bitcast(BF16).rearrange("p (r s n) -> p r s n", r=2, s=16)]
                    S("pool", partial(G.memset, tmA[:], 0.0))
                    S("pool", partial(G.memset, tmB[:], 0.0))
                    for gg in range(2):
                        sl = slice(64 * gg, 64 * gg + 64)
                        S("dve", partial(V.tensor_copy, PZ[gg][sl, :, :, :], PpB[sl, :, :, :]))
                    kb.ckpt(4)
                    kb.ckpt(5)
                    S("dve", partial(V.tensor_copy, dsr8[:], dsr[:].unsqueeze(1).to_broadcast([32, 8, 16])))
                    kb.op("pe", [r_s, r_p, r_idf], [r_pb[6]], partial(PE.transpose, pb[6][:, 0:32], dsr8[:].rearrange("p s h -> p (s h)"), idf[0:32, 0:32]))
                    kb.op("dve", [r_pb[6]], [r_s], partial(V.tensor_copy, dskT[:], pb[6][:, 0:32]))
                    kb.ckpt(6)
                    for g4 in range(8):
                        bk, rbk = pb[g4 % 2], r_pb[g4 % 2]
                        for k in range(4):
                            g = 4 * g4 + k
                            st, gg = g // 2, g % 2
                            kb.op("pe", [r_s, r_Qt], [rbk], partial(PE.matmul, bk[:, k * 128:(k + 1) * 128], lhsT=PZ[gg][:, 0, st, :], rhs=QT2[:, 0, st, :], start=True, stop=False))
                            kb.op("pe", [r_s, r_Qt], [rbk], partial(PE.matmul, bk[:, k * 128:(k + 1) * 128], lhsT=PZ[gg][:, 1, st, :], rhs=QT2[:, 1, st, :], start=False, stop=True))
                        kb.ckpt(6.5)
                        kb.op("dve", [r_s, r_p, rbk], [r_s, r_p], partial(V.tensor_tensor, ToepF[:], bk[:].rearrange("p (k n) -> p k n", k=4), tmask[:].unsqueeze(1).to_broadcast([128, 4, 128]), op=ALU.mult))
                        kb.ckpt(6.8)
                        for k in range(4):
                            g = 4 * g4 + k
                            kb.op("dve", [r_s, r_idf], [r_Toep, r_s], partial(V.scalar_tensor_tensor, out=Toep[:, g, :], in0=idf[:], scalar=dskT[:, g:g + 1], in1=ToepF[:, k, :], op0=ALU.mult, op1=ALU.add))
                    kb.ckpt(7)
                    kb.barrier()
                kb.phase = 4
                Ug = T(LS, "Ug", [128, 32, 256], BF16); r_Ug = [Reg() for _ in range(32)]
                Uc = T(LS, "Uc", [128, 4, 512], BF16); r_Uc = Reg()
                Uc2 = T(LS, "Uc2", [128, 32, 128], BF16); r_Uc2 = Reg()
                Zs = [T(LS, "Zs%d" % i, [128, 2, 256]) for i in range(2)]; r_Zs = [Reg(), Reg()]
                pc = T(LS, "pc", [128, 2, 256]); psn = T(LS, "psn", [128, 2, 256]); r_pc = Reg(); r_psn = Reg()
                Zh0_ = T(LS, "Zh0", [128, 2, 256])
                vb = [T(LS, "vb%d" % i, [128, 2, 256]) for i in range(2)]; r_vb = [Reg(), Reg()]
                carry = T(LS, "carry", [128, 2, 16]); r_carry = [Reg() for _ in range(16)]
                ctmp = T(LS, "ctmp", [128, 2, 16]); r_ctmp = [Reg() for _ in range(16)]
                lastq = T(LS, "lastq", [128, 16, 3, 2], BF16); r_lastq = [Reg() for _ in range(16)]
                qcs = [T(LS, "qcs%d" % i, [128, 3, 2, 258], BF16) for i in range(2)]; r_qcs = [Reg(), Reg()]
                gt1 = T(LS, "gt1", [128, 256]); r_gt = Reg()
                ygel = [T(LS, "ygel0", [128, 8, 256], BF16)] * 2; r_ygel = [[Reg() for _ in range(8)]] * 2
                selt = [T(LS, "selt0", [128, 1152], BF16)] * 2; r_selt = [Reg()] * 2
                yT = T(LS, "yT", [128, 4, 2048], BF16); r_yT = [Reg() for _ in range(4)]
                sg = gt1; r_sg = r_gt
                yo = T(LS, "yo", [128, 256]); r_yo = Reg()
                yst = [T(LS, "yst0", [128, 4, 256], BF16)] * 2; r_yst = [Reg()] * 2
                sqo = T(LS, "sqo", [128, 4, 256], BF16); r_sqo = Reg()
                onesb = T(LS, "onesb", [128, 2], BF16); r_onesb = Reg()
                kb.op("dve", [], [r_onesb], partial(V.memset, onesb[:], 1.0))
                r_pTh = [Reg(), Reg()]
                kb.op("dve", [], r_carry, partial(V.memset, carry[:], 0.0))
                yT3f = yT[:, 3, :].bitcast(F32)
                Zh = [Zh0_, yT3f[:, 0:512].rearrange("p (a n) -> p a n", a=2)]; r_Zh = [Reg(), Reg()]
                pcs = [pc, yT3f[:, 512:1024].rearrange("p (a n) -> p a n", a=2)]; r_pcs = [r_pc, Reg()]
                kb.op("pool", [], [r_selt[0]], partial(G.memset, selt[0][:], 0.0))
                uview = uTM.rearrange("(c j) ch -> c j ch", j=8)
                nev = 0
                for pas in range(2 if ssm_stop >= 1 else 0):
                    if pas == 1 and ssm_stop < 3:
                        break
                    for cbl in range(2):
                        cb = 2 * pas + cbl
                        kb.ckpt(8.0)
                        for jh in range(2):
                            kb.dma(Uc[:], uview[cb * 128:(cb + 1) * 128, jh * 4:jh * 4 + 4, :], [r_uTM[i] for i in range(cb * 8, cb * 8 + 8)], [r_Uc])
                            kb.op("pool", [r_Uc], [r_Uc2], partial(G.tensor_copy, Uc2[:, 0:16, :].rearrange("p g (j h) -> p j g h", j=8)[:, jh * 4:jh * 4 + 4, :, :], Uc[:, :, 0:256].rearrange("p j (g h) -> p j g h", h=16)))
                            kb.op("pool", [r_Uc], [r_Uc2], partial(G.tensor_copy, Uc2[:, 16:32, :].rearrange("p g (j h) -> p j g h", j=8)[:, jh * 4:jh * 4 + 4, :, :], Uc[:, :, 256:512].rearrange("p j (g h) -> p j g h", h=16)))
                        kb.ckpt(8.1)
                        for g4 in range(8):
                            hf = g4 % 2
                            for k in range(4):
                                g = 4 * g4 + k
                                kb.op("pe", [r_Uc2, r_idb], [r_pb[4 + hf]], partial(PE.matmul, pb[4 + hf][:, k * 128:(k + 1) * 128], lhsT=Uc2[:, g, :], rhs=idb[:], start=True, stop=True))
                            src = pb[4 + hf][:].rearrange("p (k n) -> p k n", k=4)
                            dst = Ug[:, 4 * g4:4 * g4 + 4, cbl * 128:(cbl + 1) * 128]
                            wr = [r_Ug[4 * g4 + k] for k in range(4)]
                            kb.op("act", [r_pb[4 + hf]], wr, partial(A.copy, dst, src))
                            nev += 1
                    for st in range(16 if ssm_stop >= 2 else 0):
                        b = st % 2
                        pc, r_pc = pcs[b], r_pcs[b]
                        bk, rbk = pb[b], r_pb[b]
                        for ri in range(2):
                            for gg in range(2):
                                kb.op("pe", [r_WB, r_Ug[2 * st + gg]], [rbk], partial(PE.matmul, bk[64 * gg:64 * gg + 64, ri * 256:(ri + 1) * 256], lhsT=WB[:, st, ri, 64 * gg:64 * gg + 64], rhs=Ug[:, 2 * st + gg, :], start=True, stop=True))
                        kb.op("act", [rbk], [r_Zs[b]], partial(A.copy, Zs[b][:].rearrange("p a n -> p (a n)"), bk[:]))
                        z4 = lambda ap: ap.rearrange("p a (s n) -> p a s n", s=2)
                        cb_ = cosT[:, st, :].unsqueeze(1).unsqueeze(1).to_broadcast([128, 2, 2, TS])
                        sb_ = sinT[:, st, :].unsqueeze(1).unsqueeze(1).to_broadcast([128, 2, 2, TS])
                        kb.op("pool", [r_Zs[b], r_tab], [r_pc], partial(G.tensor_tensor, z4(pc[:]), z4(Zs[b][:]), cb_, op=ALU.mult))
                        kb.op("dve", [r_Zs[b], r_tab], [r_psn], partial(V.tensor_tensor, z4(psn[:]), z4(Zs[b][:]), sb_, op=ALU.mult))
                        kb.op("pool", [r_pc, r_psn], [r_Zh[b]], partial(G.tensor_tensor, Zh[b][:, 0, :], pc[:, 0, :], psn[:, 1, :], op=ALU.add))
                        kb.op("pool", [r_pc, r_psn], [r_Zh[b]], partial(G.tensor_tensor, Zh[b][:, 1, :], pc[:, 1, :], psn[:, 0, :], op=ALU.subtract))
                        for seg in range(2):
                            cs = slice(seg * TS, (seg + 1) * TS)
                            for ri in range(2):
                                kb.op("dve", [r_Zh[b], r_par, r_carry[st]], [r_vb[b]], partial(V.tensor_tensor_scan,
                                    vb[b][:, ri, cs], rcol[:, st:st + 1].to_broadcast([128, TS]), Zh[b][:, ri, cs], carry[:, ri, st:st + 1], op0=ALU.mult, op1=ALU.add))
                            lc = seg * TS + TS - 1
                            vlr = vb[b][:, 0, lc:lc + 1]
                            vli = vb[b][:, 1, lc:lc + 1]
                            cc_, cs_ = cTs[:, 0, st:st + 1], cTs[:, 1, st:st + 1]
                            kb.op("dve", [r_vb[b], r_par], [r_ctmp[st]], partial(V.tensor_scalar, ctmp[:, 0, st:st + 1], vli, cs_, None, op0=ALU.mult))
                            kb.op("dve", [r_vb[b], r_par], [r_ctmp[st]], partial(V.tensor_scalar, ctmp[:, 1, st:st + 1], vlr, cs_, None, op0=ALU.mult))
                            kb.op("dve", [r_vb[b], r_par, r_ctmp[st]], [r_carry[st]], partial(V.scalar_tensor_tensor, out=carry[:, 0, st:st + 1], in0=vlr, scalar=cc_, in1=ctmp[:, 0, st:st + 1], op0=ALU.mult, op1=ALU.subtract))
                            kb.op("dve", [r_vb[b], r_par, r_ctmp[st]], [r_carry[st]], partial(V.scalar_tensor_tensor, out=carry[:, 1, st:st + 1], in0=vli, scalar=cc_, in1=ctmp[:, 1, st:st + 1], op0=ALU.mult, op1=ALU.add))
                        if pas == 0:
                            kb.op("dve", [r_vb[b], r_tab], [r_lastq[st]], partial(V.tensor_scalar, lastq[:, st, 0, :], vb[b][:, :, 2 * TS - 1], cosT[:, st, TS - 1:TS], None, op0=ALU.mult))
                            kb.op("dve", [r_vb[b], r_tab], [r_lastq[st]], partial(V.tensor_scalar, lastq[:, st, 1, :], vb[b][:, :, 2 * TS - 1], sinT[:, st, TS - 1:TS], -1.0, op0=ALU.mult, op1=ALU.mult))
                            kb.op("dve", [r_vb[b], r_tab], [r_lastq[st]], partial(V.tensor_scalar, lastq[:, st, 2, :], vb[b][:, :, 2 * TS - 1], cosT[:, st, TS - 1:TS], -1.0, op0=ALU.mult, op1=ALU.mult))
                            continue
                        q, rq = qcs[b], r_qcs[b]
                        kb.op("act", [r_lastq[st]], [rq], partial(A.copy, q[:, :, :, 0], lastq[:, st, :, :]))
                        kb.op("dve", [r_vb[b], r_tab], [rq], partial(V.tensor_tensor, z4(q[:, 0, :, 1:257]), z4(vb[b][:]), cb_, op=ALU.mult))
                        kb.op("act", [r_vb[b], r_pc], [r_pc], partial(A.activation, out=pc[:], in_=vb[b][:], func=AF.Copy, scale=-1.0))
                        kb.op("dve", [r_pc, r_tab], [rq], partial(V.tensor_tensor, z4(q[:, 1, :, 1:257]), z4(pc[:]), sb_, op=ALU.mult))
                        kb.op("dve", [r_pc, r_tab], [rq], partial(V.tensor_tensor, q[:, 2, 1, 1:257].rearrange("p (s n) -> p s n", s=2), pc[:, 1, :].rearrange("p (s n) -> p s n", s=2),
                                                                   cosT[:, st, :].unsqueeze(1).to_broadcast([128, 2, TS]), op=ALU.mult))
                        m = st // 4
                        yg_, ryg = ygel[m % 2], r_ygel[m % 2]
                        for gg in range(2):
                            g = 2 * st + gg
                            gl = g % 8
                            yb_, ryb = pb[2 + gg], r_pb[2 + gg]
                            kb.op("pe", [r_Toep, r_Ug[g]], [ryb], partial(PE.matmul, yb_[:, 0:256], lhsT=Toep[:, g, :], rhs=Ug[:, g, :], start=True, stop=False))
                            terms = ((0, q[:, 0, 0, 0:256]), (0, q[:, 1, 1, 0:256]), (1, q[:, 1, 0, 0:256]), (1, q[:, 2, 1, 0:256]))
                            for ti, (kq, rhs_) in enumerate(terms):
                                kb.op("pe", [r_Qt, rq], [ryb], partial(PE.matmul, yb_[:, 0:256], lhsT=Qt[:, gg, kq, st, :], rhs=rhs_, start=False, stop=(ti == 3)))
                            kb.op("act", [ryb], [r_gt], partial(A.activation, out=gt1[:], in_=yb_[:, 0:256], func=AF.Square, scale=math.sqrt(0.044715)))
                            kb.op("dve", [r_gt, ryb], [r_gt], partial(V.scalar_tensor_tensor, out=gt1[:], in0=gt1[:], scalar=1.0, in1=yb_[:, 0:256], op0=ALU.add, op1=ALU.mult))
                            kb.op("act", [r_gt], [r_gt], partial(A.activation, out=gt1[:], in_=gt1[:], func=AF.Sigmoid, scale=GK))
                            kb.op("dve", [r_gt, ryb], [ryg[gl]], partial(V.tensor_tensor, yg_[:, gl, :], yb_[:, 0:256], gt1[:], op=ALU.mult))
                        if st % 4 == 3:
                            for t in range(8):
                                se, rse = selt[t % 2], r_selt[t % 2]
                                kb.op("pool", [r_idb], [rse], partial(G.tensor_copy, se[:].rearrange("p (g x) -> p g x", x=144)[:, :, 0:16], idb[:, 16 * t:16 * t + 16].unsqueeze(1).to_broadcast([128, 8, 16])))
                                pk, rpk = pb[4 + t % 2], r_pb[4 + t % 2]
                                for gl in range(8):
                                    kb.op("pe", [rse, ryg[gl]], [rpk], partial(PE.matmul, pk[:, 0:256], lhsT=se[:, gl * 128:(gl + 1) * 128], rhs=yg_[:, gl, :], start=(gl == 0), stop=(gl == 7)))
                                dst = yT[:, m, :].rearrange("p (c t) -> p c t", t=8)[:, :, t]
                                if t % 2 == 0:
                                    kb.op("act", [rpk], [r_yT[m]], partial(A.copy, dst, pk[:, 0:256]))
                                else:
                                    kb.op("dve", [rpk], [r_yT[m]], partial(V.tensor_copy, dst, pk[:, 0:256]))
                Ucf = Uc[:].rearrange("p a b -> p (a b)").bitcast(F32)
                sg2 = [sg, Ucf[:, 0:256]]; r_sg2 = [r_sg, Reg()]
                yo2 = [yo, Ucf[:, 256:512]]; r_yo2 = [r_yo, Reg()]
                yst2 = [yst[0], Uc2[:, 0:8, :].rearrange("p (m a) n -> p m (a n)", m=4)]; r_yst2 = [r_yst[0], Reg()]
                for oc in range(8 if ssm_stop >= 4 else 0):
                    ts_ = slice(oc * 256, (oc + 1) * 256)
                    ysb, rys = yst2[oc % 2], r_yst2[oc % 2]
                    for mo in range(4):
                        gb, rgb = pb[mo % 2], r_pb[mo % 2]
                        sg, r_sg, yo, r_yo = sg2[mo % 2], r_sg2[mo % 2], yo2[mo % 2], r_yo2[mo % 2]
                        for mi in range(4):
                            kb.op("pe", [r_wglu, r_yT[mi]], [rgb], partial(PE.matmul, gb[:, 0:256], lhsT=wglu[:, mi, mo * 128:(mo + 1) * 128], rhs=yT[:, mi, ts_], start=(mi == 0), stop=(mi == 3)))
                        kb.op("act", [rgb, r_dsk], [r_sg], partial(A.activation, out=sg[:, 0:256], in_=gb[:, 0:256], func=AF.Sigmoid, bias=bgl[:, mo:mo + 1], scale=1.0))
                        kb.op("dve", [r_sg, r_yT[mo]], [r_yo], partial(V.tensor_tensor, yo[:, 0:256], yT[:, mo, ts_], sg[:, 0:256], op=ALU.mult))
                        kb.op("act", [r_yo], [rys], partial(A.copy, ysb[:, mo, :], yo[:, 0:256]))
                        kb.op("pool", [r_yo], [r_sqo], partial(G.tensor_tensor, sqo[:, mo, :], yo[:, 0:256], yo[:, 0:256], op=ALU.mult))
                    kb.dma(ysT[:, :, oc * 256:(oc + 1) * 256], ysb[:, :, :], [rys], [r_ysT[oc]])
                    for half in range(2):
                        qi = oc * 2 + half
                        for mo in range(4):
                            kb.op("pe", [r_sqo, r_onesb], [r_pb[6]], partial(PE.matmul,
                                pb[6][:, (qi % 8):(qi % 8) + 1], lhsT=sqo[:, mo, half * 128:(half + 1) * 128], rhs=onesb[:, 0:1], start=(mo == 0), stop=(mo == 3)))
                        kb.op("dve", [r_pb[6]], [r_ss_ssm[qi]], partial(V.tensor_copy, ss_ssm[:, qi:qi + 1], pb[6][:, (qi % 8):(qi % 8) + 1]))
                if "ssm" in dbg:
                    kb.dma(dout("ysT", [128, 4, 2048], BF16), ysT, r_ysT, [])
                    kb.dma(dout("ss_ssm", [128, NOWN]), ss_ssm[:], r_ss_ssm, [])
                kb.barrier()

        def phase_attn(L):
            kb.phase = 5
            with ExitStack() as LA:
                Qa = [T(LA, "Qa%d" % i, [128, 512], BF16) for i in range(2)]
                r_Qq = [Reg(), Reg()]; r_Qs = [Reg(), Reg()]
                cb4 = [T(LA, "cb4%d" % i, [128, 4, 128], BF16) for i in range(2)]; r_cb4 = [Reg(), Reg()]
                Pc2 = [T(LA, "Pc%d" % i, [128, 2, 512], BF16) for i in range(2)]; r_Pc2 = [[Reg(), Reg()] for _ in range(2)]
                Pw2 = [T(LA, "Pw%d" % i, [128, 5, 512], BF16) for i in range(2)]; r_Pw2 = [[Reg() for _ in range(5)] for _ in range(2)]
                Ps2 = [T(LA, "Ps%d" % i, [128, 32, 512], BF16) for i in range(2)]; r_Ps2 = [[Reg() for _ in range(32)] for _ in range(2)]
                impt = T(LA, "impt", [128, 64]); impw = T(LA, "impw", [128, 64]); fm = T(LA, "fm", [128, 64])
                m8 = T(LA, "m8", [128, 8]); rdi = T(LA, "rdi", [128, 4]); r_imp = Reg(); r_fm = Reg()
                selp = T(LA, "selp", [128, 128], BF16); r_selp = Reg()
                rden = T(LA, "rden", [128, 4]); wgt = T(LA, "wgt", [128, 4]); otmp = T(LA, "otmp", [128, 256]); r_fin = Reg()
                oatt2 = [T(LA, "oatt%d" % i, [128, 512]) for i in range(2)]; r_oatt2 = [Reg(), Reg()]
                oatt, r_oatt = oatt2[0], r_oatt2[0]
                oab = T(LA, "oab", [128, 512], BF16); osq = T(LA, "osq", [128, 512]); r_oab = Reg(); r_osq = Reg()
                ost = [T(LA, "ost%d" % i, [128, 4, 128], BF16) for i in range(2)]; r_ost = [Reg(), Reg()]
                kb.op("dve", [], [r_selp], partial(V.memset, selp[:], 0.0))
                kb.op("act", r_gates, r_gates, partial(A.activation, out=gates[:], in_=gates[:], func=AF.Sigmoid))
                ogc_a = T(LA, "ogc_a", [128, 8]); r_ogc_a = Reg()
                wos_a = [T(LA, "wos_a%d" % i, [128, 1024]) for i in range(2)]; r_wos_a = [Reg(), Reg()]
                wob_a = [T(LA, "wob_a%d" % i, [128, 1024], BF16) for i in range(2)]; r_wob_a = [Reg(), Reg()]
                load_cols(ogc_a[:, 0:4], r_ogc_a, D["out_g_ssm"], 4)
                load_cols(ogc_a[:, 4:8], r_ogc_a, D["out_g_att"], 4)
                for c in range(8):
                    kb.dma(wos_a[c % 2][:], D["w_out"][c * 128:(c + 1) * 128, :], [], [r_wos_a[c % 2]])
                    kb.op("dve", [r_wos_a[c % 2], r_ogc_a], [r_wob_a[c % 2]], partial(V.tensor_scalar, wob_a[c % 2][:], wos_a[c % 2][:], ogc_a[:, c:c + 1], None, op0=ALU.mult))
                    kb.dma(woutd[:, c, :], wob_a[c % 2][:], [r_wob_a[c % 2]], [r_woutd[c]])
                oT = [T(LA, "oT%d" % i, [128, 512]) for i in range(2)]; r_oT = [Reg(), Reg()]

                def finalize(acc, racc, br, first, g, qi):
                    av = acc[:, 0:260].rearrange("p (h e) -> p h e", h=4)
                    kb.op("dve", [racc], [r_fin], partial(V.tensor_scalar, rden[:], av[:, :, 64], 1e-30, None, op0=ALU.add))
                    kb.op("dve", [r_fin], [r_fin], partial(V.reciprocal, rden[:], rden[:]))
                    gv = gates[:, qi, g * 12:(g + 1) * 12].rearrange("p (h b) -> p h b", b=3)[:, :, br]
                    kb.op("dve", [r_fin, r_gates[qi]], [r_fin], partial(V.tensor_tensor, wgt[:], rden[:], gv, op=ALU.mult))
                    wb_ = wgt[:].unsqueeze(2).to_broadcast([128, 4, 64])
                    ov_ = oatt[:, g * 256:(g + 1) * 256].rearrange("p (h d) -> p h d", h=4)
                    if first:
                        kb.op("dve", [racc, r_fin], [r_oatt], partial(V.tensor_tensor, ov_, av[:, :, 0:64], wb_, op=ALU.mult))
                    else:
                        tv = otmp[:].rearrange("p (h d) -> p h d", h=4)
                        kb.op("dve", [racc, r_fin], [r_fin], partial(V.tensor_tensor, tv, av[:, :, 0:64], wb_, op=ALU.mult))
                        kb.op("dve", [r_fin, r_oatt], [r_oatt], partial(V.tensor_tensor, ov_, ov_, tv, op=ALU.add))

                sbank = [0]

                def next_s():
                    i = (0, 1, 6)[sbank[0] % 3]
                    sbank[0] += 1
                    return pb[i], r_pb[i]

                for n in range(NT - NOWN, NT):
                    qi = n - (NT - NOWN)
                    oatt, r_oatt = oatt2[qi % 2], r_oatt2[qi % 2]
                    kb.prio = 1
                    kb.op("pool", [r_fm], [r_fm], partial(G.memset, fm[:], 0.0))
                    kb.op("pool", [r_fm], [r_fm], partial(G.memset, fm[0:64, 2 * n - 1:2 * n + 1], 1e9))
                    kb.op("pool", [r_fm], [r_fm], partial(G.memset, fm[64:128, 2 * n:2 * n + 2], 1e9))
                    kb.op("dve", [r_fm, r_f0], [r_fm], partial(V.tensor_tensor, fm[:], fm[:], f0[:], op=ALU.max))
                    for j in range(2):
                        tau = float(128 * n - 2048 * j)
                        kb.op("dve", [r_dmat], [r_cb4[j]], partial(V.tensor_scalar,
                            cb4[j][:], dmat[:].unsqueeze(1).to_broadcast([128, 4, 128]), tau, -BIG, op0=ALU.is_gt, op1=ALU.mult))
                    for g in range(2):
                        qa = Qa[g]
                        kb.prio = 1
                        Pc, r_Pc, Pw, r_Pw, Ps, r_Ps = Pc2[g], r_Pc2[g], Pw2[g], r_Pw2[g], Ps2[g], r_Ps2[g]
                        for h in range(4):
                            kb.op("pe", [r_Q[qi], r_idb], [r_pT], partial(PE.transpose,
                                pT[0:64, h * 128:(h + 1) * 128], Qraw[:, qi, (4 * g + h) * 64:(4 * g + h + 1) * 64], idb[:]))
                        kb.op("act", [r_pT, r_gcols], [r_Qq[g]], partial(A.activation, out=qa[0:64, :], in_=pT[0:64, 0:512], func=AF.Copy, scale=gcols[0:64, 0:1]))
                        for j in range(2):
                            sb_, rsb = pb[3], r_pb[3]
                            kb.op("pe", [r_cmp, r_Qq[g]], [rsb], partial(PE.matmul, sb_[:], lhsT=KcT[g][:, j * 128:(j + 1) * 128], rhs=qa[0:64, :], start=True, stop=False))
                            kb.op("pe", [r_idb, r_cb4[j]], [rsb], partial(PE.matmul, sb_[:], lhsT=idb[:], rhs=cb4[j][:].rearrange("p h q -> p (h q)"), start=False, stop=True))
                            kb.op("act", [rsb, r_kbcmp], [r_Pc[j]], partial(A.activation, out=Pc[:, j, :], in_=sb_[:], func=AF.Exp, bias=kbcmp[:, j:j + 1], scale=1.0))
                        for h in range(4):
                            for j in range(2):
                                kb.op("pe", [r_Pc[j], r_cmp], [r_pb[2]], partial(PE.matmul,
                                    pb[2][:, h * 65:(h + 1) * 65], lhsT=Pc[:, j, h * 128:(h + 1) * 128], rhs=Vc[:, j, g, :], start=(j == 0), stop=(j == 1)))
                        for h in range(4):
                            for j in range(2):
                                kb.op("pe", [r_Pc[j], r_ovb], [r_pb[3]], partial(PE.matmul,
                                    pb[3][:, h * 65:(h + 1) * 65], lhsT=Pc[:, j, h * 128:(h + 1) * 128], rhs=ovb[:, j, :], start=(j == 0), stop=(j == 1)))
                        iv = pb[3][:, 0:260].rearrange("p (h e) -> p h e", h=4)
                        kb.op("dve", [r_pb[3]], [r_imp], partial(V.tensor_scalar, rdi[:], iv[:, :, 64], 1e-30, None, op0=ALU.add))
                        kb.op("dve", [r_imp], [r_imp], partial(V.reciprocal, rdi[:], rdi[:]))
                        kb.op("dve", [r_pb[3], r_imp], [r_imp], partial(V.tensor_scalar, impt[:], iv[:, 0, 0:64], rdi[:, 0:1], None, op0=ALU.mult))
                        for h in range(1, 4):
                            kb.op("dve", [r_pb[3], r_imp], [r_imp], partial(V.scalar_tensor_tensor,
                                out=impt[:], in0=iv[:, h, 0:64], scalar=rdi[:, h:h + 1], in1=impt[:], op0=ALU.mult, op1=ALU.add))
                        kb.op("dve", [r_imp, r_fm], [r_imp], partial(V.tensor_tensor, impt[:], impt[:], fm[:], op=ALU.max))
                        kb.op("dve", [r_imp], [r_imp], partial(V.max, out=m8[:], in_=impt[:]))
                        kb.op("dve", [r_imp], [r_imp], partial(V.match_replace, out=impw[:], in_to_replace=m8[:], in_values=impt[:], imm_value=-1e30))
                        kb.op("dve", [r_imp], [r_imp], partial(V.max, out=m8[:], in_=impw[:]))
                        kb.op("dve", [r_imp], [r_selp], partial(V.tensor_scalar, selp[:, 64:128], impt[:], m8[:, 7:8], -1.0, op0=ALU.is_ge, op1=ALU.add))
                        kb.op("pe", [r_selp, r_idb], [r_pT], partial(PE.transpose, pT[:, 512:640], selp[:], idb[:]))
                        kb.op("act", [r_pT], [r_Qs[g]], partial(A.copy,
                            qa[64:128, :].rearrange("p (h q) -> p h q", h=4), pT[64:128, 512:640].unsqueeze(1).to_broadcast([64, 4, 128])))
                        finalize(pb[2], r_pb[2], 0, True, g, qi)
                        kb.prio = 0
                        for kt in range(n + 1):
                            sb_, rsb = next_s()
                            kb.op("pe", [r_K[kt], r_eb, r_Qq[g], r_Qs[g]], [rsb], partial(PE.matmul,
                                sb_[:], lhsT=KsT[g][:, kt * 128:(kt + 1) * 128], rhs=qa[:, :], start=True, stop=(kt != n)))
                            if kt == n:
                                kb.op("pe", [r_idb, r_tlb4], [rsb], partial(PE.matmul, sb_[:], lhsT=idb[:], rhs=tlb4[:].rearrange("p h q -> p (h q)"), start=False, stop=True))
                            kb.op("act", [rsb, r_kbtok], [r_Ps[kt]], partial(A.activation, out=Ps[:, kt, :], in_=sb_[:], func=AF.Exp, bias=kbtok[:, kt:kt + 1], scale=1.0))
                        for kt in range(n + 1):
                            kb.op("pe", [r_Ps[kt], r_K[kt]], [r_pb[4]], partial(PE.matmul, pb[4][0:65, :], lhsT=Vs[:, kt, g, :], rhs=Ps[:, kt, :], start=(kt == 0), stop=(kt == n)))
                        kb.op("dve", [r_pb[4]], [r_oT[0]], partial(V.tensor_copy, oT[0][0:65, :], pb[4][0:65, :]))
                        for h in range(4):
                            kb.op("pe", [r_oT[0], r_idf], [r_pb[4]], partial(PE.transpose, pb[4][:, h * 65:(h + 1) * 65], oT[0][0:65, h * 128:(h + 1) * 128], idf[0:65, 0:65]))
                        kts = list(range(n - 4, n + 1))
                        for wi, kt in enumerate(kts):
                            sb_, rsb = next_s()
                            plain = (kt != n and kt != n - 4)
                            kb.op("pe", [r_K[kt], r_Qq[g]], [rsb], partial(PE.matmul,
                                sb_[:], lhsT=KwT[g][:, kt * 128:(kt + 1) * 128], rhs=qa[0:64, :], start=True, stop=plain))
                            if not plain:
                                bt, rbt = (tlb4, r_tlb4) if kt == n else (sub4, r_sub4)
                                kb.op("pe", [r_idb, rbt], [rsb], partial(PE.matmul, sb_[:], lhsT=idb[:], rhs=bt[:].rearrange("p h q -> p (h q)"), start=False, stop=True))
                            kb.op("act", [rsb, r_kbtok], [r_Pw[wi]], partial(A.activation, out=Pw[:, wi, :], in_=sb_[:], func=AF.Exp, bias=kbtok[:, kt:kt + 1], scale=1.0))
                        for wi, kt in enumerate(kts):
                            kb.op("pe", [r_Pw[wi], r_K[kt]], [r_pb[5]], partial(PE.matmul, pb[5][0:65, :], lhsT=Vw[:, kt, g, :], rhs=Pw[:, wi, :], start=(wi == 0), stop=(wi == 4)))
                        kb.op("dve", [r_pb[5]], [r_oT[1]], partial(V.tensor_copy, oT[1][0:65, :], pb[5][0:65, :]))
                        for h in range(4):
                            kb.op("pe", [r_oT[1], r_idf], [r_pb[5]], partial(PE.transpose, pb[5][:, h * 65:(h + 1) * 65], oT[1][0:65, h * 128:(h + 1) * 128], idf[0:65, 0:65]))
                        finalize(pb[4], r_pb[4], 1, False, g, qi)
                        finalize(pb[5], r_pb[5], 2, False, g, qi)
                    kb.op("dve", [r_oatt], [r_osq], partial(V.tensor_tensor, osq[:], oatt[:], oatt[:], op=ALU.mult))
                    kb.op("dve", [r_osq], [r_ss_att[qi]], partial(V.tensor_reduce, out=ss_att[:, qi:qi + 1], in_=osq[:], axis=AX.X, op=ALU.add))
                    for m in range(4):
                        kb.op("pe", [r_oatt, r_idf], [r_pb[5]], partial(PE.transpose, pb[5][:, m * 128:(m + 1) * 128], oatt[:, m * 128:(m + 1) * 128], idf[:]))
                    osb, ros = ost[qi % 2], r_ost[qi % 2]
                    kb.op("dve", [r_pb[5]], [ros], partial(V.tensor_copy, osb[:], pb[5][:].rearrange("p (m n) -> p m n", m=4)))
                    kb.dma(oattT[:, :, qi * 128:(qi + 1) * 128], osb[:], [ros], [r_oattT[qi]])
                    if "att" in dbg and qi == NOWN - 1:
                        pass
                if "att" in dbg:
                    kb.dma(dout("oattT", [128, 4, 2048], BF16), oattT, r_oattT, [])
                    kb.dma(dout("ss_att", [128, NOWN]), ss_att[:], r_ss_att, [])
                kb.barrier()

        def phase_post(L):
            kb.phase = 6
            with ExitStack() as LO:
                wout = T(LO, "wout", [128, 8, 1024], BF16); r_wout = Reg()
                wr = T(LO, "wr", [128, 8, 36]); r_wr = Reg()
                wrs = T(LO, "wrs", [128, 8, 36]); r_wrs = Reg()
                g2c = T(LO, "g2c", [128, 8]); r_g2c = Reg()
                brt = T(LO, "brt", [128, 36]); r_brt = Reg()
                ysb = [T(LO, "ysb%d" % i, [128, 4, 128], BF16) for i in range(2)]; r_ysb = [Reg(), Reg()]
                oab2 = [T(LO, "oab2%d" % i, [128, 4, 128], BF16) for i in range(2)]; r_oab2 = [Reg(), Reg()]
                xt = [T(LO, "xt%d" % i, [128, 1024]) for i in range(2)]; r_xt = [Reg(), Reg()]
                x1 = [T(LO, "x1%d" % i, [128, 1024]) for i in range(2)]; r_x1 = [Reg(), Reg()]
                rs2 = T(LO, "rs2", [128, 2]); r_rs2 = Reg()
                junk2s = [T(LO, "junk2%d" % i, [128, 1024]) for i in range(2)]; r_junk2s = [Reg(), Reg()]
                ssn = T(LO, "ssn", [128, 1]); r_ssn = Reg()
                t32s = [T(LO, "t32%d" % i, [128, 1024]) for i in range(2)]; r_t32s = [Reg(), Reg()]
                tTfs = [T(LO, "tTf%d" % i, [128, 8, 128]) for i in range(2)]; r_tTfs = [Reg(), Reg()]
                tTb = [T(LO, "tTb%d" % i, [128, 8, 128], BF16) for i in range(2)]; r_tTb = [Reg(), Reg()]
                lg = T(LO, "lg", [128, 36]); r_lg = Reg()
                rt = T(LO, "rt", [128, 64]); r_rt = Reg()
                m8r = T(LO, "m8r", [128, 8])
                load_cols(g2c[:], r_g2c, D["norm2_g"], 8)
                for c2 in range(2):
                    kb.dma(wout[:, 4 * c2:4 * c2 + 4, :], woutd[:, 4 * c2:4 * c2 + 4, :], r_woutd[4 * c2:4 * c2 + 4], [r_wout])
                load_cols(MB["g2m"][:], MB["r_g2m"], D["norm2_g"], 8)
                if stage >= 6:
                    moe_load_expert(0)
                kb.dma(wrs[:, :, 0:4], D["w_grp"].rearrange("(c p) n -> p c n", p=128), [], [r_wrs])
                kb.dma(wrs[:, :, 4:36], D["w_exp"].rearrange("(c p) n -> p c n", p=128), [], [r_wrs])
                kb.op("dve", [r_wrs, r_g2c], [r_wr], partial(V.tensor_tensor, wr[:], wrs[:], g2c[:].unsqueeze(2).to_broadcast([128, 8, 36]), op=ALU.mult))
                kb.dma(brt[:, 0:4], D["b_grp"].rearrange("(o n) -> o n", o=1).partition_broadcast(128), [], [r_brt])
                kb.dma(brt[:, 4:36], D["b_exp"].rearrange("(o n) -> o n", o=1).partition_broadcast(128), [], [r_brt])
                for qi in range(NOWN if stage >= 5.15 else 0):
                    b = qi % 2
                    junk2, r_junk2, t32, r_t32, tTf, r_tTf = junk2s[b], r_junk2s[b], t32s[b], r_t32s[b], tTfs[b], r_tTfs[b]
                    kb.prio = 1
                    kb.dma(ysb[b][:], ysT[:, :, qi * 128:(qi + 1) * 128], [r_ysT[qi // 2]], [r_ysb[b]])
                    kb.dma(oab2[b][:], oattT[:, :, qi * 128:(qi + 1) * 128], [r_oattT[qi]], [r_oab2[b]])
                    kb.dma(xt[b][:], xown[qi * 128:(qi + 1) * 128, :], [], [r_xt[b]])
                    for nb in range(2):
                        for m in range(4):
                            kb.op("pe", [r_ysb[b], r_wout], [r_pb[nb]], partial(PE.matmul, pb[nb][:], lhsT=ysb[b][:, m, :], rhs=wout[:, m, nb * 512:(nb + 1) * 512], start=(m == 0), stop=(m == 3)))
                        for m in range(4):
                            kb.op("pe", [r_oab2[b], r_wout], [r_pb[2 + nb]], partial(PE.matmul, pb[2 + nb][:], lhsT=oab2[b][:, m, :], rhs=wout[:, 4 + m, nb * 512:(nb + 1) * 512], start=(m == 0), stop=(m == 3)))
                    kb.op("dve", [r_ss_ssm[qi]], [r_rs2], partial(V.tensor_copy, rs2[:, 0:1], ss_ssm[:, qi:qi + 1]))
                    kb.op("dve", [r_ss_att[qi]], [r_rs2], partial(V.tensor_copy, rs2[:, 1:2], ss_att[:, qi:qi + 1]))
                    rstd_lnexp(rs2[:], 512, [r_rs2])
                    for nb in range(2):
                        sl = slice(nb * 512, (nb + 1) * 512)
                        kb.op("dve", [r_pb[nb], r_rs2, r_xt[b]], [r_x1[b]], partial(V.scalar_tensor_tensor,
                            out=x1[b][:, sl], in0=pb[nb][:], scalar=rs2[:, 0:1], in1=xt[b][:, sl], op0=ALU.mult, op1=ALU.add))
                        kb.op("dve", [r_pb[2 + nb], r_rs2, r_x1[b]], [r_x1[b]], partial(V.scalar_tensor_tensor,
                            out=x1[b][:, sl], in0=pb[2 + nb][:], scalar=rs2[:, 1:2], in1=x1[b][:, sl], op0=ALU.mult, op1=ALU.add))
                    kb.dma(x1d[qi * 128:(qi + 1) * 128, :], x1[b][:], [r_x1[b]], [r_x1d[qi]])
                    if stage < 5.25:
                        continue
                    kb.op("act", [r_x1[b]], [r_junk2], partial(A.activation, out=junk2[:], in_=x1[b][:], func=AF.Square))
                    kb.op("dve", [r_junk2], [r_ssn], partial(V.tensor_reduce, out=ssn[:], in_=junk2[:], axis=AX.X, op=ALU.add))
                    rstd_lnexp(ssn[:], 1024, [r_ssn])
                    kb.op("dve", [r_x1[b], r_ssn], [r_t32], partial(V.tensor_scalar, t32[:], x1[b][:], ssn[:, 0:1], None, op0=ALU.mult))
                    if stage < 5.26:
                        continue
                    for c in range(8):
                        bk = 4 + c // 4
                        kb.op("pe", [r_t32, r_idf], [r_pb[bk]], partial(PE.transpose, pb[bk][:, (c % 4) * 128:(c % 4 + 1) * 128], t32[:, c * 128:(c + 1) * 128], idf[:]))
                    if stage < 5.27:
                        continue
                    for hh in range(2):
                        kb.op("act", [r_pb[4 + hh]], [r_tTf], partial(A.copy, tTf[:, hh * 4:(hh + 1) * 4, :], pb[4 + hh][:].rearrange("p (c n) -> p c n", c=4)))
                        kb.op("dve", [r_tTf], [MB["r_tTq"][qi]], partial(V.tensor_copy, MB["tT"][:, hh * 4:(hh + 1) * 4, qi * 128:(qi + 1) * 128], tTf[:, hh * 4:(hh + 1) * 4, :]))
                    if stage < 5.28:
                        continue
                    if stage < 5.35:
                        continue
                    for c in range(8):
                        kb.op("pe", [r_tTf, r_wr], [r_pb[6]], partial(PE.matmul, pb[6][:, 0:36], lhsT=tTf[:, c, :], rhs=wr[:, c, :], start=(c == 0), stop=(c == 7)))
                    kb.op("dve", [r_pb[6], r_brt], [r_lg], partial(V.tensor_tensor, lg[:], pb[6][:, 0:36], brt[:], op=ALU.add))
                    if stage < 5.45:
                        continue
                    kb.prio = 0
                    def R(e, fn, extra_r=(), extra_w=()):
                        kb.op(e, [r_rt, r_lg] + list(extra_r), [r_rt] + list(extra_w), fn)
                    gl = lg[:, 0:4]
                    el = lg[:, 4:36].rearrange("p (g j) -> p g j", g=4)
                    gmax, ngmax, goh, gex, gsum = rt[:, 0:1], rt[:, 1:2], rt[:, 2:6], rt[:, 6:10], rt[:, 10:11]
                    els, msk, ee, wsum, nv1 = rt[:, 16:24], rt[:, 24:32], rt[:, 32:40], rt[:, 11:12], rt[:, 12:13]
                    R("dve", partial(V.tensor_reduce, out=gmax, in_=gl, axis=AX.X, op=ALU.max))
                    R("dve", partial(V.tensor_scalar, ngmax, gmax, -1.0, None, op0=ALU.mult))
                    R("dve", partial(V.tensor_scalar, goh, gl, gmax, None, op0=ALU.is_ge))
                    R("act", partial(A.activation, out=gex, in_=gl, func=AF.Exp, bias=ngmax, scale=1.0))
                    R("dve", partial(V.tensor_reduce, out=gsum, in_=gex, axis=AX.X, op=ALU.add))
                    R("dve", partial(V.reciprocal, gsum, gsum))
                    R("dve", partial(V.tensor_scalar, els, el[:, 0, :], goh[:, 0:1], None, op0=ALU.mult))
                    for g in range(1, 4):
                        R("dve", partial(V.scalar_tensor_tensor, out=els, in0=el[:, g, :], scalar=goh[:, g:g + 1], in1=els, op0=ALU.mult, op1=ALU.add))
                    R("dve", partial(V.max, out=m8r[:], in_=els))
                    R("dve", partial(V.tensor_scalar, msk, els, m8r[:, 1:2], None, op0=ALU.is_ge))
                    R("dve", partial(V.tensor_scalar, nv1, m8r[:, 0:1], -1.0, None, op0=ALU.mult))
                    R("act", partial(A.activation, out=ee, in_=els, func=AF.Exp, bias=nv1, scale=1.0))
                    R("dve", partial(V.tensor_tensor, ee, ee, msk, op=ALU.mult))
                    R("dve", partial(V.tensor_reduce, out=wsum, in_=ee, axis=AX.X, op=ALU.add))
                    R("dve", partial(V.reciprocal, wsum, wsum))
                    R("dve", partial(V.tensor_tensor, wsum, wsum, gsum, op=ALU.mult))
                    R("dve", partial(V.tensor_scalar, ee, ee, wsum, None, op0=ALU.mult))
                    for g in range(4):
                        R("dve", partial(V.tensor_scalar, comb[:, qi, g * 8:(g + 1) * 8], ee, goh[:, g:g + 1], None, op0=ALU.mult), extra_w=[r_comb[qi]])
                if "post" in dbg:
                    dx1 = dout("x1", [2048, 1024])
                    for qi in range(NOWN):
                        kb.dma(dx1[qi * 128:(qi + 1) * 128, :], x1d[qi * 128:(qi + 1) * 128, :], [r_x1d[qi]], [])
                    kb.dma(dout("comb", [128, NOWN, 32]), comb[:], r_comb, [])
                kb.barrier()

        def moe_load_expert(e):
            s2 = e % 2
            g2b = MB["g2m"][:].unsqueeze(2).to_broadcast([128, 8, 256])
            kb.dma(MB["wgs_"][:], D["w_gate"][e].rearrange("(c p) f -> p c f", p=128), [], [MB["r_wgs"]])
            kb.dma(MB["wus_"][:], D["w_up"][e].rearrange("(c p) f -> p c f", p=128), [], [MB["r_wus"]])
            kb.dma(MB["wds_"][:], D["w_down"][e].rearrange("(c p) d -> p c d", p=128), [], [MB["r_wds"]])
            kb.op("pool", [MB["r_wgs"], MB["r_g2m"]], [MB["r_wb"][s2]], partial(G.tensor_tensor, MB["wgb"][s2][:], MB["wgs_"][:], g2b, op=ALU.mult))
            kb.op("pool", [MB["r_wus"], MB["r_g2m"]], [MB["r_wb"][s2]], partial(G.tensor_tensor, MB["wub"][s2][:], MB["wus_"][:], g2b, op=ALU.mult))
            kb.op("act", [MB["r_wds"]], [MB["r_wb"][s2]], partial(A.copy, MB["wdb"][s2][:], MB["wds_"][:]))

        def phase_moe(L):
            kb.phase = 7
            acc = T(L, "acc", [128, NOWN, 1024]); r_acc = [Reg() for _ in range(NOWN)]
            tT, r_tTq = MB["tT"], MB["r_tTq"]
            wgb, wub, wdb, r_wb = MB["wgb"], MB["wub"], MB["wdb"], MB["r_wb"]
            abf = T(L, "abf", [128, 2, 2048], BF16); r_abf = [Reg() for _ in range(4)]
            sil = [T(L, "sil%d" % i, [128, 512]) for i in range(2)]; r_sil = [Reg(), Reg()]
            for qi in range(NOWN):
                kb.dma(acc[:, qi, :], x1d[qi * 128:(qi + 1) * 128, :], [r_x1d[qi]], [r_acc[qi]])
            k = 0
            for e in range(32):
                s2 = e % 2
                if e > 0:
                    moe_load_expert(e)
                for nt in range(4):
                    for f in range(2):
                        bg, rbg = pb[k % 2], r_pb[k % 2]
                        bu, rbu = pb[2 + k % 2], r_pb[2 + k % 2]
                        sl_, rsl = sil[k % 2], r_sil[k % 2]
                        k += 1
                        for c in range(8):
                            kb.op("pe", [r_wb[s2]] + r_tTq[4 * nt:4 * nt + 4], [rbg], partial(PE.matmul, bg[:], lhsT=wgb[s2][:, c, f * 128:(f + 1) * 128], rhs=tT[:, c, nt * 512:(nt + 1) * 512], start=(c == 0), stop=(c == 7)))
                        for c in range(8):
                            kb.op("pe", [r_wb[s2]] + r_tTq[4 * nt:4 * nt + 4], [rbu], partial(PE.matmul, bu[:], lhsT=wub[s2][:, c, f * 128:(f + 1) * 128], rhs=tT[:, c, nt * 512:(nt + 1) * 512], start=(c == 0), stop=(c == 7)))
                        kb.op("act", [rbg], [rsl], partial(A.activation, out=sl_[:], in_=bg[:], func=AF.Silu))
                        kb.op("dve", [rsl, rbu], [r_abf[nt]], partial(V.tensor_tensor, abf[:, f, nt * 512:(nt + 1) * 512], sl_[:], bu[:], op=ALU.mult))
                for tt in range(NOWN):
                    for nb in range(2):
                        bd, rbd = pb[4 + (tt * 2 + nb) % 3], r_pb[4 + (tt * 2 + nb) % 3]
                        for f in range(2):
                            kb.op("pe", [r_abf[tt // 4], r_wb[s2]], [rbd], partial(PE.matmul, bd[:], lhsT=abf[:, f, tt * 128:(tt + 1) * 128], rhs=wdb[s2][:, f, nb * 512:(nb + 1) * 512], start=(f == 0), stop=(f == 1)))
                        kb.op("dve", [rbd, r_comb[tt], r_acc[tt]], [r_acc[tt]], partial(V.scalar_tensor_tensor,
                            out=acc[:, tt, nb * 512:(nb + 1) * 512], in0=bd[:], scalar=comb[:, tt, e:e + 1], in1=acc[:, tt, nb * 512:(nb + 1) * 512], op0=ALU.mult, op1=ALU.add))
            for qi in range(NOWN):
                kb.dma(out[qi * 128:(qi + 1) * 128, :], acc[:, qi, :], [r_acc[qi]], [])

        with ExitStack() as L1:
            ysT = ysTd; r_ysT = [Reg() for _ in range(8)]
            oattT = oattTd; r_oattT = [Reg() for _ in range(NOWN)]
            with ExitStack() as L2:
                KsT = [T(L2, "KsT%d" % g, [128, 4096], BF16) for g in range(2)]
                KwT = [T(L2, "KwT%d" % g, [64, 4096], BF16) for g in range(2)]
                Vs = T(L2, "Vs", [128, NT, 2, 65], BF16)
                Vw = T(L2, "Vw", [128, NT, 2, 65], BF16)
                r_K = [Reg() for _ in range(NT)]
                KcT = [T(L2, "KcT%d" % g, [64, 256], BF16) for g in range(2)]
                Vc = T(L2, "Vc", [128, 2, 2, 65], BF16)
                r_cmp = Reg()
                Qraw = T(L2, "Qraw", [128, NOWN, 512], BF16); r_Q = [Reg() for _ in range(NOWN)]
                gates = T(L2, "gates", [128, NOWN, 24]); r_gates = [Reg() for _ in range(NOWN)]
                r_eb = Reg()
                kb.op("pool", [], [r_K[i] for i in range(NT)], partial(G.memset, Vs[:], 1.0))
                kb.op("pool", [], [r_K[i] for i in range(NT)], partial(G.memset, Vw[:], 1.0))
                kb.op("pool", [], [r_cmp], partial(G.memset, Vc[:], 1.0))
                for g in range(2):
                    kb.op("pool", [], [r_cmp], partial(G.memset, KcT[g][:], 0.0))
                with ExitStack() as L3:
                    r_uTM = [Reg() for _ in range(NT)]
                    with ExitStack() as L4:
                        wtm = T(L4, "wtm", [128, 8, 1048], BF16)
                        wfu = T(L4, "wfu", [128, 8, 512], BF16)
                        wfc = T(L4, "wfc", [128, 8, 2, 128], BF16)
                        r_w4 = [Reg() for _ in range(4)]
                        g1c = T(L4, "g1c", [128, 8]); r_g1c = Reg()
                        kcvT = T(L4, "kcvT", [128, 2, 16, 256], BF16); r_kcv = [Reg() for _ in range(8)]
                        load_cols(g1c[:], r_g1c, D["norm1_g"], 8)
                        if "uT" in dbg:
                            kb.dma(dout("g1c", [128, 8]), g1c[:], [r_g1c], [])
                            kb.dma(dout("gcols", [128, 4]), gcols[:], [r_gcols], [])
                        with ExitStack() as L5:
                            wst = [T(L5, "wst%d" % i, [128, 1816]) for i in range(2)]
                            r_wst = [Reg(), Reg()]
                            for q4 in range(4):
                                ebs = wst[q4 % 2]; r_ebs = r_wst[q4 % 2]
                                kb.dma(ebs[0:64, 0:1024], D["c_eband"][:, q4 * 1024:(q4 + 1) * 1024], [], [r_ebs])
                                for g in range(2):
                                    kb.op("pool", [r_ebs], [r_eb], partial(G.tensor_copy, KsT[g][64:128, q4 * 1024:(q4 + 1) * 1024], ebs[0:64, 0:1024]))
                            for c in range(8):
                                ws = wst[c % 2]; rw = r_wst[c % 2]
                                kb.dma(ws[:], D["w_in"][c * 128:(c + 1) * 128, :], [], [rw])
                                sc = g1c[:, c:c + 1]
                                e1, e2 = ("dve", V), ("pool", G)
                                kb.op("dve", [rw, r_g1c], [r_w4[0]], partial(V.tensor_scalar, wfu[:, c, :], ws[:, 0:512], sc, None, op0=ALU.mult))
                                kb.op("act", [rw, r_g1c], [r_w4[1]], partial(A.activation, out=wtm[:, c, 0:512], in_=ws[:, 512:1024], func=AF.Copy, scale=sc))
                                kb.op("dve", [rw, r_g1c], [r_w4[2]], partial(V.tensor_scalar, wtm[:, c, 512:1048], ws[:, 1280:1816], sc, None, op0=ALU.mult))
                                kb.op("dve", [rw, r_g1c], [r_w4[3]], partial(V.tensor_scalar,
                                    wfc[:, c, :, :].rearrange("p g (k d) -> p g k d", k=2),
                                    ws[:, 1024:1280].rearrange("p (k g d) -> p g k d", k=2, g=2), sc, None, op0=ALU.mult))
                            kb.barrier()
                        kb.phase = 1
                        xs = [T(L4, "xs%d" % i, [128, 1024]) for i in range(2)]; r_xs = [Reg(), Reg()]
                        utm = [T(L4, "utm%d" % i, [128, 512], BF16) for i in range(2)]; r_utm = [Reg(), Reg()]
                        junk = T(L4, "junk", [128, 1024]); r_junk = Reg()
                        ssx = T(L4, "ssx", [128, NT]); r_ssx = [Reg() for _ in range(NT)]
                        xn = [T(L4, "xn%d" % i, [128, 1024], BF16) for i in range(2)]; r_xn = [Reg(), Reg()]
                        hT = [T(L4, "hT%d" % i, [128, 8, 512], BF16) for i in range(2)]
                        r_hT = [[Reg() for _ in range(4)] for _ in range(2)]
                        sq = [T(L4, "sq%d" % i, [128, 512]) for i in range(2)]; r_sq = [Reg(), Reg()]
                        ss8 = [T(L4, "ss8%d" % i, [128, 8]) for i in range(2)]; r_ss8 = [Reg(), Reg()]
                        kn = [T(L4, "kn%d" % i, [128, 256], BF16) for i in range(2)]; r_kn = [Reg(), Reg()]
                        sqq = T(L4, "sqq", [128, 512]); r_sqq = Reg()
                        ssq = T(L4, "ssq", [128, 8]); r_ssq = Reg()

                        kb.dma(xs[0][:], xp[0:128, :], [], [r_xs[0]])
                        for sup in range(8):
                            hb = hT[sup % 2]; rhb = r_hT[sup % 2]
                            for t in range(4):
                                i = sup * 4 + t
                                xb = xs[i % 2]; rxb = r_xs[i % 2]
                                if i + 1 < NT:
                                    kb.dma(xs[(i + 1) % 2][:], xp[(i + 1) * 128:(i + 2) * 128, :], [], [r_xs[(i + 1) % 2]])
                                own = i >= NT - NOWN
                                qi = i - (NT - NOWN)
                                xnb = xn[i % 2]; rxn = r_xn[i % 2]
                                kb.prio = 1
                                kb.op("act", [rxb], [r_junk], partial(A.activation, out=junk[:], in_=xb[:], func=AF.Square))
                                kb.op("dve", [r_junk], [r_ssx[i]], partial(V.tensor_reduce, out=ssx[:, i:i + 1], in_=junk[:], axis=AX.X, op=ALU.add))
                                rstd_from_ss(ssx[:, i:i + 1], 1024, [r_ssx[i]])
                                kb.op("dve", [rxb, r_ssx[i]], [rxn], partial(V.tensor_scalar, xnb[:], xb[:], ssx[:, i:i + 1], None, op0=ALU.mult))
                                for c in range(8):
                                    kb.op("pe", [rxn, r_idb], [r_pT], partial(PE.transpose, pT[:, c * 128:(c + 1) * 128], xnb[:, c * 128:(c + 1) * 128], idb[:]))
                                kb.op("act", [r_pT], [rhb[t]], partial(A.copy, hb[:, :, t * 128:(t + 1) * 128], pT[:].rearrange("p (c n) -> p c n", c=8)))
                                kb.prio = 0
                                bA, rA = pb[0 + (i % 2)], r_pb[0 + (i % 2)]
                                for c in range(8):
                                    kb.op("pe", [rhb[t]] + r_w4, [rA], partial(PE.matmul, bA[:], lhsT=hb[:, c, t * 128:(t + 1) * 128], rhs=wtm[:, c, 512:1024], start=(c == 0), stop=(c == 7)))
                                bU, rU = pb[6], r_pb[6]
                                for c in range(8):
                                    kb.op("pe", [rhb[t]] + r_w4, [rU], partial(PE.matmul, bU[:], lhsT=hb[:, c, t * 128:(t + 1) * 128], rhs=wfu[:, c, :], start=(c == 0), stop=(c == 7)))
                                kb.op("dve", [rU], [r_utm[i % 2]], partial(V.tensor_copy, utm[i % 2][:], bU[:]))
                                kb.dma(uTM[i * 128:(i + 1) * 128, :], utm[i % 2][:], [r_utm[i % 2]], [r_uTM[i]])
                                sqb, rsq = sq[i % 2], r_sq[i % 2]
                                s8, rs8 = ss8[i % 2], r_ss8[i % 2]
                                knb, rkn = kn[i % 2], r_kn[i % 2]
                                kb.op("act", [rA], [r_K[i]], partial(A.copy, Vs[:, i, :, 0:64], bA[:, 128:256].rearrange("p (g d) -> p g d", g=2)))
                                kb.op("act", [rA], [r_K[i]], partial(A.copy, Vw[:, i, :, 0:64], bA[:, 384:512].rearrange("p (g d) -> p g d", g=2)))
                                kb.op("act", [rA], [rsq], partial(A.activation, out=sqb[:], in_=bA[:], func=AF.Square))
                                kb.op("dve", [rsq], [rs8], partial(V.tensor_reduce, out=s8[:], in_=sqb[:].rearrange("p (g d) -> p g d", g=8), axis=AX.X, op=ALU.add))
                                rstd_from_ss(s8[:], 64, [rs8])
                                kb.op("dve", [rA, rs8], [rkn], partial(V.tensor_tensor,
                                    knb[:].rearrange("p (a g d) -> p a g d", a=2, g=2),
                                    bA[:].rearrange("p (a v g d) -> p a v g d", a=2, v=2, g=2)[:, :, 0, :, :],
                                    s8[:].rearrange("p (a v g) -> p a v g", a=2, v=2)[:, :, 0, :].unsqueeze(3).to_broadcast([128, 2, 2, 64]),
                                    op=ALU.mult))
                                for a in range(2):
                                    kb.op("pe", [rkn, r_idb], [r_pb[3]], partial(PE.matmul, pb[3][:, 256 + a * 128:256 + (a + 1) * 128], lhsT=knb[:, a * 128:(a + 1) * 128], rhs=idb[:], start=True, stop=True))
                                for g in range(2):
                                    kb.op("act", [r_pb[3], r_gcols, r_eb], [r_K[i]], partial(A.activation,
                                        out=KsT[g][0:64, i * 128:(i + 1) * 128], in_=pb[3][64 * g:64 * g + 64, 256:384], func=AF.Copy, scale=gcols[64 * g:64 * g + 64, 2:3]))
                                    kb.op("act", [r_pb[3], r_gcols], [r_K[i]], partial(A.activation,
                                        out=KwT[g][0:64, i * 128:(i + 1) * 128], in_=pb[3][64 * g:64 * g + 64, 384:512], func=AF.Copy, scale=gcols[64 * g:64 * g + 64, 3:4]))
                                if own:
                                    bQ, rQ = pb[2], r_pb[2]
                                    bG, rG = pb[3], r_pb[3]
                                    for c in range(8):
                                        kb.op("pe", [rhb[t]] + r_w4, [rQ], partial(PE.matmul, bQ[:], lhsT=hb[:, c, t * 128:(t + 1) * 128], rhs=wtm[:, c, 0:512], start=(c == 0), stop=(c == 7)))
                                    for c in range(8):
                                        kb.op("pe", [rhb[t]] + r_w4, [rG], partial(PE.matmul, bG[:, 0:24], lhsT=hb[:, c, t * 128:(t + 1) * 128], rhs=wtm[:, c, 1024:1048], start=(c == 0), stop=(c == 7)))
                                    kb.op("act", [rG], [r_gates[qi]], partial(A.copy, gates[:, qi, :], bG[:, 0:24]))
                                    kb.op("act", [rQ], [r_sqq], partial(A.activation, out=sqq[:], in_=bQ[:], func=AF.Square))
                                    kb.op("dve", [r_sqq], [r_ssq], partial(V.tensor_reduce, out=ssq[:], in_=sqq[:].rearrange("p (g d) -> p g d", g=8), axis=AX.X, op=ALU.add))
                                    rstd_from_ss(ssq[:], 64, [r_ssq])
                                    kb.op("dve", [rQ, r_ssq], [r_Q[qi]], partial(V.tensor_tensor,
                                        Qraw[:, qi, :].rearrange("p (g d) -> p g d", g=8), bQ[:].rearrange("p (g d) -> p g d", g=8),
                                        ssq[:].unsqueeze(2).to_broadcast([128, 8, 64]), op=ALU.mult))
                            for m in (4, 5):
                                bF, rF = pb[4 + (m % 2)], r_pb[4 + (m % 2)]
                                for c in range(8):
                                    lhs = wfc[:, c, m - 4, :]
                                    kb.op("pe", rhb + r_w4, [rF], partial(PE.matmul, bF[:], lhsT=lhs, rhs=hb[:, c, :], start=(c == 0), stop=(c == 7)))
                                kb.op("act", [rF], [r_kcv[sup]], partial(A.copy, kcvT[:, m - 4, :, sup * 32:(sup + 1) * 32], bF[:].rearrange("p (c r) -> p r c", r=16)))

                        if "uT" in dbg:
                            kb.dma(dout("uTM", [4096, 512], BF16), uTM, r_uTM, [])
                            kb.dma(dout("kcvT", [128, 2, 16, 256], BF16), kcvT[:], r_kcv, [])
                            kb.dma(dout("KsT0", [128, 4096], BF16), KsT[0][:], r_K + [r_eb], [])
                            kb.dma(dout("KwT1", [64, 4096], BF16), KwT[1][:], r_K, [])
                            kb.dma(dout("Vs", [128, NT, 2, 65], BF16), Vs[:], r_K, [])
                            kb.dma(dout("Qraw", [128, NOWN, 512], BF16), Qraw[:], r_Q, [])
                            kb.dma(dout("gates", [128, NOWN, 24]), gates[:], r_gates, [])
                            kb.dma(dout("ssx", [128, NT]), ssx[:], r_ssx, [])
                            kb.dma(dout("xn1", [128, 1024], BF16), xn[1][:], r_xn, [])
                            kb.dma(dout("hT", [128, 8, 512], BF16), hT[0][:], r_hT[0], [])
                            kb.dma(dout("wfu", [128, 8, 512], BF16), wfu[:], r_w4, [])
                            kb.dma(dout("wtm", [128, 8, 1048], BF16), wtm[:], r_w4, [])

                        if stage >= 2:
                            kb.barrier()
                            phase_compress(L4)
                    kb.barrier()
                    if stage >= 3:
                        phase_ssm(L3)
                kb.barrier()
                if stage >= 4:
                    phase_attn(L2)
            kb.barrier()
            if stage >= 5:
                MB = {}
                MB["tT"] = T(L1, "tT", [128, 8, 2048], BF16); MB["r_tTq"] = [Reg() for _ in range(NOWN)]
                MB["g2m"] = T(L1, "g2m", [128, 8]); MB["r_g2m"] = Reg()
                MB["wgs_"] = T(L1, "wgs_", [128, 8, 256]); MB["wus_"] = T(L1, "wus_", [128, 8, 256]); MB["wds_"] = T(L1, "wds_", [128, 2, 1024])
                MB["r_wgs"], MB["r_wus"], MB["r_wds"] = Reg(), Reg(), Reg()
                MB["wgb"] = [T(L1, "wgb%d" % i, [128, 8, 256], BF16) for i in range(2)]
                MB["wub"] = [T(L1, "wub%d" % i, [128, 8, 256], BF16) for i in range(2)]
                MB["wdb"] = [T(L1, "wdb%d" % i, [128, 2, 1024], BF16) for i in range(2)]
                MB["r_wb"] = [Reg(), Reg()]
                phase_post(L1)
                if stage >= 6:
                    phase_moe(L1)
        kb.barrier(("sp",))
    build_nc.sim_log = kb.sim_log
    return nc, dbg_out


def _consts(s):
    a = np.arange(128)
    c = {}
    c["c_ident"] = np.eye(128, dtype=np.float32)
    c["c_tlb"] = np.where(a[:, None] > a[None, :], -BIG, 0.0).astype(np.float32)
    c["c_sub"] = np.where(a[:, None] <= a[None, :], -BIG, 0.0).astype(np.float32)
    cc = np.arange(256)[:, None]
    ss = np.arange(64)[None, :]
    ov = ((cc * 16 < ss * 64 + 64) & (cc * 16 + 32 > ss * 64)).astype(np.float32)
    c["c_ov"] = np.concatenate([ov, np.ones((256, 1), np.float32)], axis=1)
    c["c_dmat"] = (16.0 * a[:, None] + 31.0 - a[None, :]).astype(np.float32)
    eb = np.zeros((64, 4096), np.float32)
    for blk in range(64):
        eb[blk, blk * 64:(blk + 1) * 64] = BIG
    c["c_eband"] = eb
    off = 2048 * (1 - s)
    pos = (np.arange(32)[None, :] * 128 + a[:, None])
    c["c_kbtok"] = np.where(pos < off, -BIG, 0.0).astype(np.float32)
    cb = (np.arange(2)[None, :] * 128 + a[:, None])
    c["c_kbcmp"] = np.where((cb < off // 16) | (cb > 254), -BIG, 0.0).astype(np.float32)
    f0 = np.zeros((128, 64), np.float32)
    f0[:, off // 64] = 1e9
    c["c_f0"] = f0
    c["c_iota"] = np.broadcast_to(np.arange(256, dtype=np.float32)[None, :], (128, 256)).copy()
    kall = np.concatenate([np.arange(7, -1, -1), -np.arange(1, 9), np.arange(1, 9)]).astype(np.float32)
    c["c_kall"] = np.broadcast_to(kall[None, :], (128, 24)).copy()
    kab = np.concatenate([16.0 * np.arange(8), np.arange(16)]).astype(np.float32)
    c["c_kab"] = np.broadcast_to(kab[None, :], (128, 24)).copy()
    c["c_tmask"] = ((a[None, :] // 16) >= (a[:, None] // 16)).astype(np.float32)
    return c


_WNAMES = ["norm1_g", "w_in", "lam_re", "lam_im", "log_step", "b_re", "b_im", "c_re", "c_im", "d_skip",
           "w_glu", "b_glu", "g_q", "g_kc", "g_ks", "g_kw", "pos_k", "pos_v", "w_ck1", "w_ck2", "w_cv1", "w_cv2",
           "out_g_ssm", "out_g_att", "w_out", "norm2_g", "w_grp", "b_grp", "w_exp", "b_exp", "w_gate", "w_up", "w_down"]


def make_in_maps(inputs, cores=range(8)):
    x = np.asarray(inputs["x"], dtype=np.float32)
    w = {k: np.ascontiguousarray(np.asarray(inputs[k], dtype=np.float32)[0]) for k in _WNAMES}
    maps = []
    for core in cores:
        b, s = core // 2, core % 2
        m = dict(w)
        if s == 1:
            m["xp"] = np.ascontiguousarray(x[b])
        else:
            m["xp"] = np.concatenate([np.zeros((2048, 1024), np.float32), x[b, :2048]], axis=0)
        m["xown"] = np.ascontiguousarray(x[b, 2048 * s:2048 * (s + 1)])
        m.update(_consts(s))
        maps.append(m)
    return maps


def kernel(**inputs):
    nc, _ = build_nc()
    maps = make_in_maps(inputs)
    res = run_bass_kernel_spmd(nc, maps, core_ids=list(range(8)))
    outp = np.empty((4, 4096, 1024), np.float32)
    for core in range(8):
        b, s = core // 2, core % 2
        outp[b, 2048 * s:2048 * (s + 1)] = res.results[core]["out"]
    return outp
```

```python
import math
from contextlib import ExitStack
from functools import partial

import numpy as np
import concourse.bass as bass
import concourse.mybir as mybir
from concourse.bass_utils import run_bass_kernel_spmd

F32 = mybir.dt.float32
BF16 = mybir.dt.bfloat16
I32 = mybir.dt.int32
ALU = mybir.AluOpType
AF = mybir.ActivationFunctionType
AX = mybir.AxisListType

BIG = 250.0
EPS = 1e-6
PI = math.pi
TCH = 256
NT = 32
NOWN = 16


class Reg:
    __slots__ = ("w", "r")

    def __init__(self):
        self.w = None
        self.r = []


class Op:
    __slots__ = ("idx", "eng", "fn", "preds", "dur", "lat", "ev", "nun", "succ", "ready", "fin", "is_dma", "ph", "prio")


def _free_elems(ap):
    n = 1
    for s in ap.shape[1:]:
        n *= int(s)
    return n


def _c0(f):
    return f()


def _c1(f):
    return f()


def _c2(f):
    return f()


def _c3(f):
    return f()


def _c4(f):
    return f()


def _c5(f):
    return f()


def _c6(f):
    return f()


def _c7(f):
    return f()


_CALLERS = [_c0, _c1, _c2, _c3, _c4, _c5, _c6, _c7]


class KB:
    NDMA = 24
    SCHED = True

    def __init__(self, nc, es):
        self.nc = nc
        self.eng = {"pe": nc.tensor, "act": nc.scalar, "dve": nc.vector,
                    "pool": nc.gpsimd, "sp": nc.sync}
        self.sems = {}
        self.cnt = {}
        for k in ("pe", "act", "dve", "pool"):
            self.sems[k] = es.enter_context(nc.semaphore("s_" + k))
            self.cnt[k] = 0
        for i in range(self.NDMA):
            k = "d%d" % i
            self.sems[k] = es.enter_context(nc.semaphore("s_" + k))
            self.cnt[k] = 0
        self.seen = {e: {} for e in self.eng}
        self.dma_rr = 0
        self.pending = []
        self.nops = 0
        self.phase = 0
        self.sim_log = []
        self.mute = False
        self.ck_limit = 99
        self.prio = 0

    def _wait(self, e, ev):
        if ev is None:
            return
        k, v = ev
        if e == "pe" and k == "pe":
            return
        if self.seen[e].get(k, 0) >= v:
            return
        self.eng[e].wait_ge(self.sems[k], v)
        self.seen[e][k] = v

    def _mk(self, e, reads, writes, fn, is_dma):
        o = Op()
        o.idx = self.nops
        self.nops += 1
        o.eng = e
        o.fn = fn
        o.ev = None
        o.is_dma = is_dma
        o.ph = self.phase
        o.prio = self.prio
        o.dur = 0.1
        o.lat = 0.0
        preds = {}
        for rg in reads:
            if rg.w is not None:
                preds[id(rg.w)] = rg.w
        for rg in writes:
            if rg.w is not None:
                preds[id(rg.w)] = rg.w
            for x in rg.r:
                preds[id(x)] = x
        preds.pop(id(o), None)
        o.preds = list(preds.values())
        for rg in reads:
            rg.r.append(o)
        for rg in writes:
            rg.w = o
            rg.r = []
        if self.mute:
            o.ev = ("pe", 0)
            o.preds = None
            return o
        self.pending.append(o)
        if not self.SCHED:
            self.flush()
        return o

    def op(self, e, reads, writes, fn):
        o = self._mk(e, reads, writes, fn, False)
        try:
            out = fn.keywords.get("out", fn.args[0] if fn.args else None)
            n = _free_elems(out)
            nm = fn.func.__name__
        except Exception:
            n, nm = 256, ""
        if e == "pe":
            o.dur = 0.035 + n / 2400.0
        elif e == "act":
            o.dur = 0.2 + n * 0.00105
        elif e == "dve":
            o.dur = 0.1 + n * (0.0029 if "scan" in nm else 0.0013)
        else:
            o.dur = 0.18 + n * 0.00212
        return o

    def dma(self, out, in_, reads, writes, q="sp", **kw):
        o = self._mk(q, reads, writes, partial(self.eng[q].dma_start, out=out, in_=in_, **kw), True)
        try:
            nb = 1
            for s in out.shape:
                nb *= int(s)
            nb *= 4
        except Exception:
            nb = 65536
        o.dur = 0.06
        o.lat = 2.0 + nb / 60000.0
        return o

    def _emit(self, o):
        e = o.eng
        if o.is_dma:
            dk = "d%d" % self.dma_rr
            self.dma_rr = (self.dma_rr + 1) % self.NDMA
            if self.cnt[dk] > 0:
                self._wait(e, (dk, self.cnt[dk]))
            for p in o.preds:
                self._wait(e, p.ev)
            ins = _CALLERS[o.ph](o.fn)
            self.cnt[dk] += 16
            ins.then_inc(self.sems[dk], 16)
            o.ev = (dk, self.cnt[dk])
        else:
            for p in o.preds:
                self._wait(e, p.ev)
            ins = _CALLERS[o.ph](o.fn)
            self.cnt[e] += 1
            ins.then_inc(self.sems[e], 1)
            o.ev = (e, self.cnt[e])
        o.fn = None
        o.preds = None
        o.succ = None

    def flush(self):
        ops = self.pending
        self.pending = []
        if not ops:
            return
        if len(ops) == 1:
            self._emit(ops[0])
            return
        for o in ops:
            o.succ = []
            o.ready = 0.0
        for o in ops:
            o.nun = 0
            for p in o.preds:
                if p.ev is None:
                    o.nun += 1
                    p.succ.append(o)
        tfree = {e: 0.0 for e in self.eng}
        cand = {e: [] for e in self.eng}
        for o in ops:
            if o.nun == 0:
                cand[o.eng].append(o)
        dma_fin = []
        nleft = len(ops)
        ndma = self.NDMA
        order = []
        while nleft:
            best = None
            bo = None
            for e, cl in cand.items():
                if not cl:
                    continue
                tf = tfree[e]
                if e == "sp" and len(dma_fin) >= ndma and dma_fin[-ndma] > tf:
                    tf = dma_fin[-ndma]
                for o in cl:
                    st = o.ready if o.ready > tf else tf
                    key = (st, -o.prio, o.idx)
                    if best is None or key < best:
                        best = key
                        bo = o
            o = bo
            st = best[0]
            e = o.eng
            cand[e].remove(o)
            tfree[e] = st + o.dur
            if o.is_dma:
                o.fin = st + o.lat
                dma_fin.append(o.fin)
            else:
                o.fin = tfree[e]
            order.append(o)
            for s in o.succ:
                if o.fin > s.ready:
                    s.ready = o.fin
                s.nun -= 1
                if s.nun == 0:
                    cand[s.eng].append(s)
            nleft -= 1
        self.sim_log.append((order[0].ph, len(order), max(o.fin for o in order), dict(tfree)))
        for o in order:
            self._emit(o)

    def ckpt(self, k):
        if k > self.ck_limit:
            self.mute = True

    def barrier(self, engines=("pe", "act", "dve", "pool", "sp")):
        self.mute = False
        self.flush()
        for e in engines:
            for k in self.sems:
                if self.cnt[k] > 0:
                    self._wait(e, (k, self.cnt[k]))


def build_nc(dbg=(), stage=99, ssm_stop=99, ck=99):
    nc = bass.Bass("TRN2", target_bir_lowering=False)
    D = {}

    def din(name, shape):
        D[name] = nc.dram_tensor(name, list(shape), F32, kind="ExternalInput").ap()
        return D[name]

    xp = din("xp", [4096, 1024])
    xown = din("xown", [2048, 1024])
    din("norm1_g", [1024]); din("w_in", [1024, 1816])
    din("lam_re", [32, 64]); din("lam_im", [32, 64]); din("log_step", [32])
    din("b_re", [32, 64, 16]); din("b_im", [32, 64, 16])
    din("c_re", [32, 16, 64]); din("c_im", [32, 16, 64]); din("d_skip", [32, 16])
    din("w_glu", [512, 512]); din("b_glu", [512])
    din("g_q", [64]); din("g_kc", [64]); din("g_ks", [64]); din("g_kw", [64])
    din("pos_k", [32, 64]); din("pos_v", [32, 64])
    din("w_ck1", [2048, 256]); din("w_ck2", [256, 64]); din("w_cv1", [2048, 256]); din("w_cv2", [256, 64])
    din("out_g_ssm", [512]); din("out_g_att", [512]); din("w_out", [1024, 1024])
    din("norm2_g", [1024]); din("w_grp", [1024, 4]); din("b_grp", [4]); din("w_exp", [1024, 32]); din("b_exp", [32])
    din("w_gate", [32, 1024, 256]); din("w_up", [32, 1024, 256]); din("w_down", [32, 256, 1024])
    din("c_ident", [128, 128]); din("c_tlb", [128, 128]); din("c_sub", [128, 128])
    din("c_ov", [256, 65]); din("c_dmat", [128, 128]); din("c_eband", [64, 4096])
    din("c_kbtok", [128, 32]); din("c_kbcmp", [128, 2]); din("c_f0", [128, 64])
    din("c_iota", [128, 256]); din("c_kall", [128, 24]); din("c_kab", [128, 24]); din("c_tmask", [128, 128])
    out = nc.dram_tensor("out", [2048, 1024], F32, kind="ExternalOutput").ap()
    x1d = nc.dram_tensor("x1d", [2048, 1024], F32, kind="Internal").ap()
    tTd = nc.dram_tensor("tTd", [128, 8, 2048], BF16, kind="Internal").ap()
    uTM = nc.dram_tensor("uTMd", [4096, 512], BF16, kind="Internal").ap()
    woutd = nc.dram_tensor("woutd", [128, 8, 1024], BF16, kind="Internal").ap()
    r_woutd = [Reg() for _ in range(8)]
    ysTd = nc.dram_tensor("ysTd", [128, 4, 2048], BF16, kind="Internal").ap()
    oattTd = nc.dram_tensor("oattTd", [128, 4, 2048], BF16, kind="Internal").ap()
    dbg_out = {}
    r_x1d = [Reg() for _ in range(NOWN)]
    r_tTd = [Reg() for _ in range(NOWN)]

    def dout(name, shape, dt=F32):
        dbg_out[name] = nc.dram_tensor("dbg_" + name, list(shape), dt, kind="ExternalOutput").ap()
        return dbg_out[name]

    with ExitStack() as es:
        kb = KB(nc, es)
        kb.ck_limit = ck
        V, G, A, PE = nc.vector, nc.gpsimd, nc.scalar, nc.tensor

        def T(st, name, shape, dt=F32):
            return st.enter_context(nc.sbuf_tensor(name, list(shape), dt))

        pb = [es.enter_context(nc.psum_tensor("pb%d" % i, [128, 512], F32)) for i in range(7)]
        r_pb = [Reg() for _ in range(7)]
        pT = es.enter_context(nc.psum_tensor("pT", [128, 1024], BF16))
        r_pT = Reg()

        idf = T(es, "idf", [128, 128]); r_idf = Reg()
        idb = T(es, "idb", [128, 128], BF16); r_idb = Reg()
        tlb4 = T(es, "tlb4", [128, 4, 128], BF16); r_tlb4 = Reg()
        sub4 = T(es, "sub4", [128, 4, 128], BF16); r_sub4 = Reg()
        dmat = T(es, "dmat", [128, 128]); r_dmat = Reg()
        ovb = T(es, "ovb", [128, 2, 65], BF16); r_ovb = Reg()
        kbtok = T(es, "kbtok", [128, 32]); r_kbtok = Reg()
        kbcmp = T(es, "kbcmp", [128, 2]); r_kbcmp = Reg()
        f0 = T(es, "f0", [128, 64]); r_f0 = Reg()
        gcols = T(es, "gcols", [128, 4]); r_gcols = Reg()
        comb = T(es, "comb", [128, NOWN, 32]); r_comb = [Reg() for _ in range(NOWN)]
        ss_ssm = T(es, "ss_ssm", [128, NOWN]); r_ss_ssm = [Reg() for _ in range(NOWN)]
        ss_att = T(es, "ss_att", [128, NOWN]); r_ss_att = [Reg() for _ in range(NOWN)]
        cst = T(es, "cst", [128, 512]); r_cst = Reg()
        onesf = T(es, "onesf", [128, 1]); r_onesf = Reg()

        kb.dma(idf[:], D["c_ident"], [], [r_idf])
        kb.dma(dmat[:], D["c_dmat"], [], [r_dmat])
        kb.dma(kbtok[:], D["c_kbtok"], [], [r_kbtok])
        kb.dma(kbcmp[:], D["c_kbcmp"], [], [r_kbcmp])
        kb.dma(f0[:], D["c_f0"], [], [r_f0])
        kb.op("dve", [r_idf], [r_idb], partial(V.tensor_copy, idb[:], idf[:]))
        kb.op("dve", [], [r_onesf], partial(V.memset, onesf[:], 1.0))
        kb.dma(cst[:, 0:128], D["c_tlb"], [], [r_cst])
        kb.dma(cst[:, 128:256], D["c_sub"], [], [r_cst])
        kb.op("dve", [r_cst], [r_tlb4], partial(V.tensor_copy, tlb4[:], cst[:, 0:128].unsqueeze(1).to_broadcast([128, 4, 128])))
        kb.op("dve", [r_cst], [r_sub4], partial(V.tensor_copy, sub4[:], cst[:, 128:256].unsqueeze(1).to_broadcast([128, 4, 128])))
        kb.dma(cst[:, 256:386].rearrange("p (j n) -> p j n", j=2), D["c_ov"].rearrange("(j p) n -> p j n", p=128), [], [r_cst])
        kb.op("dve", [r_cst], [r_ovb], partial(V.tensor_copy, ovb[:], cst[:, 256:386].rearrange("p (j n) -> p j n", j=2)))
        for gi, gname in enumerate(("g_q", "g_kc", "g_ks", "g_kw")):
            src = D[gname].rearrange("(d o) -> d o", o=1)
            kb.dma(gcols[0:64, gi:gi + 1], src, [], [r_gcols])
            kb.dma(gcols[64:128, gi:gi + 1], src, [], [r_gcols])
        kb.op("dve", [r_gcols], [r_gcols], partial(V.tensor_scalar, gcols[:, 0:1], gcols[:, 0:1], 0.125, None, op0=ALU.mult))

        rowst = T(es, "rowst", [16, 128]); r_rowst = Reg()

        def load_cols(dst, r_dst, src1d, n, bank=6):
            kb.dma(rowst[0:n, :], src1d.rearrange("(c p) -> c p", p=128), [], [r_rowst])
            kb.op("pe", [r_rowst, r_idf], [r_pb[bank]], partial(PE.transpose, pb[bank][:, 0:n], rowst[0:n, :], idf[0:n, 0:n]))
            kb.op("dve", [r_pb[bank]], [r_dst], partial(V.tensor_copy, dst, pb[bank][:, 0:n]))

        def rstd_from_ss(ss_ap, n, regs):
            kb.op("act", regs, regs, partial(A.activation, out=ss_ap, in_=ss_ap, func=AF.Sqrt, scale=1.0 / n, bias=EPS))
            kb.op("dve", regs, regs, partial(V.reciprocal, ss_ap, ss_ap))

        def rstd_lnexp(ss_ap, n, regs):
            kb.op("act", regs, regs, partial(A.activation, out=ss_ap, in_=ss_ap, func=AF.Ln, scale=1.0 / n, bias=EPS))
            kb.op("act", regs, regs, partial(A.activation, out=ss_ap, in_=ss_ap, func=AF.Exp, scale=-0.5))

        GK = 1.5957691216057308

        def gelu_ops(out_ap, y_ap, t1, t2, regs_in, r_t, r_out, shape_view=None):
            kb.op("act", regs_in, [r_t], partial(A.activation, out=t1, in_=y_ap, func=AF.Square, scale=math.sqrt(0.044715)))
            kb.op("dve", regs_in + [r_t], [r_t], partial(V.scalar_tensor_tensor, out=t1, in0=t1, scalar=1.0, in1=y_ap, op0=ALU.add, op1=ALU.mult))
            kb.op("act", [r_t], [r_t], partial(A.activation, out=t2, in_=t1, func=AF.Sigmoid, scale=GK))
            kb.op("dve", regs_in + [r_t], [r_out], partial(V.tensor_tensor, out_ap, y_ap, t2, op=ALU.mult))

        def phase_compress(L):
            kb.phase = 2
            with ExitStack() as LC:
                W1 = T(LC, "W1", [128, 32, 256], BF16); r_W1 = Reg()
                W2 = T(LC, "W2", [128, 2, 2, 64], BF16); r_W2 = Reg()
                posT = T(LC, "posT", [128, 32], BF16); r_posT = Reg()
                hcb = T(LC, "hcb", [128, 4]); r_hcb = Reg()
                hcT = T(LC, "hcT", [128, 2, 256], BF16); r_hcT = Reg()
                w1s = [T(LC, "w1s%d" % i, [128, 8, 256]) for i in range(2)]; r_w1s = [Reg(), Reg()]
                w2s = T(LC, "w2s", [128, 2, 2, 64]); r_w2s = Reg()
                pst = T(LC, "pst", [32, 128]); r_pst = Reg()
                yb = T(LC, "cyb", [128, 256]); r_yb = Reg()
                ct1 = T(LC, "ct1", [128, 256]); ct2 = T(LC, "ct2", [128, 256]); r_ct = Reg()
                csq = T(LC, "csq", [128, 64]); r_csq = Reg()
                css = T(LC, "css", [128, 1]); r_css = Reg()
                ckn = T(LC, "ckn", [128, 64], BF16); r_ckn = Reg()
                kb.op("pool", [], [r_hcT], partial(G.memset, hcT[:], 0.0))
                srcs = (D["w_ck1"].rearrange("(j d) h -> d j h", d=64), D["w_cv1"].rearrange("(j d) h -> d j h", d=64))
                for q4 in range(4):
                    ws, rw = w1s[q4 % 2], r_w1s[q4 % 2]
                    for kv in range(2):
                        kb.dma(ws[64 * kv:64 * kv + 64, :, :], srcs[kv][:, q4 * 8:(q4 + 1) * 8, :], [], [rw])
                    kb.op("dve", [rw], [r_W1], partial(V.tensor_copy, W1[:, q4 * 8:(q4 + 1) * 8, :], ws[:]))
                for kv, nm in enumerate(("w_ck2", "w_cv2")):
                    kb.dma(w2s[:, kv, :, :], D[nm].rearrange("(m p) d -> p m d", p=128), [], [r_w2s])
                kb.op("dve", [r_w2s], [r_W2], partial(V.tensor_copy, W2[:], w2s[:]))
                kb.dma(pst[:, 0:64], D["pos_k"], [], [r_pst])
                kb.dma(pst[:, 64:128], D["pos_v"], [], [r_pst])
                kb.op("pe", [r_pst, r_idf], [r_pb[6]], partial(PE.transpose, pb[6][:, 0:32], pst[:], idf[0:32, 0:32]))
                kb.op("dve", [r_pb[6]], [r_posT], partial(V.tensor_copy, posT[:], pb[6][:, 0:32]))
                for kv in range(2):
                    for m in range(2):
                        idx = kv * 2 + m
                        for j in range(32):
                            kb.op("pe", [r_W1, r_posT], [r_pb[5]], partial(PE.matmul,
                                pb[5][:, idx:idx + 1], lhsT=W1[64 * kv:64 * kv + 64, j, m * 128:(m + 1) * 128],
                                rhs=posT[64 * kv:64 * kv + 64, j:j + 1], start=(j == 0), stop=(j == 31)))
                kb.op("dve", [r_pb[5]], [r_hcb], partial(V.tensor_copy, hcb[:], pb[5][:, 0:4]))
                nb = 0
                for kv in range(2):
                    for g in range(2):
                        for m in range(2):
                            bk, rbk = pb[nb % 2], r_pb[nb % 2]; nb += 1
                            for j in range(32):
                                kb.op("pe", [r_W1] + r_kcv, [rbk], partial(PE.matmul,
                                    bk[:, 0:255], lhsT=W1[64 * kv:64 * kv + 64, j, m * 128:(m + 1) * 128],
                                    rhs=kcvT[64 * kv:64 * kv + 64, g, j % 16, (j // 16):(j // 16) + 255], start=(j == 0), stop=(j == 31)))
                            kb.op("act", [rbk, r_hcb], [r_yb], partial(A.activation,
                                out=yb[:, 0:255], in_=bk[:, 0:255], func=AF.Identity, bias=hcb[:, kv * 2 + m:kv * 2 + m + 1], scale=1.0))
                            gelu_ops(hcT[:, m, 0:255], yb[:, 0:255], ct1[:, 0:255], ct2[:, 0:255], [r_yb], r_ct, r_hcT)
                        for ct in range(2):
                            b2, rb2 = pb[2 + ct], r_pb[2 + ct]
                            for m in range(2):
                                kb.op("pe", [r_hcT, r_W2], [rb2], partial(PE.matmul,
                                    b2[:, 0:64], lhsT=hcT[:, m, ct * 128:(ct + 1) * 128], rhs=W2[:, kv, m, :], start=(m == 0), stop=(m == 1)))
                            if kv == 1:
                                kb.op("act", [rb2], [r_cmp], partial(A.copy, Vc[:, ct, g, 0:64], b2[:, 0:64]))
                            else:
                                kb.op("act", [rb2], [r_csq], partial(A.activation, out=csq[:], in_=b2[:, 0:64], func=AF.Square))
                                kb.op("dve", [r_csq], [r_css], partial(V.tensor_reduce, out=css[:], in_=csq[:], axis=AX.X, op=ALU.add))
                                rstd_from_ss(css[:], 64, [r_css])
                                kb.op("dve", [rb2, r_css], [r_ckn], partial(V.tensor_scalar, ckn[:], b2[:, 0:64], css[:, 0:1], None, op0=ALU.mult))
                                kb.op("pe", [r_ckn, r_idb], [r_pT], partial(PE.transpose, pT[0:64, 0:128], ckn[:], idb[:]))
                                kb.op("act", [r_pT, r_gcols], [r_cmp], partial(A.activation,
                                    out=KcT[g][:, ct * 128:(ct + 1) * 128], in_=pT[0:64, 0:128], func=AF.Copy, scale=gcols[0:64, 1:2]))
                if "cmp" in dbg:
                    kb.dma(dout("KcT0", [64, 256], BF16), KcT[0][:], [r_cmp], [])
                    kb.dma(dout("KcT1", [64, 256], BF16), KcT[1][:], [r_cmp], [])
                    kb.dma(dout("Vc", [128, 2, 2, 65], BF16), Vc[:], [r_cmp], [])
                kb.barrier()

        def phase_ssm(L):
            kb.phase = 3
            TS = 128
            with ExitStack() as LS:
                WB = T(LS, "WB", [128, 16, 2, 128], BF16); r_WB = Reg()
                Toep = T(LS, "Toep", [128, 32, 128], BF16); r_Toep = Reg()
                Qt = T(LS, "Qt", [128, 2, 2, 16, 128], BF16); r_Qt = Reg()
                cosT = T(LS, "cosT", [128, 16, TS]); sinT = T(LS, "sinT", [128, 16, TS]); r_tab = Reg()
                rcol = T(LS, "rcol", [128, 16]); cTs = T(LS, "cTs", [128, 2, 16]); r_par = Reg()
                bgl = T(LS, "bgl", [128, 4]); r_dsk = Reg()
                wglu = T(LS, "wglu", [128, 4, 512], BF16); r_wglu = Reg()
                with ExitStack() as LP:
                    lamre = T(LP, "lamre", [128, 16]); lamim = T(LP, "lamim", [128, 16]); lsb = T(LP, "lsb", [128, 32])
                    dlt = T(LP, "dlt", [128, 16]); th = T(LP, "th", [128, 16]); r_p = Reg()
                    cth = T(LP, "cth", [128, 16]); sth = T(LP, "sth", [128, 16])
                    er = T(LP, "er", [128, 16]); ei = T(LP, "ei", [128, 16]); cr = T(LP, "cr", [128, 16]); ci = T(LP, "ci", [128, 16])
                    tA = T(LP, "tA", [128, 16]); tB = T(LP, "tB", [128, 16]); tD = T(LP, "tD", [128, 16])
                    ki = T(LP, "ki", [128, 384], I32); kf = T(LP, "kf", [128, 384]); red = T(LP, "red", [128, 384]); ang = T(LP, "ang", [128, TS])
                    kab = T(LP, "kab", [128, 24]); kall = T(LP, "kall", [128, 24])
                    bre = T(LP, "bre", [128, 16, 16]); bim = T(LP, "bim", [128, 16, 16])
                    Bb = T(LP, "Bb", [128, 16, 2, 16]); tb1 = T(LP, "tb1", [128, 16, 16]); tb2 = T(LP, "tb2", [128, 16, 16])
                    cnat = T(LP, "cnat", [128, 2, 4, 64]); CTt = T(LP, "CTt", [64, 2, 4, 128]); CQ = T(LP, "CQ", [128, 16, 2, 16])
                    wgs = T(LP, "wgs", [128, 2, 512]); r_wgs = Reg()
                    angk = T(LP, "angk", [128, 16, 24]); magk = T(LP, "magk", [128, 16, 24])
                    ckk = T(LP, "ckk", [128, 16, 24]); skk = T(LP, "skk", [128, 16, 24])
                    PWr = T(LP, "PWr", [128, 16, 24]); PWi = T(LP, "PWi", [128, 16, 24])
                    tmA = T(LP, "tmA", [128, 16, 8, 16]); tmB = T(LP, "tmB", [128, 16, 8, 16])
                    PwB = T(LP, "PwB", [128, 2, 16, 128], BF16); PpB = T(LP, "PpB", [128, 2, 16, 128], BF16); QT2 = T(LP, "QT2", [128, 2, 16, 128], BF16)
                    tmask = T(LP, "tmask", [128, 128]); ToepF = T(LP, "ToepF", [128, 4, 128])
                    dsr = T(LP, "dsr", [32, 16]); dsr8 = T(LP, "dsr8", [32, 8, 16]); dskT = T(LP, "dskT", [128, 32])
                    r_s = Reg()

                    def S(e, fn):
                        kb.op(e, [r_s, r_p], [r_s, r_p], fn)

                    def sincos(cos_out, sin_out, a_ap, n):
                        K_, F_, R_ = ki[:, 0:n], kf[:, 0:n], red[:, 0:n]
                        S("dve", partial(V.tensor_scalar, K_, a_ap, 1.0 / (2 * PI), None, op0=ALU.mult))
                        S("dve", partial(V.tensor_copy, F_, K_))
                        S("dve", partial(V.scalar_tensor_tensor, out=R_, in0=F_, scalar=-2 * PI, in1=a_ap, op0=ALU.mult, op1=ALU.add))
                        S("dve", partial(V.tensor_scalar, F_, R_, PI, None, op0=ALU.is_gt))
                        S("dve", partial(V.scalar_tensor_tensor, out=R_, in0=F_, scalar=-2 * PI, in1=R_, op0=ALU.mult, op1=ALU.add))
                        S("dve", partial(V.tensor_scalar, F_, R_, -PI, None, op0=ALU.is_lt))
                        S("dve", partial(V.scalar_tensor_tensor, out=R_, in0=F_, scalar=2 * PI, in1=R_, op0=ALU.mult, op1=ALU.add))
                        S("act", partial(A.activation, out=sin_out, in_=R_, func=AF.Sin))
                        S("dve", partial(V.tensor_scalar, R_, R_, PI / 2, None, op0=ALU.add))
                        S("dve", partial(V.tensor_scalar, F_, R_, PI, None, op0=ALU.is_gt))
                        S("dve", partial(V.scalar_tensor_tensor, out=R_, in0=F_, scalar=-2 * PI, in1=R_, op0=ALU.mult, op1=ALU.add))
                        S("act", partial(A.activation, out=cos_out, in_=R_, func=AF.Sin))

                    load_cols(lamre[:], r_p, D["lam_re"].rearrange("g p -> (g p)"), 16)
                    load_cols(lamim[:], r_p, D["lam_im"].rearrange("g p -> (g p)"), 16)
                    load_cols(bgl[:], r_dsk, D["b_glu"], 4)
                    kb.dma(lsb[:], D["log_step"].rearrange("(o g) -> o g", o=1).partition_broadcast(128), [], [r_p])
                    kb.dma(kab[:], D["c_kab"], [], [r_p])
                    kb.dma(kall[:], D["c_kall"], [], [r_p])
                    kb.dma(tmask[:], D["c_tmask"], [], [r_p])
                    kb.dma(dsr[:], D["d_skip"], [], [r_p])
                    kb.dma(bre[:], D["b_re"].rearrange("g p h -> (g p) h").rearrange("(st q) h -> q st h", q=128), [], [r_p])
                    kb.dma(bim[:], D["b_im"].rearrange("g p h -> (g p) h").rearrange("(st q) h -> q st h", q=128), [], [r_p])
                    kb.dma(cnat[:, 0, :, :], D["c_re"].rearrange("g h p -> (g h) p").rearrange("(m q) p -> q m p", q=128), [], [r_p])
                    kb.dma(cnat[:, 1, :, :], D["c_im"].rearrange("g h p -> (g h) p").rearrange("(m q) p -> q m p", q=128), [], [r_p])
                    for hh in range(2):
                        kb.dma(wgs[:], D["w_glu"].rearrange("(m p) n -> p m n", p=128)[:, 2 * hh:2 * hh + 2, :], [], [r_wgs])
                        kb.op("pool", [r_wgs], [r_wglu], partial(G.tensor_copy, wglu[:, 2 * hh:2 * hh + 2, :], wgs[:]))
                    lsv = lsb[:].rearrange("p (st gg) -> p gg st", gg=2)
                    S("dve", partial(V.tensor_copy, dlt[0:64, :], lsv[0:64, 0, :]))
                    S("dve", partial(V.tensor_copy, dlt[64:128, :], lsv[64:128, 1, :]))
                    S("act", partial(A.activation, out=dlt[:], in_=dlt[:], func=AF.Exp))
                    S("dve", partial(V.tensor_tensor, th[:], lamim[:], dlt[:], op=ALU.mult))
                    S("dve", partial(V.tensor_tensor, tD[:], lamre[:], dlt[:], op=ALU.mult))
                    kb_ = kall[:].unsqueeze(1).to_broadcast([128, 16, 24])
                    S("dve", partial(V.tensor_tensor, angk[:], th[:].unsqueeze(2).to_broadcast([128, 16, 24]), kb_, op=ALU.mult))
                    S("dve", partial(V.tensor_tensor, magk[:], tD[:].unsqueeze(2).to_broadcast([128, 16, 24]), kb_, op=ALU.mult))
                    S("act", partial(A.activation, out=magk[:], in_=magk[:], func=AF.Exp))
                    sincos(ckk[:].rearrange("p a b -> p (a b)"), skk[:].rearrange("p a b -> p (a b)"), angk[:].rearrange("p a b -> p (a b)"), 384)
                    S("dve", partial(V.tensor_tensor, PWr[:], magk[:], ckk[:], op=ALU.mult))
                    S("dve", partial(V.tensor_tensor, PWi[:], magk[:], skk[:], op=ALU.mult))
                    kb.ckpt(1)
                    S("act", partial(A.activation, out=tA[:], in_=tD[:], func=AF.Exp))
                    sincos(cth[:], sth[:], th[:], 16)
                    S("dve", partial(V.tensor_tensor, er[:], tA[:], cth[:], op=ALU.mult))
                    S("dve", partial(V.tensor_scalar, er[:], er[:], -1.0, None, op0=ALU.add))
                    S("dve", partial(V.tensor_tensor, ei[:], tA[:], sth[:], op=ALU.mult))
                    S("dve", partial(V.tensor_tensor, tA[:], lamre[:], lamre[:], op=ALU.mult))
                    S("dve", partial(V.tensor_tensor, tB[:], lamim[:], lamim[:], op=ALU.mult))
                    S("dve", partial(V.tensor_tensor, tA[:], tA[:], tB[:], op=ALU.add))
                    S("dve", partial(V.reciprocal, tA[:], tA[:]))
                    S("dve", partial(V.tensor_tensor, cr[:], er[:], lamre[:], op=ALU.mult))
                    S("dve", partial(V.tensor_tensor, tB[:], ei[:], lamim[:], op=ALU.mult))
                    S("dve", partial(V.tensor_tensor, cr[:], cr[:], tB[:], op=ALU.add))
                    S("dve", partial(V.tensor_tensor, cr[:], cr[:], tA[:], op=ALU.mult))
                    S("dve", partial(V.tensor_tensor, ci[:], ei[:], lamre[:], op=ALU.mult))
                    S("dve", partial(V.tensor_tensor, tB[:], er[:], lamim[:], op=ALU.mult))
                    S("dve", partial(V.tensor_tensor, ci[:], ci[:], tB[:], op=ALU.subtract))
                    S("dve", partial(V.tensor_tensor, ci[:], ci[:], tA[:], op=ALU.mult))
                    crb = cr[:].unsqueeze(2).to_broadcast([128, 16, 16])
                    cib = ci[:].unsqueeze(2).to_broadcast([128, 16, 16])
                    S("dve", partial(V.tensor_tensor, tb1[:], bre[:], crb, op=ALU.mult))
                    S("dve", partial(V.tensor_tensor, tb2[:], bim[:], cib, op=ALU.mult))
                    S("dve", partial(V.tensor_tensor, Bb[:, :, 0, :], tb1[:], tb2[:], op=ALU.subtract))
                    S("dve", partial(V.tensor_tensor, tb1[:], bim[:], crb, op=ALU.mult))
                    S("dve", partial(V.tensor_tensor, tb2[:], bre[:], cib, op=ALU.mult))
                    S("dve", partial(V.tensor_tensor, Bb[:, :, 1, :], tb1[:], tb2[:], op=ALU.add))
                    kb.ckpt(2)
                    for ri in range(2):
                        for m in range(4):
                            kb.op("pe", [r_s, r_p, r_idf], [r_pb[5]], partial(PE.transpose, pb[5][0:64, m * 128:(m + 1) * 128], cnat[:, ri, m, :], idf[:]))
                        kb.op("dve", [r_pb[5]], [r_s], partial(V.tensor_copy, CTt[:, ri, :, :], pb[5][0:64, :].rearrange("p (m n) -> p m n", m=4)))
                    for ri in range(2):
                        cv = CTt[0:64, ri, :, :].rearrange("p m (gl h) -> p (m gl) h", h=16)
                        for gg in range(2):
                            S("act", partial(A.copy, CQ[64 * gg:64 * gg + 64, :, ri, :], cv[:, gg::2, :]))

                    def cmul(k0, bsrc, out_re, out_im, neg_im=False, asrc=None, extra_w=()):
                        def SO(e, fn):
                            kb.op(e, [r_s, r_p], [r_s, r_p] + list(extra_w), fn)
                        a_r, a_i = asrc if asrc is not None else (PWr, PWi)
                        ar = a_r[:, :, k0:k0 + 8].unsqueeze(3).to_broadcast([128, 16, 8, 16])
                        ai = a_i[:, :, k0:k0 + 8].unsqueeze(3).to_broadcast([128, 16, 8, 16])
                        br_ = bsrc[:, :, 0, :].unsqueeze(2).to_broadcast([128, 16, 8, 16])
                        bi_ = bsrc[:, :, 1, :].unsqueeze(2).to_broadcast([128, 16, 8, 16])
                        S("dve", partial(V.tensor_tensor, tmA[:], ar, br_, op=ALU.mult))
                        S("pool", partial(G.tensor_tensor, tmB[:], ai, bi_, op=ALU.mult))
                        SO("dve", partial(V.tensor_tensor, out_re, tmA[:], tmB[:], op=ALU.subtract))
                        S("dve", partial(V.tensor_tensor, tmA[:], ar, bi_, op=ALU.mult))
                        S("pool", partial(G.tensor_tensor, tmB[:], ai, br_, op=ALU.mult))
                        if neg_im:
                            SO("dve", partial(V.scalar_tensor_tensor, out=out_im, in0=tmA[:], scalar=-1.0, in1=tmB[:], op0=ALU.mult, op1=ALU.subtract))
                        else:
                            SO("dve", partial(V.tensor_tensor, out_im, tmA[:], tmB[:], op=ALU.add))

                    kb.ckpt(3)
                    v4 = lambda ap: ap.rearrange("p st (s h) -> p st s h", h=16)
                    r_PwB = Reg()
                    cmul(0, Bb, v4(PwB[:, 0, :, :]), v4(PwB[:, 1, :, :]), extra_w=[r_PwB])
                    for ri in range(2):
                        for q4 in range(4):
                            for k in range(4):
                                kb.op("pe", [r_PwB, r_idb], [r_pT], partial(PE.transpose, pT[:, k * 128:(k + 1) * 128], PwB[:, ri, q4 * 4 + k, :], idb[:]))
                            kb.op("act", [r_pT], [r_WB], partial(A.copy, WB[:, q4 * 4:(q4 + 1) * 4, ri, :], pT[:, 0:512].rearrange("p (k n) -> p k n", k=4)))
                    cmul(8, Bb, v4(PpB[:, 0, :, :]), v4(PpB[:, 1, :, :]))
                    S("dve", partial(V.tensor_scalar, tB[:], th[:], 8.0, None, op0=ALU.mult))
                    S("dve", partial(V.tensor_tensor, angk[:], tB[:].unsqueeze(2).to_broadcast([128, 16, 24]), kab[:].unsqueeze(1).to_broadcast([128, 16, 24]), op=ALU.mult))
                    sincos(ckk[:].rearrange("p a b -> p (a b)"), skk[:].rearrange("p a b -> p (a b)"), angk[:].rearrange("p a b -> p (a b)"), 384)
                    S("dve", partial(V.tensor_copy, Bb[:, :, 0, :], ckk[:, :, 8:24]))
                    S("dve", partial(V.tensor_copy, Bb[:, :, 1, :], skk[:, :, 8:24]))
                    cmul(0, Bb, cosT[:].rearrange("p st (a b) -> p st a b", b=16), sinT[:].rearrange("p st (a b) -> p st a b", b=16), asrc=(ckk, skk))
                    S("dve", partial(V.tensor_scalar, tA[:], tB[:], float(TS), None, op0=ALU.mult))
                    sincos(cTs[:, 0, :], cTs[:, 1, :], tA[:], 16)
                    kb.op("act", [r_s, r_p], [r_par, r_tab, r_s], partial(A.activation, out=rcol[:], in_=tD[:], func=AF.Exp, scale=8.0))
                    cmul(16, CQ, v4(QT2[:, 0, :, :]), v4(QT2[:, 1, :, :]), neg_im=True)
                    kb.op("pool", [r_s, r_p], [r_Qt, r_s], partial(G.memset, Qt[:], 0.0))
                    for gg in range(2):
                        sl = slice(64 * gg, 64 * gg + 64)
                        kb.op("dve", [r_s, r_p, r_Qt], [r_Qt, r_s], partial(V.tensor_copy, Qt[sl, gg, 0, :, :], QT2[sl, 0, :, :]))
                        kb.op("act", [r_s, r_p, r_Qt], [r_Qt, r_s], partial(A.activation, out=Qt[sl, gg, 1, :, :], in_=QT2[sl, 1, :, :], func=AF.Copy, scale=-1.0))
                    PZ = [tmA[:].rearrange("p a b c -> p (a b c)").bitcast(BF16).rearrange("p (r s n) -> p r s n", r=2, s=16),
                          tmB[:].rearrange("p a b c -> p (a b c)").bitcast(BF16).rearrange("p (r s n) -> p r s n", r=2, s=16)]
                    S("pool", partial(G.memset, tmA[:], 0.0))
                    S("pool", partial(G.memset, tmB[:], 0.0))
                    for gg in range(2):
                        sl = slice(64 * gg, 64 * gg + 64)
                        S("dve", partial(V.tensor_copy, PZ[gg][sl, :, :, :], PpB[sl, :, :, :]))
                    kb.ckpt(4)
                    kb.ckpt(5)
                    S("dve", partial(V.tensor_copy, dsr8[:], dsr[:].unsqueeze(1).to_broadcast([32, 8, 16])))
                    kb.op("pe", [r_s, r_p, r_idf], [r_pb[6]], partial(PE.transpose, pb[6][:, 0:32], dsr8[:].rearrange("p s h -> p (s h)"), idf[0:32, 0:32]))
                    kb.op("dve", [r_pb[6]], [r_s], partial(V.tensor_copy, dskT[:], pb[6][:, 0:32]))
                    kb.ckpt(6)
                    for g4 in range(8):
                        bk, rbk = pb[g4 % 2], r_pb[g4 % 2]
                        for k in range(4):
                            g = 4 * g4 + k
                            st, gg = g // 2, g % 2
                            kb.op("pe", [r_s, r_Qt], [rbk], partial(PE.matmul, bk[:, k * 128:(k + 1) * 128], lhsT=PZ[gg][:, 0, st, :], rhs=QT2[:, 0, st, :], start=True, stop=False))
                            kb.op("pe", [r_s, r_Qt], [rbk], partial(PE.matmul, bk[:, k * 128:(k + 1) * 128], lhsT=PZ[gg][:, 1, st, :], rhs=QT2[:, 1, st, :], start=False, stop=True))
                        kb.ckpt(6.5)
                        kb.op("dve", [r_s, r_p, rbk], [r_s, r_p], partial(V.tensor_tensor, ToepF[:], bk[:].rearrange("p (k n) -> p k n", k=4), tmask[:].unsqueeze(1).to_broadcast([128, 4, 128]), op=ALU.mult))
                        kb.ckpt(6.8)
                        for k in range(4):
                            g = 4 * g4 + k
                            kb.op("dve", [r_s, r_idf], [r_Toep, r_s], partial(V.scalar_tensor_tensor, out=Toep[:, g, :], in0=idf[:], scalar=dskT[:, g:g + 1], in1=ToepF[:, k, :], op0=ALU.mult, op1=ALU.add))
                    kb.ckpt(7)
                    kb.barrier()
                kb.phase = 4
                Ug = T(LS, "Ug", [128, 32, 256], BF16); r_Ug = [Reg() for _ in range(32)]
                Uc = T(LS, "Uc", [128, 4, 512], BF16); r_Uc = Reg()
                Uc2 = T(LS, "Uc2", [128, 32, 128], BF16); r_Uc2 = Reg()
                Zs = [T(LS, "Zs%d" % i, [128, 2, 256]) for i in range(2)]; r_Zs = [Reg(), Reg()]
                pc = T(LS, "pc", [128, 2, 256]); psn = T(LS, "psn", [128, 2, 256]); r_pc = Reg(); r_psn = Reg()
                Zh0_ = T(LS, "Zh0", [128, 2, 256])
                vb = [T(LS, "vb%d" % i, [128, 2, 256]) for i in range(2)]; r_vb = [Reg(), Reg()]
                carry = T(LS, "carry", [128, 2, 16]); r_carry = [Reg() for _ in range(16)]
                ctmp = T(LS, "ctmp", [128, 2, 16]); r_ctmp = [Reg() for _ in range(16)]
                lastq = T(LS, "lastq", [128, 16, 3, 2], BF16); r_lastq = [Reg() for _ in range(16)]
                qcs = [T(LS, "qcs%d" % i, [128, 3, 2, 258], BF16) for i in range(2)]; r_qcs = [Reg(), Reg()]
                gt1 = T(LS, "gt1", [128, 256]); r_gt = Reg()
                ygel = [T(LS, "ygel0", [128, 8, 256], BF16)] * 2; r_ygel = [[Reg() for _ in range(8)]] * 2
                selt = [T(LS, "selt0", [128, 1152], BF16)] * 2; r_selt = [Reg()] * 2
                yT = T(LS, "yT", [128, 4, 2048], BF16); r_yT = [Reg() for _ in range(4)]
                sg = gt1; r_sg = r_gt
                yo = T(LS, "yo", [128, 256]); r_yo = Reg()
                yst = [T(LS, "yst0", [128, 4, 256], BF16)] * 2; r_yst = [Reg()] * 2
                sqo = T(LS, "sqo", [128, 4, 256], BF16); r_sqo = Reg()
                onesb = T(LS, "onesb", [128, 2], BF16); r_onesb = Reg()
                kb.op("dve", [], [r_onesb], partial(V.memset, onesb[:], 1.0))
                r_pTh = [Reg(), Reg()]
                kb.op("dve", [], r_carry, partial(V.memset, carry[:], 0.0))
                yT3f = yT[:, 3, :].bitcast(F32)
                Zh = [Zh0_, yT3f[:, 0:512].rearrange("p (a n) -> p a n", a=2)]; r_Zh = [Reg(), Reg()]
                pcs = [pc, yT3f[:, 512:1024].rearrange("p (a n) -> p a n", a=2)]; r_pcs = [r_pc, Reg()]
                kb.op("pool", [], [r_selt[0]], partial(G.memset, selt[0][:], 0.0))
                uview = uTM.rearrange("(c j) ch -> c j ch", j=8)
                nev = 0
                for pas in range(2 if ssm_stop >= 1 else 0):
                    if pas == 1 and ssm_stop < 3:
                        break
                    for cbl in range(2):
                        cb = 2 * pas + cbl
                        kb.ckpt(8.0)
                        for jh in range(2):
                            kb.dma(Uc[:], uview[cb * 128:(cb + 1) * 128, jh * 4:jh * 4 + 4, :], [r_uTM[i] for i in range(cb * 8, cb * 8 + 8)], [r_Uc])
                            kb.op("pool", [r_Uc], [r_Uc2], partial(G.tensor_copy, Uc2[:, 0:16, :].rearrange("p g (j h) -> p j g h", j=8)[:, jh * 4:jh * 4 + 4, :, :], Uc[:, :, 0:256].rearrange("p j (g h) -> p j g h", h=16)))
                            kb.op("pool", [r_Uc], [r_Uc2], partial(G.tensor_copy, Uc2[:, 16:32, :].rearrange("p g (j h) -> p j g h", j=8)[:, jh * 4:jh * 4 + 4, :, :], Uc[:, :, 256:512].rearrange("p j (g h) -> p j g h", h=16)))
                        kb.ckpt(8.1)
                        for g4 in range(8):
                            hf = g4 % 2
                            for k in range(4):
                                g = 4 * g4 + k
                                kb.op("pe", [r_Uc2, r_idb], [r_pb[4 + hf]], partial(PE.matmul, pb[4 + hf][:, k * 128:(k + 1) * 128], lhsT=Uc2[:, g, :], rhs=idb[:], start=True, stop=True))
                            src = pb[4 + hf][:].rearrange("p (k n) -> p k n", k=4)
                            dst = Ug[:, 4 * g4:4 * g4 + 4, cbl * 128:(cbl + 1) * 128]
                            wr = [r_Ug[4 * g4 + k] for k in range(4)]
                            kb.op("act", [r_pb[4 + hf]], wr, partial(A.copy, dst, src))
                            nev += 1
                    for st in range(16 if ssm_stop >= 2 else 0):
                        b = st % 2
                        pc, r_pc = pcs[b], r_pcs[b]
                        bk, rbk = pb[b], r_pb[b]
                        for ri in range(2):
                            for gg in range(2):
                                kb.op("pe", [r_WB, r_Ug[2 * st + gg]], [rbk], partial(PE.matmul, bk[64 * gg:64 * gg + 64, ri * 256:(ri + 1) * 256], lhsT=WB[:, st, ri, 64 * gg:64 * gg + 64], rhs=Ug[:, 2 * st + gg, :], start=True, stop=True))
                        kb.op("act", [rbk], [r_Zs[b]], partial(A.copy, Zs[b][:].rearrange("p a n -> p (a n)"), bk[:]))
                        z4 = lambda ap: ap.rearrange("p a (s n) -> p a s n", s=2)
                        cb_ = cosT[:, st, :].unsqueeze(1).unsqueeze(1).to_broadcast([128, 2, 2, TS])
                        sb_ = sinT[:, st, :].unsqueeze(1).unsqueeze(1).to_broadcast([128, 2, 2, TS])
                        kb.op("pool", [r_Zs[b], r_tab], [r_pc], partial(G.tensor_tensor, z4(pc[:]), z4(Zs[b][:]), cb_, op=ALU.mult))
                        kb.op("dve", [r_Zs[b], r_tab], [r_psn], partial(V.tensor_tensor, z4(psn[:]), z4(Zs[b][:]), sb_, op=ALU.mult))
                        kb.op("pool", [r_pc, r_psn], [r_Zh[b]], partial(G.tensor_tensor, Zh[b][:, 0, :], pc[:, 0, :], psn[:, 1, :], op=ALU.add))
                        kb.op("pool", [r_pc, r_psn], [r_Zh[b]], partial(G.tensor_tensor, Zh[b][:, 1, :], pc[:, 1, :], psn[:, 0, :], op=ALU.subtract))
                        for seg in range(2):
                            cs = slice(seg * TS, (seg + 1) * TS)
                            for ri in range(2):
                                kb.op("dve", [r_Zh[b], r_par, r_carry[st]], [r_vb[b]], partial(V.tensor_tensor_scan,
                                    vb[b][:, ri, cs], rcol[:, st:st + 1].to_broadcast([128, TS]), Zh[b][:, ri, cs], carry[:, ri, st:st + 1], op0=ALU.mult, op1=ALU.add))
                            lc = seg * TS + TS - 1
                            vlr = vb[b][:, 0, lc:lc + 1]
                            vli = vb[b][:, 1, lc:lc + 1]
                            cc_, cs_ = cTs[:, 0, st:st + 1], cTs[:, 1, st:st + 1]
                            kb.op("dve", [r_vb[b], r_par], [r_ctmp[st]], partial(V.tensor_scalar, ctmp[:, 0, st:st + 1], vli, cs_, None, op0=ALU.mult))
                            kb.op("dve", [r_vb[b], r_par], [r_ctmp[st]], partial(V.tensor_scalar, ctmp[:, 1, st:st + 1], vlr, cs_, None, op0=ALU.mult))
                            kb.op("dve", [r_vb[b], r_par, r_ctmp[st]], [r_carry[st]], partial(V.scalar_tensor_tensor, out=carry[:, 0, st:st + 1], in0=vlr, scalar=cc_, in1=ctmp[:, 0, st:st + 1], op0=ALU.mult, op1=ALU.subtract))
                            kb.op("dve", [r_vb[b], r_par, r_ctmp[st]], [r_carry[st]], partial(V.scalar_tensor_tensor, out=carry[:, 1, st:st + 1], in0=vli, scalar=cc_, in1=ctmp[:, 1, st:st + 1], op0=ALU.mult, op1=ALU.add))
                        if pas == 0:
                            kb.op("dve", [r_vb[b], r_tab], [r_lastq[st]], partial(V.tensor_scalar, lastq[:, st, 0, :], vb[b][:, :, 2 * TS - 1], cosT[:, st, TS - 1:TS], None, op0=ALU.mult))
                            kb.op("dve", [r_vb[b], r_tab], [r_lastq[st]], partial(V.tensor_scalar, lastq[:, st, 1, :], vb[b][:, :, 2 * TS - 1], sinT[:, st, TS - 1:TS], -1.0, op0=ALU.mult, op1=ALU.mult))
                            kb.op("dve", [r_vb[b], r_tab], [r_lastq[st]], partial(V.tensor_scalar, lastq[:, st, 2, :], vb[b][:, :, 2 * TS - 1], cosT[:, st, TS - 1:TS], -1.0, op0=ALU.mult, op1=ALU.mult))
                            continue
                        q, rq = qcs[b], r_qcs[b]
                        kb.op("act", [r_lastq[st]], [rq], partial(A.copy, q[:, :, :, 0], lastq[:, st, :, :]))
                        kb.op("dve", [r_vb[b], r_tab], [rq], partial(V.tensor_tensor, z4(q[:, 0, :, 1:257]), z4(vb[b][:]), cb_, op=ALU.mult))
                        kb.op("act", [r_vb[b], r_pc], [r_pc], partial(A.activation, out=pc[:], in_=vb[b][:], func=AF.Copy, scale=-1.0))
                        kb.op("dve", [r_pc, r_tab], [rq], partial(V.tensor_tensor, z4(q[:, 1, :, 1:257]), z4(pc[:]), sb_, op=ALU.mult))
                        kb.op("dve", [r_pc, r_tab], [rq], partial(V.tensor_tensor, q[:, 2, 1, 1:257].rearrange("p (s n) -> p s n", s=2), pc[:, 1, :].rearrange("p (s n) -> p s n", s=2),
                                                                   cosT[:, st, :].unsqueeze(1).to_broadcast([128, 2, TS]), op=ALU.mult))
                        m = st // 4
                        yg_, ryg = ygel[m % 2], r_ygel[m % 2]
                        for gg in range(2):
                            g = 2 * st + gg
                            gl = g % 8
                            yb_, ryb = pb[2 + gg], r_pb[2 + gg]
                            kb.op("pe", [r_Toep, r_Ug[g]], [ryb], partial(PE.matmul, yb_[:, 0:256], lhsT=Toep[:, g, :], rhs=Ug[:, g, :], start=True, stop=False))
                            terms = ((0, q[:, 0, 0, 0:256]), (0, q[:, 1, 1, 0:256]), (1, q[:, 1, 0, 0:256]), (1, q[:, 2, 1, 0:256]))
                            for ti, (kq, rhs_) in enumerate(terms):
                                kb.op("pe", [r_Qt, rq], [ryb], partial(PE.matmul, yb_[:, 0:256], lhsT=Qt[:, gg, kq, st, :], rhs=rhs_, start=False, stop=(ti == 3)))
                            kb.op("act", [ryb], [r_gt], partial(A.activation, out=gt1[:], in_=yb_[:, 0:256], func=AF.Square, scale=math.sqrt(0.044715)))
                            kb.op("dve", [r_gt, ryb], [r_gt], partial(V.scalar_tensor_tensor, out=gt1[:], in0=gt1[:], scalar=1.0, in1=yb_[:, 0:256], op0=ALU.add, op1=ALU.mult))
                            kb.op("act", [r_gt], [r_gt], partial(A.activation, out=gt1[:], in_=gt1[:], func=AF.Sigmoid, scale=GK))
                            kb.op("dve", [r_gt, ryb], [ryg[gl]], partial(V.tensor_tensor, yg_[:, gl, :], yb_[:, 0:256], gt1[:], op=ALU.mult))
                        if st % 4 == 3:
                            for t in range(8):
                                se, rse = selt[t % 2], r_selt[t % 2]
                                kb.op("pool", [r_idb], [rse], partial(G.tensor_copy, se[:].rearrange("p (g x) -> p g x", x=144)[:, :, 0:16], idb[:, 16 * t:16 * t + 16].unsqueeze(1).to_broadcast([128, 8, 16])))
                                pk, rpk = pb[4 + t % 2], r_pb[4 + t % 2]
                                for gl in range(8):
                                    kb.op("pe", [rse, ryg[gl]], [rpk], partial(PE.matmul, pk[:, 0:256], lhsT=se[:, gl * 128:(gl + 1) * 128], rhs=yg_[:, gl, :], start=(gl == 0), stop=(gl == 7)))
                                dst = yT[:, m, :].rearrange("p (c t) -> p c t", t=8)[:, :, t]
                                if t % 2 == 0:
                                    kb.op("act", [rpk], [r_yT[m]], partial(A.copy, dst, pk[:, 0:256]))
                                else:
                                    kb.op("dve", [rpk], [r_yT[m]], partial(V.tensor_copy, dst, pk[:, 0:256]))
                Ucf = Uc[:].rearrange("p a b -> p (a b)").bitcast(F32)
                sg2 = [sg, Ucf[:, 0:256]]; r_sg2 = [r_sg, Reg()]
                yo2 = [yo, Ucf[:, 256:512]]; r_yo2 = [r_yo, Reg()]
                yst2 = [yst[0], Uc2[:, 0:8, :].rearrange("p (m a) n -> p m (a n)", m=4)]; r_yst2 = [r_yst[0], Reg()]
                for oc in range(8 if ssm_stop >= 4 else 0):
                    ts_ = slice(oc * 256, (oc + 1) * 256)
                    ysb, rys = yst2[oc % 2], r_yst2[oc % 2]
                    for mo in range(4):
                        gb, rgb = pb[mo % 2], r_pb[mo % 2]
                        sg, r_sg, yo, r_yo = sg2[mo % 2], r_sg2[mo % 2], yo2[mo % 2], r_yo2[mo % 2]
                        for mi in range(4):
                            kb.op("pe", [r_wglu, r_yT[mi]], [rgb], partial(PE.matmul, gb[:, 0:256], lhsT=wglu[:, mi, mo * 128:(mo + 1) * 128], rhs=yT[:, mi, ts_], start=(mi == 0), stop=(mi == 3)))
                        kb.op("act", [rgb, r_dsk], [r_sg], partial(A.activation, out=sg[:, 0:256], in_=gb[:, 0:256], func=AF.Sigmoid, bias=bgl[:, mo:mo + 1], scale=1.0))
                        kb.op("dve", [r_sg, r_yT[mo]], [r_yo], partial(V.tensor_tensor, yo[:, 0:256], yT[:, mo, ts_], sg[:, 0:256], op=ALU.mult))
                        kb.op("act", [r_yo], [rys], partial(A.copy, ysb[:, mo, :], yo[:, 0:256]))
                        kb.op("pool", [r_yo], [r_sqo], partial(G.tensor_tensor, sqo[:, mo, :], yo[:, 0:256], yo[:, 0:256], op=ALU.mult))
                    kb.dma(ysT[:, :, oc * 256:(oc + 1) * 256], ysb[:, :, :], [rys], [r_ysT[oc]])
                    for half in range(2):
                        qi = oc * 2 + half
                        for mo in range(4):
                            kb.op("pe", [r_sqo, r_onesb], [r_pb[6]], partial(PE.matmul,
                                pb[6][:, (qi % 8):(qi % 8) + 1], lhsT=sqo[:, mo, half * 128:(half + 1) * 128], rhs=onesb[:, 0:1], start=(mo == 0), stop=(mo == 3)))
                        kb.op("dve", [r_pb[6]], [r_ss_ssm[qi]], partial(V.tensor_copy, ss_ssm[:, qi:qi + 1], pb[6][:, (qi % 8):(qi % 8) + 1]))
                if "ssm" in dbg:
                    kb.dma(dout("ysT", [128, 4, 2048], BF16), ysT, r_ysT, [])
                    kb.dma(dout("ss_ssm", [128, NOWN]), ss_ssm[:], r_ss_ssm, [])
                kb.barrier()

        def phase_attn(L):
            kb.phase = 5
            with ExitStack() as LA:
                Qa = [T(LA, "Qa%d" % i, [128, 512], BF16) for i in range(2)]
                r_Qq = [Reg(), Reg()]; r_Qs = [Reg(), Reg()]
                cb4 = [T(LA, "cb4%d" % i, [128, 4, 128], BF16) for i in range(2)]; r_cb4 = [Reg(), Reg()]
                Pc2 = [T(LA, "Pc%d" % i, [128, 2, 512], BF16) for i in range(2)]; r_Pc2 = [[Reg(), Reg()] for _ in range(2)]
                Pw2 = [T(LA, "Pw%d" % i, [128, 5, 512], BF16) for i in range(2)]; r_Pw2 = [[Reg() for _ in range(5)] for _ in range(2)]
                Ps2 = [T(LA, "Ps%d" % i, [128, 32, 512], BF16) for i in range(2)]; r_Ps2 = [[Reg() for _ in range(32)] for _ in range(2)]
                impt = T(LA, "impt", [128, 64]); impw = T(LA, "impw", [128, 64]); fm = T(LA, "fm", [128, 64])
                m8 = T(LA, "m8", [128, 8]); rdi = T(LA, "rdi", [128, 4]); r_imp = Reg(); r_fm = Reg()
                selp = T(LA, "selp", [128, 128], BF16); r_selp = Reg()
                rden = T(LA, "rden", [128, 4]); wgt = T(LA, "wgt", [128, 4]); otmp = T(LA, "otmp", [128, 256]); r_fin = Reg()
                oatt2 = [T(LA, "oatt%d" % i, [128, 512]) for i in range(2)]; r_oatt2 = [Reg(), Reg()]
                oatt, r_oatt = oatt2[0], r_oatt2[0]
                oab = T(LA, "oab", [128, 512], BF16); osq = T(LA, "osq", [128, 512]); r_oab = Reg(); r_osq = Reg()
                ost = [T(LA, "ost%d" % i, [128, 4, 128], BF16) for i in range(2)]; r_ost = [Reg(), Reg()]
                kb.op("dve", [], [r_selp], partial(V.memset, selp[:], 0.0))
                kb.op("act", r_gates, r_gates, partial(A.activation, out=gates[:], in_=gates[:], func=AF.Sigmoid))
                ogc_a = T(LA, "ogc_a", [128, 8]); r_ogc_a = Reg()
                wos_a = [T(LA, "wos_a%d" % i, [128, 1024]) for i in range(2)]; r_wos_a = [Reg(), Reg()]
                wob_a = [T(LA, "wob_a%d" % i, [128, 1024], BF16) for i in range(2)]; r_wob_a = [Reg(), Reg()]
                load_cols(ogc_a[:, 0:4], r_ogc_a, D["out_g_ssm"], 4)
                load_cols(ogc_a[:, 4:8], r_ogc_a, D["out_g_att"], 4)
                for c in range(8):
                    kb.dma(wos_a[c % 2][:], D["w_out"][c * 128:(c + 1) * 128, :], [], [r_wos_a[c % 2]])
                    kb.op("dve", [r_wos_a[c % 2], r_ogc_a], [r_wob_a[c % 2]], partial(V.tensor_scalar, wob_a[c % 2][:], wos_a[c % 2][:], ogc_a[:, c:c + 1], None, op0=ALU.mult))
                    kb.dma(woutd[:, c, :], wob_a[c % 2][:], [r_wob_a[c % 2]], [r_woutd[c]])
                oT = [T(LA, "oT%d" % i, [128, 512]) for i in range(2)]; r_oT = [Reg(), Reg()]

                def finalize(acc, racc, br, first, g, qi):
                    av = acc[:, 0:260].rearrange("p (h e) -> p h e", h=4)
                    kb.op("dve", [racc], [r_fin], partial(V.tensor_scalar, rden[:], av[:, :, 64], 1e-30, None, op0=ALU.add))
                    kb.op("dve", [r_fin], [r_fin], partial(V.reciprocal, rden[:], rden[:]))
                    gv = gates[:, qi, g * 12:(g + 1) * 12].rearrange("p (h b) -> p h b", b=3)[:, :, br]
                    kb.op("dve", [r_fin, r_gates[qi]], [r_fin], partial(V.tensor_tensor, wgt[:], rden[:], gv, op=ALU.mult))
                    wb_ = wgt[:].unsqueeze(2).to_broadcast([128, 4, 64])
                    ov_ = oatt[:, g * 256:(g + 1) * 256].rearrange("p (h d) -> p h d", h=4)
                    if first:
                        kb.op("dve", [racc, r_fin], [r_oatt], partial(V.tensor_tensor, ov_, av[:, :, 0:64], wb_, op=ALU.mult))
                    else:
                        tv = otmp[:].rearrange("p (h d) -> p h d", h=4)
                        kb.op("dve", [racc, r_fin], [r_fin], partial(V.tensor_tensor, tv, av[:, :, 0:64], wb_, op=ALU.mult))
                        kb.op("dve", [r_fin, r_oatt], [r_oatt], partial(V.tensor_tensor, ov_, ov_, tv, op=ALU.add))

                sbank = [0]

                def next_s():
                    i = (0, 1, 6)[sbank[0] % 3]
                    sbank[0] += 1
                    return pb[i], r_pb[i]

                for n in range(NT - NOWN, NT):
                    qi = n - (NT - NOWN)
                    oatt, r_oatt = oatt2[qi % 2], r_oatt2[qi % 2]
                    kb.prio = 1
                    kb.op("pool", [r_fm], [r_fm], partial(G.memset, fm[:], 0.0))
                    kb.op("pool", [r_fm], [r_fm], partial(G.memset, fm[0:64, 2 * n - 1:2 * n + 1], 1e9))
                    kb.op("pool", [r_fm], [r_fm], partial(G.memset, fm[64:128, 2 * n:2 * n + 2], 1e9))
                    kb.op("dve", [r_fm, r_f0], [r_fm], partial(V.tensor_tensor, fm[:], fm[:], f0[:], op=ALU.max))
                    for j in range(2):
                        tau = float(128 * n - 2048 * j)
                        kb.op("dve", [r_dmat], [r_cb4[j]], partial(V.tensor_scalar,
                            cb4[j][:], dmat[:].unsqueeze(1).to_broadcast([128, 4, 128]), tau, -BIG, op0=ALU.is_gt, op1=ALU.mult))
                    for g in range(2):
                        qa = Qa[g]
                        kb.prio = 1
                        Pc, r_Pc, Pw, r_Pw, Ps, r_Ps = Pc2[g], r_Pc2[g], Pw2[g], r_Pw2[g], Ps2[g], r_Ps2[g]
                        for h in range(4):
                            kb.op("pe", [r_Q[qi], r_idb], [r_pT], partial(PE.transpose,
                                pT[0:64, h * 128:(h + 1) * 128], Qraw[:, qi, (4 * g + h) * 64:(4 * g + h + 1) * 64], idb[:]))
                        kb.op("act", [r_pT, r_gcols], [r_Qq[g]], partial(A.activation, out=qa[0:64, :], in_=pT[0:64, 0:512], func=AF.Copy, scale=gcols[0:64, 0:1]))
                        for j in range(2):
                            sb_, rsb = pb[3], r_pb[3]
                            kb.op("pe", [r_cmp, r_Qq[g]], [rsb], partial(PE.matmul, sb_[:], lhsT=KcT[g][:, j * 128:(j + 1) * 128], rhs=qa[0:64, :], start=True, stop=False))
                            kb.op("pe", [r_idb, r_cb4[j]], [rsb], partial(PE.matmul, sb_[:], lhsT=idb[:], rhs=cb4[j][:].rearrange("p h q -> p (h q)"), start=False, stop=True))
                            kb.op("act", [rsb, r_kbcmp], [r_Pc[j]], partial(A.activation, out=Pc[:, j, :], in_=sb_[:], func=AF.Exp, bias=kbcmp[:, j:j + 1], scale=1.0))
                        for h in range(4):
                            for j in range(2):
                                kb.op("pe", [r_Pc[j], r_cmp], [r_pb[2]], partial(PE.matmul,
                                    pb[2][:, h * 65:(h + 1) * 65], lhsT=Pc[:, j, h * 128:(h + 1) * 128], rhs=Vc[:, j, g, :], start=(j == 0), stop=(j == 1)))
                        for h in range(4):
                            for j in range(2):
                                kb.op("pe", [r_Pc[j], r_ovb], [r_pb[3]], partial(PE.matmul,
                                    pb[3][:, h * 65:(h + 1) * 65], lhsT=Pc[:, j, h * 128:(h + 1) * 128], rhs=ovb[:, j, :], start=(j == 0), stop=(j == 1)))
                        iv = pb[3][:, 0:260].rearrange("p (h e) -> p h e", h=4)
                        kb.op("dve", [r_pb[3]], [r_imp], partial(V.tensor_scalar, rdi[:], iv[:, :, 64], 1e-30, None, op0=ALU.add))
                        kb.op("dve", [r_imp], [r_imp], partial(V.reciprocal, rdi[:], rdi[:]))
                        kb.op("dve", [r_pb[3], r_imp], [r_imp], partial(V.tensor_scalar, impt[:], iv[:, 0, 0:64], rdi[:, 0:1], None, op0=ALU.mult))
                        for h in range(1, 4):
                            kb.op("dve", [r_pb[3], r_imp], [r_imp], partial(V.scalar_tensor_tensor,
                                out=impt[:], in0=iv[:, h, 0:64], scalar=rdi[:, h:h + 1], in1=impt[:], op0=ALU.mult, op1=ALU.add))
                        kb.op("dve", [r_imp, r_fm], [r_imp], partial(V.tensor_tensor, impt[:], impt[:], fm[:], op=ALU.max))
                        kb.op("dve", [r_imp], [r_imp], partial(V.max, out=m8[:], in_=impt[:]))
                        kb.op("dve", [r_imp], [r_imp], partial(V.match_replace, out=impw[:], in_to_replace=m8[:], in_values=impt[:], imm_value=-1e30))
                        kb.op("dve", [r_imp], [r_imp], partial(V.max, out=m8[:], in_=impw[:]))
                        kb.op("dve", [r_imp], [r_selp], partial(V.tensor_scalar, selp[:, 64:128], impt[:], m8[:, 7:8], -1.0, op0=ALU.is_ge, op1=ALU.add))
                        kb.op("pe", [r_selp, r_idb], [r_pT], partial(PE.transpose, pT[:, 512:640], selp[:], idb[:]))
                        kb.op("act", [r_pT], [r_Qs[g]], partial(A.copy,
                            qa[64:128, :].rearrange("p (h q) -> p h q", h=4), pT[64:128, 512:640].unsqueeze(1).to_broadcast([64, 4, 128])))
                        finalize(pb[2], r_pb[2], 0, True, g, qi)
                        kb.prio = 0
                        for kt in range(n + 1):
                            sb_, rsb = next_s()
                            kb.op("pe", [r_K[kt], r_eb, r_Qq[g], r_Qs[g]], [rsb], partial(PE.matmul,
                                sb_[:], lhsT=KsT[g][:, kt * 128:(kt + 1) * 128], rhs=qa[:, :], start=True, stop=(kt != n)))
                            if kt == n:
                                kb.op("pe", [r_idb, r_tlb4], [rsb], partial(PE.matmul, sb_[:], lhsT=idb[:], rhs=tlb4[:].rearrange("p h q -> p (h q)"), start=False, stop=True))
                            kb.op("act", [rsb, r_kbtok], [r_Ps[kt]], partial(A.activation, out=Ps[:, kt, :], in_=sb_[:], func=AF.Exp, bias=kbtok[:, kt:kt + 1], scale=1.0))
                        for kt in range(n + 1):
                            kb.op("pe", [r_Ps[kt], r_K[kt]], [r_pb[4]], partial(PE.matmul, pb[4][0:65, :], lhsT=Vs[:, kt, g, :], rhs=Ps[:, kt, :], start=(kt == 0), stop=(kt == n)))
                        kb.op("dve", [r_pb[4]], [r_oT[0]], partial(V.tensor_copy, oT[0][0:65, :], pb[4][0:65, :]))
                        for h in range(4):
                            kb.op("pe", [r_oT[0], r_idf], [r_pb[4]], partial(PE.transpose, pb[4][:, h * 65:(h + 1) * 65], oT[0][0:65, h * 128:(h + 1) * 128], idf[0:65, 0:65]))
                        kts = list(range(n - 4, n + 1))
                        for wi, kt in enumerate(kts):
                            sb_, rsb = next_s()
                            plain = (kt != n and kt != n - 4)
                            kb.op("pe", [r_K[kt], r_Qq[g]], [rsb], partial(PE.matmul,
                                sb_[:], lhsT=KwT[g][:, kt * 128:(kt + 1) * 128], rhs=qa[0:64, :], start=True, stop=plain))
                            if not plain:
                                bt, rbt = (tlb4, r_tlb4) if kt == n else (sub4, r_sub4)
                                kb.op("pe", [r_idb, rbt], [rsb], partial(PE.matmul, sb_[:], lhsT=idb[:], rhs=bt[:].rearrange("p h q -> p (h q)"), start=False, stop=True))
                            kb.op("act", [rsb, r_kbtok], [r_Pw[wi]], partial(A.activation, out=Pw[:, wi, :], in_=sb_[:], func=AF.Exp, bias=kbtok[:, kt:kt + 1], scale=1.0))
                        for wi, kt in enumerate(kts):
                            kb.op("pe", [r_Pw[wi], r_K[kt]], [r_pb[5]], partial(PE.matmul, pb[5][0:65, :], lhsT=Vw[:, kt, g, :], rhs=Pw[:, wi, :], start=(wi == 0), stop=(wi == 4)))
                        kb.op("dve", [r_pb[5]], [r_oT[1]], partial(V.tensor_copy, oT[1][0:65, :], pb[5][0:65, :]))
                        for h in range(4):
                            kb.op("pe", [r_oT[1], r_idf], [r_pb[5]], partial(PE.transpose, pb[5][:, h * 65:(h + 1) * 65], oT[1][0:65, h * 128:(h + 1) * 128], idf[0:65, 0:65]))
                        finalize(pb[4], r_pb[4], 1, False, g, qi)
                        finalize(pb[5], r_pb[5], 2, False, g, qi)
                    kb.op("dve", [r_oatt], [r_osq], partial(V.tensor_tensor, osq[:], oatt[:], oatt[:], op=ALU.mult))
                    kb.op("dve", [r_osq], [r_ss_att[qi]], partial(V.tensor_reduce, out=ss_att[:, qi:qi + 1], in_=osq[:], axis=AX.X, op=ALU.add))
                    for m in range(4):
                        kb.op("pe", [r_oatt, r_idf], [r_pb[5]], partial(PE.transpose, pb[5][:, m * 128:(m + 1) * 128], oatt[:, m * 128:(m + 1) * 128], idf[:]))
                    osb, ros = ost[qi % 2], r_ost[qi % 2]
                    kb.op("dve", [r_pb[5]], [ros], partial(V.tensor_copy, osb[:], pb[5][:].rearrange("p (m n) -> p m n", m=4)))
                    kb.dma(oattT[:, :, qi * 128:(qi + 1) * 128], osb[:], [ros], [r_oattT[qi]])
                    if "att" in dbg and qi == NOWN - 1:
                        pass
                if "att" in dbg:
                    kb.dma(dout("oattT", [128, 4, 2048], BF16), oattT, r_oattT, [])
                    kb.dma(dout("ss_att", [128, NOWN]), ss_att[:], r_ss_att, [])
                kb.barrier()

        def phase_post(L):
            kb.phase = 6
            with ExitStack() as LO:
                wout = T(LO, "wout", [128, 8, 1024], BF16); r_wout = Reg()
                wr = T(LO, "wr", [128, 8, 36]); r_wr = Reg()
                wrs = T(LO, "wrs", [128, 8, 36]); r_wrs = Reg()
                g2c = T(LO, "g2c", [128, 8]); r_g2c = Reg()
                brt = T(LO, "brt", [128, 36]); r_brt = Reg()
                ysb = [T(LO, "ysb%d" % i, [128, 4, 128], BF16) for i in range(2)]; r_ysb = [Reg(), Reg()]
                oab2 = [T(LO, "oab2%d" % i, [128, 4, 128], BF16) for i in range(2)]; r_oab2 = [Reg(), Reg()]
                xt = [T(LO, "xt%d" % i, [128, 1024]) for i in range(2)]; r_xt = [Reg(), Reg()]
                x1 = [T(LO, "x1%d" % i, [128, 1024]) for i in range(2)]; r_x1 = [Reg(), Reg()]
                rs2 = T(LO, "rs2", [128, 2]); r_rs2 = Reg()
                junk2s = [T(LO, "junk2%d" % i, [128, 1024]) for i in range(2)]; r_junk2s = [Reg(), Reg()]
                ssn = T(LO, "ssn", [128, 1]); r_ssn = Reg()
                t32s = [T(LO, "t32%d" % i, [128, 1024]) for i in range(2)]; r_t32s = [Reg(), Reg()]
                tTfs = [T(LO, "tTf%d" % i, [128, 8, 128]) for i in range(2)]; r_tTfs = [Reg(), Reg()]
                tTb = [T(LO, "tTb%d" % i, [128, 8, 128], BF16) for i in range(2)]; r_tTb = [Reg(), Reg()]
                lg = T(LO, "lg", [128, 36]); r_lg = Reg()
                rt = T(LO, "rt", [128, 64]); r_rt = Reg()
                m8r = T(LO, "m8r", [128, 8])
                load_cols(g2c[:], r_g2c, D["norm2_g"], 8)
                for c2 in range(2):
                    kb.dma(wout[:, 4 * c2:4 * c2 + 4, :], woutd[:, 4 * c2:4 * c2 + 4, :], r_woutd[4 * c2:4 * c2 + 4], [r_wout])
                load_cols(MB["g2m"][:], MB["r_g2m"], D["norm2_g"], 8)
                if stage >= 6:
                    moe_load_expert(0)
                kb.dma(wrs[:, :, 0:4], D["w_grp"].rearrange("(c p) n -> p c n", p=128), [], [r_wrs])
                kb.dma(wrs[:, :, 4:36], D["w_exp"].rearrange("(c p) n -> p c n", p=128), [], [r_wrs])
                kb.op("dve", [r_wrs, r_g2c], [r_wr], partial(V.tensor_tensor, wr[:], wrs[:], g2c[:].unsqueeze(2).to_broadcast([128, 8, 36]), op=ALU.mult))
                kb.dma(brt[:, 0:4], D["b_grp"].rearrange("(o n) -> o n", o=1).partition_broadcast(128), [], [r_brt])
                kb.dma(brt[:, 4:36], D["b_exp"].rearrange("(o n) -> o n", o=1).partition_broadcast(128), [], [r_brt])
                for qi in range(NOWN if stage >= 5.15 else 0):
                    b = qi % 2
                    junk2, r_junk2, t32, r_t32, tTf, r_tTf = junk2s[b], r_junk2s[b], t32s[b], r_t32s[b], tTfs[b], r_tTfs[b]
                    kb.prio = 1
                    kb.dma(ysb[b][:], ysT[:, :, qi * 128:(qi + 1) * 128], [r_ysT[qi // 2]], [r_ysb[b]])
                    kb.dma(oab2[b][:], oattT[:, :, qi * 128:(qi + 1) * 128], [r_oattT[qi]], [r_oab2[b]])
                    kb.dma(xt[b][:], xown[qi * 128:(qi + 1) * 128, :], [], [r_xt[b]])
                    for nb in range(2):
                        for m in range(4):
                            kb.op("pe", [r_ysb[b], r_wout], [r_pb[nb]], partial(PE.matmul, pb[nb][:], lhsT=ysb[b][:, m, :], rhs=wout[:, m, nb * 512:(nb + 1) * 512], start=(m == 0), stop=(m == 3)))
                        for m in range(4):
                            kb.op("pe", [r_oab2[b], r_wout], [r_pb[2 + nb]], partial(PE.matmul, pb[2 + nb][:], lhsT=oab2[b][:, m, :], rhs=wout[:, 4 + m, nb * 512:(nb + 1) * 512], start=(m == 0), stop=(m == 3)))
                    kb.op("dve", [r_ss_ssm[qi]], [r_rs2], partial(V.tensor_copy, rs2[:, 0:1], ss_ssm[:, qi:qi + 1]))
                    kb.op("dve", [r_ss_att[qi]], [r_rs2], partial(V.tensor_copy, rs2[:, 1:2], ss_att[:, qi:qi + 1]))
                    rstd_lnexp(rs2[:], 512, [r_rs2])
                    for nb in range(2):
                        sl = slice(nb * 512, (nb + 1) * 512)
                        kb.op("dve", [r_pb[nb], r_rs2, r_xt[b]], [r_x1[b]], partial(V.scalar_tensor_tensor,
                            out=x1[b][:, sl], in0=pb[nb][:], scalar=rs2[:, 0:1], in1=xt[b][:, sl], op0=ALU.mult, op1=ALU.add))
                        kb.op("dve", [r_pb[2 + nb], r_rs2, r_x1[b]], [r_x1[b]], partial(V.scalar_tensor_tensor,
                            out=x1[b][:, sl], in0=pb[2 + nb][:], scalar=rs2[:, 1:2], in1=x1[b][:, sl], op0=ALU.mult, op1=ALU.add))
                    kb.dma(x1d[qi * 128:(qi + 1) * 128, :], x1[b][:], [r_x1[b]], [r_x1d[qi]])
                    if stage < 5.25:
                        continue
                    kb.op("act", [r_x1[b]], [r_junk2], partial(A.activation, out=junk2[:], in_=x1[b][:], func=AF.Square))
                    kb.op("dve", [r_junk2], [r_ssn], partial(V.tensor_reduce, out=ssn[:], in_=junk2[:], axis=AX.X, op=ALU.add))
                    rstd_lnexp(ssn[:], 1024, [r_ssn])
                    kb.op("dve", [r_x1[b], r_ssn], [r_t32], partial(V.tensor_scalar, t32[:], x1[b][:], ssn[:, 0:1], None, op0=ALU.mult))
                    if stage < 5.26:
                        continue
                    for c in range(8):
                        bk = 4 + c // 4
                        kb.op("pe", [r_t32, r_idf], [r_pb[bk]], partial(PE.transpose, pb[bk][:, (c % 4) * 128:(c % 4 + 1) * 128], t32[:, c * 128:(c + 1) * 128], idf[:]))
                    if stage < 5.27:
                        continue
                    for hh in range(2):
                        kb.op("act", [r_pb[4 + hh]], [r_tTf], partial(A.copy, tTf[:, hh * 4:(hh + 1) * 4, :], pb[4 + hh][:].rearrange("p (c n) -> p c n", c=4)))
                        kb.op("dve", [r_tTf], [MB["r_tTq"][qi]], partial(V.tensor_copy, MB["tT"][:, hh * 4:(hh + 1) * 4, qi * 128:(qi + 1) * 128], tTf[:, hh * 4:(hh + 1) * 4, :]))
                    if stage < 5.28:
                        continue
                    if stage < 5.35:
                        continue
                    for c in range(8):
                        kb.op("pe", [r_tTf, r_wr], [r_pb[6]], partial(PE.matmul, pb[6][:, 0:36], lhsT=tTf[:, c, :], rhs=wr[:, c, :], start=(c == 0), stop=(c == 7)))
                    kb.op("dve", [r_pb[6], r_brt], [r_lg], partial(V.tensor_tensor, lg[:], pb[6][:, 0:36], brt[:], op=ALU.add))
                    if stage < 5.45:
                        continue
                    kb.prio = 0
                    def R(e, fn, extra_r=(), extra_w=()):
                        kb.op(e, [r_rt, r_lg] + list(extra_r), [r_rt] + list(extra_w), fn)
                    gl = lg[:, 0:4]
                    el = lg[:, 4:36].rearrange("p (g j) -> p g j", g=4)
                    gmax, ngmax, goh, gex, gsum = rt[:, 0:1], rt[:, 1:2], rt[:, 2:6], rt[:, 6:10], rt[:, 10:11]
                    els, msk, ee, wsum, nv1 = rt[:, 16:24], rt[:, 24:32], rt[:, 32:40], rt[:, 11:12], rt[:, 12:13]
                    R("dve", partial(V.tensor_reduce, out=gmax, in_=gl, axis=AX.X, op=ALU.max))
                    R("dve", partial(V.tensor_scalar, ngmax, gmax, -1.0, None, op0=ALU.mult))
                    R("dve", partial(V.tensor_scalar, goh, gl, gmax, None, op0=ALU.is_ge))
                    R("act", partial(A.activation, out=gex, in_=gl, func=AF.Exp, bias=ngmax, scale=1.0))
                    R("dve", partial(V.tensor_reduce, out=gsum, in_=gex, axis=AX.X, op=ALU.add))
                    R("dve", partial(V.reciprocal, gsum, gsum))
                    R("dve", partial(V.tensor_scalar, els, el[:, 0, :], goh[:, 0:1], None, op0=ALU.mult))
                    for g in range(1, 4):
                        R("dve", partial(V.scalar_tensor_tensor, out=els, in0=el[:, g, :], scalar=goh[:, g:g + 1], in1=els, op0=ALU.mult, op1=ALU.add))
                    R("dve", partial(V.max, out=m8r[:], in_=els))
                    R("dve", partial(V.tensor_scalar, msk, els, m8r[:, 1:2], None, op0=ALU.is_ge))
                    R("dve", partial(V.tensor_scalar, nv1, m8r[:, 0:1], -1.0, None, op0=ALU.mult))
                    R("act", partial(A.activation, out=ee, in_=els, func=AF.Exp, bias=nv1, scale=1.0))
                    R("dve", partial(V.tensor_tensor, ee, ee, msk, op=ALU.mult))
                    R("dve", partial(V.tensor_reduce, out=wsum, in_=ee, axis=AX.X, op=ALU.add))
                    R("dve", partial(V.reciprocal, wsum, wsum))
                    R("dve", partial(V.tensor_tensor, wsum, wsum, gsum, op=ALU.mult))
                    R("dve", partial(V.tensor_scalar, ee, ee, wsum, None, op0=ALU.mult))
                    for g in range(4):
                        R("dve", partial(V.tensor_scalar, comb[:, qi, g * 8:(g + 1) * 8], ee, goh[:, g:g + 1], None, op0=ALU.mult), extra_w=[r_comb[qi]])
                if "post" in dbg:
                    dx1 = dout("x1", [2048, 1024])
                    for qi in range(NOWN):
                        kb.dma(dx1[qi * 128:(qi + 1) * 128, :], x1d[qi * 128:(qi + 1) * 128, :], [r_x1d[qi]], [])
                    kb.dma(dout("comb", [128, NOWN, 32]), comb[:], r_comb, [])
                kb.barrier()

        def moe_load_expert(e):
            s2 = e % 2
            g2b = MB["g2m"][:].unsqueeze(2).to_broadcast([128, 8, 256])
            kb.dma(MB["wgs_"][:], D["w_gate"][e].rearrange("(c p) f -> p c f", p=128), [], [MB["r_wgs"]])
            kb.dma(MB["wus_"][:], D["w_up"][e].rearrange("(c p) f -> p c f", p=128), [], [MB["r_wus"]])
            kb.dma(MB["wds_"][:], D["w_down"][e].rearrange("(c p) d -> p c d", p=128), [], [MB["r_wds"]])
            kb.op("pool", [MB["r_wgs"], MB["r_g2m"]], [MB["r_wb"][s2]], partial(G.tensor_tensor, MB["wgb"][s2][:], MB["wgs_"][:], g2b, op=ALU.mult))
            kb.op("pool", [MB["r_wus"], MB["r_g2m"]], [MB["r_wb"][s2]], partial(G.tensor_tensor, MB["wub"][s2][:], MB["wus_"][:], g2b, op=ALU.mult))
            kb.op("act", [MB["r_wds"]], [MB["r_wb"][s2]], partial(A.copy, MB["wdb"][s2][:], MB["wds_"][:]))

        def phase_moe(L):
            kb.phase = 7
            acc = T(L, "acc", [128, NOWN, 1024]); r_acc = [Reg() for _ in range(NOWN)]
            tT, r_tTq = MB["tT"], MB["r_tTq"]
            wgb, wub, wdb, r_wb = MB["wgb"], MB["wub"], MB["wdb"], MB["r_wb"]
            abf = T(L, "abf", [128, 2, 2048], BF16); r_abf = [Reg() for _ in range(4)]
            sil = [T(L, "sil%d" % i, [128, 512]) for i in range(2)]; r_sil = [Reg(), Reg()]
            for qi in range(NOWN):
                kb.dma(acc[:, qi, :], x1d[qi * 128:(qi + 1) * 128, :], [r_x1d[qi]], [r_acc[qi]])
            k = 0
            for e in range(32):
                s2 = e % 2
                if e > 0:
                    moe_load_expert(e)
                for nt in range(4):
                    for f in range(2):
                        bg, rbg = pb[k % 2], r_pb[k % 2]
                        bu, rbu = pb[2 + k % 2], r_pb[2 + k % 2]
                        sl_, rsl = sil[k % 2], r_sil[k % 2]
                        k += 1
                        for c in range(8):
                            kb.op("pe", [r_wb[s2]] + r_tTq[4 * nt:4 * nt + 4], [rbg], partial(PE.matmul, bg[:], lhsT=wgb[s2][:, c, f * 128:(f + 1) * 128], rhs=tT[:, c, nt * 512:(nt + 1) * 512], start=(c == 0), stop=(c == 7)))
                        for c in range(8):
                            kb.op("pe", [r_wb[s2]] + r_tTq[4 * nt:4 * nt + 4], [rbu], partial(PE.matmul, bu[:], lhsT=wub[s2][:, c, f * 128:(f + 1) * 128], rhs=tT[:, c, nt * 512:(nt + 1) * 512], start=(c == 0), stop=(c == 7)))
                        kb.op("act", [rbg], [rsl], partial(A.activation, out=sl_[:], in_=bg[:], func=AF.Silu))
                        kb.op("dve", [rsl, rbu], [r_abf[nt]], partial(V.tensor_tensor, abf[:, f, nt * 512:(nt + 1) * 512], sl_[:], bu[:], op=ALU.mult))
                for tt in range(NOWN):
                    for nb in range(2):
                        bd, rbd = pb[4 + (tt * 2 + nb) % 3], r_pb[4 + (tt * 2 + nb) % 3]
                        for f in range(2):
                            kb.op("pe", [r_abf[tt // 4], r_wb[s2]], [rbd], partial(PE.matmul, bd[:], lhsT=abf[:, f, tt * 128:(tt + 1) * 128], rhs=wdb[s2][:, f, nb * 512:(nb + 1) * 512], start=(f == 0), stop=(f == 1)))
                        kb.op("dve", [rbd, r_comb[tt], r_acc[tt]], [r_acc[tt]], partial(V.scalar_tensor_tensor,
                            out=acc[:, tt, nb * 512:(nb + 1) * 512], in0=bd[:], scalar=comb[:, tt, e:e + 1], in1=acc[:, tt, nb * 512:(nb + 1) * 512], op0=ALU.mult, op1=ALU.add))
            for qi in range(NOWN):
                kb.dma(out[qi * 128:(qi + 1) * 128, :], acc[:, qi, :], [r_acc[qi]], [])

        with ExitStack() as L1:
            ysT = ysTd; r_ysT = [Reg() for _ in range(8)]
            oattT = oattTd; r_oattT = [Reg() for _ in range(NOWN)]
            with ExitStack() as L2:
                KW = [T(L2, "KW%d" % g, [128, 2, 4096], BF16) for g in range(2)]
                KsT = [KW[g][:, 0, :] for g in range(2)]
                KwT = [KW[g][0:64, 1, :] for g in range(2)]
                Vs = T(L2, "Vs", [128, NT, 2, 65], BF16)
                Vw = T(L2, "Vw", [128, NT, 2, 65], BF16)
                r_K = [Reg() for _ in range(NT)]
                KcT = [T(L2, "KcT%d" % g, [64, 256], BF16) for g in range(2)]
                Vc = T(L2, "Vc", [128, 2, 2, 65], BF16)
                r_cmp = Reg()
                Qraw = T(L2, "Qraw", [128, NOWN, 512], BF16); r_Q = [Reg() for _ in range(NOWN)]
                gates = T(L2, "gates", [128, NOWN, 24]); r_gates = [Reg() for _ in range(NOWN)]
                r_eb = Reg()
                kb.op("pool", [], [r_K[i] for i in range(NT)], partial(G.memset, Vs[:], 1.0))
                kb.op("pool", [], [r_K[i] for i in range(NT)], partial(G.memset, Vw[:], 1.0))
                kb.op("pool", [], [r_cmp], partial(G.memset, Vc[:], 1.0))
                for g in range(2):
                    kb.op("pool", [], [r_cmp], partial(G.memset, KcT[g][:], 0.0))
                with ExitStack() as L3:
                    r_uTM = [Reg() for _ in range(NT)]
                    with ExitStack() as L4:
                        wtm = T(L4, "wtm", [128, 8, 1048], BF16)
                        wfu = T(L4, "wfu", [128, 8, 512], BF16)
                        wfc = T(L4, "wfc", [128, 8, 2, 128], BF16)
                        r_w4 = [Reg() for _ in range(4)]
                        g1c = T(L4, "g1c", [128, 8]); r_g1c = Reg()
                        kcvT = T(L4, "kcvT", [128, 2, 16, 256], BF16); r_kcv = [Reg() for _ in range(8)]
                        load_cols(g1c[:], r_g1c, D["norm1_g"], 8)
                        gr = T(L4, "gr", [128, 256]); r_gr = Reg()
                        for a_, nm_ in enumerate(("g_ks", "g_kw")):
                            for g_ in range(2):
                                kb.dma(gr[:, a_ * 128 + g_ * 64:a_ * 128 + g_ * 64 + 64], D[nm_].rearrange("(o n) -> o n", o=1).partition_broadcast(128), [], [r_gr])
                        if "uT" in dbg:
                            kb.dma(dout("g1c", [128, 8]), g1c[:], [r_g1c], [])
                            kb.dma(dout("gcols", [128, 4]), gcols[:], [r_gcols], [])
                        with ExitStack() as L5:
                            wst = [T(L5, "wst%d" % i, [128, 1816]) for i in range(2)]
                            r_wst = [Reg(), Reg()]
                            for q4 in range(4):
                                ebs = wst[q4 % 2]; r_ebs = r_wst[q4 % 2]
                                kb.dma(ebs[0:64, 0:1024], D["c_eband"][:, q4 * 1024:(q4 + 1) * 1024], [], [r_ebs])
                                for g in range(2):
                                    kb.op("pool", [r_ebs], [r_eb], partial(G.tensor_copy, KsT[g][64:128, q4 * 1024:(q4 + 1) * 1024], ebs[0:64, 0:1024]))
                            for c in range(8):
                                ws = wst[c % 2]; rw = r_wst[c % 2]
                                kb.dma(ws[:], D["w_in"][c * 128:(c + 1) * 128, :], [], [rw])
                                sc = g1c[:, c:c + 1]
                                e1, e2 = ("dve", V), ("pool", G)
                                kb.op("dve", [rw, r_g1c], [r_w4[0]], partial(V.tensor_scalar, wfu[:, c, :], ws[:, 0:512], sc, None, op0=ALU.mult))
                                kb.op("act", [rw, r_g1c], [r_w4[1]], partial(A.activation, out=wtm[:, c, 0:512], in_=ws[:, 512:1024], func=AF.Copy, scale=sc))
                                kb.op("dve", [rw, r_g1c], [r_w4[2]], partial(V.tensor_scalar, wtm[:, c, 512:1048], ws[:, 1280:1816], sc, None, op0=ALU.mult))
                                kb.op("dve", [rw, r_g1c], [r_w4[3]], partial(V.tensor_scalar,
                                    wfc[:, c, :, :].rearrange("p g (k d) -> p g k d", k=2),
                                    ws[:, 1024:1280].rearrange("p (k g d) -> p g k d", k=2, g=2), sc, None, op0=ALU.mult))
                            kb.barrier()
                        kb.phase = 1
                        xs = [T(L4, "xs%d" % i, [128, 1024]) for i in range(2)]; r_xs = [Reg(), Reg()]
                        utm = [T(L4, "utm%d" % i, [128, 512], BF16) for i in range(2)]; r_utm = [Reg(), Reg()]
                        junk = T(L4, "junk", [128, 1024]); r_junk = Reg()
                        ssx = T(L4, "ssx", [128, NT]); r_ssx = [Reg() for _ in range(NT)]
                        xn = [T(L4, "xn%d" % i, [128, 1024], BF16) for i in range(2)]; r_xn = [Reg(), Reg()]
                        hT = [T(L4, "hT%d" % i, [128, 8, 512], BF16) for i in range(2)]
                        r_hT = [[Reg() for _ in range(4)] for _ in range(2)]
                        sq = [T(L4, "sq%d" % i, [128, 512]) for i in range(2)]; r_sq = [Reg(), Reg()]
                        ss8 = [T(L4, "ss8%d" % i, [128, 8]) for i in range(2)]; r_ss8 = [Reg(), Reg()]
                        kn = [T(L4, "kn%d" % i, [128, 256], BF16) for i in range(2)]; r_kn = [Reg(), Reg()]
                        sqq = T(L4, "sqq", [128, 512]); r_sqq = Reg()
                        ssq = T(L4, "ssq", [128, 8]); r_ssq = Reg()

                        kb.dma(xs[0][:], xp[0:128, :], [], [r_xs[0]])
                        for sup in range(8):
                            hb = hT[sup % 2]; rhb = r_hT[sup % 2]
                            for t in range(4):
                                i = sup * 4 + t
                                xb = xs[i % 2]; rxb = r_xs[i % 2]
                                if i + 1 < NT:
                                    kb.dma(xs[(i + 1) % 2][:], xp[(i + 1) * 128:(i + 2) * 128, :], [], [r_xs[(i + 1) % 2]])
                                own = i >= NT - NOWN
                                qi = i - (NT - NOWN)
                                xnb = xn[i % 2]; rxn = r_xn[i % 2]
                                kb.prio = 1
                                kb.op("act", [rxb], [r_junk], partial(A.activation, out=junk[:], in_=xb[:], func=AF.Square))
                                kb.op("dve", [r_junk], [r_ssx[i]], partial(V.tensor_reduce, out=ssx[:, i:i + 1], in_=junk[:], axis=AX.X, op=ALU.add))
                                rstd_from_ss(ssx[:, i:i + 1], 1024, [r_ssx[i]])
                                kb.op("dve", [rxb, r_ssx[i]], [rxn], partial(V.tensor_scalar, xnb[:], xb[:], ssx[:, i:i + 1], None, op0=ALU.mult))
                                for c in range(8):
                                    kb.op("pe", [rxn, r_idb], [r_pT], partial(PE.transpose, pT[:, c * 128:(c + 1) * 128], xnb[:, c * 128:(c + 1) * 128], idb[:]))
                                kb.op("act", [r_pT], [rhb[t]], partial(A.copy, hb[:, :, t * 128:(t + 1) * 128], pT[:].rearrange("p (c n) -> p c n", c=8)))
                                kb.prio = 0
                                bA, rA = pb[0 + (i % 2)], r_pb[0 + (i % 2)]
                                for c in range(8):
                                    kb.op("pe", [rhb[t]] + r_w4, [rA], partial(PE.matmul, bA[:], lhsT=hb[:, c, t * 128:(t + 1) * 128], rhs=wtm[:, c, 512:1024], start=(c == 0), stop=(c == 7)))
                                bU, rU = pb[6], r_pb[6]
                                for c in range(8):
                                    kb.op("pe", [rhb[t]] + r_w4, [rU], partial(PE.matmul, bU[:], lhsT=hb[:, c, t * 128:(t + 1) * 128], rhs=wfu[:, c, :], start=(c == 0), stop=(c == 7)))
                                kb.op("dve", [rU], [r_utm[i % 2]], partial(V.tensor_copy, utm[i % 2][:], bU[:]))
                                kb.dma(uTM[i * 128:(i + 1) * 128, :], utm[i % 2][:], [r_utm[i % 2]], [r_uTM[i]])
                                sqb, rsq = sq[i % 2], r_sq[i % 2]
                                s8, rs8 = ss8[i % 2], r_ss8[i % 2]
                                knb, rkn = kn[i % 2], r_kn[i % 2]
                                kb.op("act", [rA], [r_K[i]], partial(A.copy, Vs[:, i, :, 0:64], bA[:, 128:256].rearrange("p (g d) -> p g d", g=2)))
                                kb.op("act", [rA], [r_K[i]], partial(A.copy, Vw[:, i, :, 0:64], bA[:, 384:512].rearrange("p (g d) -> p g d", g=2)))
                                kb.op("act", [rA], [rsq], partial(A.activation, out=sqb[:], in_=bA[:], func=AF.Square))
                                kb.op("dve", [rsq], [rs8], partial(V.tensor_reduce, out=s8[:], in_=sqb[:].rearrange("p (g d) -> p g d", g=8), axis=AX.X, op=ALU.add))
                                rstd_from_ss(s8[:], 64, [rs8])
                                kb.op("dve", [rA, rs8], [rkn], partial(V.tensor_tensor,
                                    knb[:].rearrange("p (a g d) -> p a g d", a=2, g=2),
                                    bA[:].rearrange("p (a v g d) -> p a v g d", a=2, v=2, g=2)[:, :, 0, :, :],
                                    s8[:].rearrange("p (a v g) -> p a v g", a=2, v=2)[:, :, 0, :].unsqueeze(3).to_broadcast([128, 2, 2, 64]),
                                    op=ALU.mult))
                                kb.op("dve", [rkn, r_gr], [rkn], partial(V.tensor_tensor, knb[:], knb[:], gr[:], op=ALU.mult))
                                for a in range(2):
                                    kb.op("pe", [rkn, r_idb], [r_pb[3]], partial(PE.matmul, pb[3][:, 256 + a * 128:256 + (a + 1) * 128], lhsT=knb[:, a * 128:(a + 1) * 128], rhs=idb[:], start=True, stop=True))
                                for g in range(2):
                                    kb.op("act", [r_pb[3], r_eb], [r_K[i]], partial(A.copy,
                                        KW[g][0:64, :, i * 128:(i + 1) * 128], pb[3][64 * g:64 * g + 64, 256:512].rearrange("p (a n) -> p a n", a=2)))
                                if own:
                                    bQ, rQ = pb[2], r_pb[2]
                                    bG, rG = pb[3], r_pb[3]
                                    for c in range(8):
                                        kb.op("pe", [rhb[t]] + r_w4, [rQ], partial(PE.matmul, bQ[:], lhsT=hb[:, c, t * 128:(t + 1) * 128], rhs=wtm[:, c, 0:512], start=(c == 0), stop=(c == 7)))
                                    for c in range(8):
                                        kb.op("pe", [rhb[t]] + r_w4, [rG], partial(PE.matmul, bG[:, 0:24], lhsT=hb[:, c, t * 128:(t + 1) * 128], rhs=wtm[:, c, 1024:1048], start=(c == 0), stop=(c == 7)))
                                    kb.op("act", [rG], [r_gates[qi]], partial(A.copy, gates[:, qi, :], bG[:, 0:24]))
                                    kb.op("act", [rQ], [r_sqq], partial(A.activation, out=sqq[:], in_=bQ[:], func=AF.Square))
                                    kb.op("dve", [r_sqq], [r_ssq], partial(V.tensor_reduce, out=ssq[:], in_=sqq[:].rearrange("p (g d) -> p g d", g=8), axis=AX.X, op=ALU.add))
                                    rstd_from_ss(ssq[:], 64, [r_ssq])
                                    kb.op("dve", [rQ, r_ssq], [r_Q[qi]], partial(V.tensor_tensor,
                                        Qraw[:, qi, :].rearrange("p (g d) -> p g d", g=8), bQ[:].rearrange("p (g d) -> p g d", g=8),
                                        ssq[:].unsqueeze(2).to_broadcast([128, 8, 64]), op=ALU.mult))
                            for m in (4, 5):
                                bF, rF = pb[4 + (m % 2)], r_pb[4 + (m % 2)]
                                for c in range(8):
                                    lhs = wfc[:, c, m - 4, :]
                                    kb.op("pe", rhb + r_w4, [rF], partial(PE.matmul, bF[:], lhsT=lhs, rhs=hb[:, c, :], start=(c == 0), stop=(c == 7)))
                                kb.op("act", [rF], [r_kcv[sup]], partial(A.copy, kcvT[:, m - 4, :, sup * 32:(sup + 1) * 32], bF[:].rearrange("p (c r) -> p r c", r=16)))

                        if "uT" in dbg:
                            kb.dma(dout("uTM", [4096, 512], BF16), uTM, r_uTM, [])
                            kb.dma(dout("kcvT", [128, 2, 16, 256], BF16), kcvT[:], r_kcv, [])
                            kb.dma(dout("KsT0", [128, 4096], BF16), KsT[0][:], r_K + [r_eb], [])
                            kb.dma(dout("KwT1", [64, 4096], BF16), KwT[1][:], r_K, [])
                            kb.dma(dout("Vs", [128, NT, 2, 65], BF16), Vs[:], r_K, [])
                            kb.dma(dout("Qraw", [128, NOWN, 512], BF16), Qraw[:], r_Q, [])
                            kb.dma(dout("gates", [128, NOWN, 24]), gates[:], r_gates, [])
                            kb.dma(dout("ssx", [128, NT]), ssx[:], r_ssx, [])
                            kb.dma(dout("xn1", [128, 1024], BF16), xn[1][:], r_xn, [])
                            kb.dma(dout("hT", [128, 8, 512], BF16), hT[0][:], r_hT[0], [])
                            kb.dma(dout("wfu", [128, 8, 512], BF16), wfu[:], r_w4, [])
                            kb.dma(dout("wtm", [128, 8, 1048], BF16), wtm[:], r_w4, [])

                        if stage >= 2:
                            kb.barrier()
                            phase_compress(L4)
                    kb.barrier()
                    if stage >= 3:
                        phase_ssm(L3)
                kb.barrier()
                if stage >= 4:
                    phase_attn(L2)
            kb.barrier()
            if stage >= 5:
                MB = {}
                MB["tT"] = T(L1, "tT", [128, 8, 2048], BF16); MB["r_tTq"] = [Reg() for _ in range(NOWN)]
                MB["g2m"] = T(L1, "g2m", [128, 8]); MB["r_g2m"] = Reg()
                MB["wgs_"] = T(L1, "wgs_", [128, 8, 256]); MB["wus_"] = T(L1, "wus_", [128, 8, 256]); MB["wds_"] = T(L1, "wds_", [128, 2, 1024])
                MB["r_wgs"], MB["r_wus"], MB["r_wds"] = Reg(), Reg(), Reg()
                MB["wgb"] = [T(L1, "wgb%d" % i, [128, 8, 256], BF16) for i in range(2)]
                MB["wub"] = [T(L1, "wub%d" % i, [128, 8, 256], BF16) for i in range(2)]
                MB["wdb"] = [T(L1, "wdb%d" % i, [128, 2, 1024], BF16) for i in range(2)]
                MB["r_wb"] = [Reg(), Reg()]
                phase_post(L1)
                if stage >= 6:
                    phase_moe(L1)
        kb.barrier(("sp",))
    build_nc.sim_log = kb.sim_log
    return nc, dbg_out


def _consts(s):
    a = np.arange(128)
    c = {}
    c["c_ident"] = np.eye(128, dtype=np.float32)
    c["c_tlb"] = np.where(a[:, None] > a[None, :], -BIG, 0.0).astype(np.float32)
    c["c_sub"] = np.where(a[:, None] <= a[None, :], -BIG, 0.0).astype(np.float32)
    cc = np.arange(256)[:, None]
    ss = np.arange(64)[None, :]
    ov = ((cc * 16 < ss * 64 + 64) & (cc * 16 + 32 > ss * 64)).astype(np.float32)
    c["c_ov"] = np.concatenate([ov, np.ones((256, 1), np.float32)], axis=1)
    c["c_dmat"] = (16.0 * a[:, None] + 31.0 - a[None, :]).astype(np.float32)
    eb = np.zeros((64, 4096), np.float32)
    for blk in range(64):
        eb[blk, blk * 64:(blk + 1) * 64] = BIG
    c["c_eband"] = eb
    off = 2048 * (1 - s)
    pos = (np.arange(32)[None, :] * 128 + a[:, None])
    c["c_kbtok"] = np.where(pos < off, -BIG, 0.0).astype(np.float32)
    cb = (np.arange(2)[None, :] * 128 + a[:, None])
    c["c_kbcmp"] = np.where((cb < off // 16) | (cb > 254), -BIG, 0.0).astype(np.float32)
    f0 = np.zeros((128, 64), np.float32)
    f0[:, off // 64] = 1e9
    c["c_f0"] = f0
    c["c_iota"] = np.broadcast_to(np.arange(256, dtype=np.float32)[None, :], (128, 256)).copy()
    kall = np.concatenate([np.arange(7, -1, -1), -np.arange(1, 9), np.arange(1, 9)]).astype(np.float32)
    c["c_kall"] = np.broadcast_to(kall[None, :], (128, 24)).copy()
    kab = np.concatenate([16.0 * np.arange(8), np.arange(16)]).astype(np.float32)
    c["c_kab"] = np.broadcast_to(kab[None, :], (128, 24)).copy()
    c["c_tmask"] = ((a[None, :] // 16) >= (a[:, None] // 16)).astype(np.float32)
    return c


_WNAMES = ["norm1_g", "w_in", "lam_re", "lam_im", "log_step", "b_re", "b_im", "c_re", "c_im", "d_skip",
           "w_glu", "b_glu", "g_q", "g_kc", "g_ks", "g_kw", "pos_k", "pos_v", "w_ck1", "w_ck2", "w_cv1", "w_cv2",
           "out_g_ssm", "out_g_att", "w_out", "norm2_g", "w_grp", "b_grp", "w_exp", "b_exp", "w_gate", "w_up", "w_down"]


def make_in_maps(inputs, cores=range(8)):
    x = np.asarray(inputs["x"], dtype=np.float32)
    w = {k: np.ascontiguousarray(np.asarray(inputs[k], dtype=np.float32)[0]) for k in _WNAMES}
    maps = []
    for core in cores:
        b, s = core // 2, core % 2
        m = dict(w)
        if s == 1:
            m["xp"] = np.ascontiguousarray(x[b])
        else:
            m["xp"] = np.concatenate([np.zeros((2048, 1024), np.float32), x[b, :2048]], axis=0)
        m["xown"] = np.ascontiguousarray(x[b, 2048 * s:2048 * (s + 1)])
        m.update(_consts(s))
        maps.append(m)
    return maps


def kernel(**inputs):
    nc, _ = build_nc()
    maps = make_in_maps(inputs)
    res = run_bass_kernel_spmd(nc, maps, core_ids=list(range(8)))
    outp = np.empty((4, 4096, 1024), np.float32)
    for core in range(8):
        b, s = core // 2, core % 2
        outp[b, 2048 * s:2048 * (s + 1)] = res.results[core]["out"]
    return outp
```
